# Optimizing a Trainium2 kernel written in Bass

```python
import math
import jax, jax.numpy as jnp
from jax import lax
import numpy as np

D_MODEL = 1024
BATCH = 8
SEQ = 4096
DEPTH = 4

HEAD_DIM = 64
GRID_W = 64
NA_HEADS = 4
NA_KH = 8
NA_KW = 16
NA_QC = 16
DIL_GROUPS = ((128, 1), (512, 4), (2048, 16))
DIL_HEADS_PER_GROUP = 2
DIL_HEADS = 6
DIL_QB = 64
DIFF_HEADS = 4
DIFF_DIM = 48
DIFF_QB = 128
A_W = NA_HEADS * HEAD_DIM
B_W = DIL_HEADS * HEAD_DIM
B_OUT = DIL_HEADS_PER_GROUP * HEAD_DIM
C_W = DIFF_HEADS * 2 * DIFF_DIM
N_BRANCH = 3
IN_COLS = 3 * A_W + 3 * B_W + 3 * C_W + N_BRANCH * D_MODEL
N_EXPERTS = 32
TOP_K = 4
D_EXPERT = 1024
SWIGLU_ALPHA = 1.702
SWIGLU_LIMIT = 7.0
ROPE_THETA = 10000.0
LN_EPS = 1e-5
NEG_INF = -1e30
DEEPNORM_ALPHA = (2 * DEPTH) ** 0.25
DEEPNORM_BETA = (8 * DEPTH) ** -0.25

kernel_name = "hybrid_natten_dilated_diffattn_moe_encoder"

F32 = jnp.float32


def rope_tables(seq, dim):
    inv = 1.0 / (ROPE_THETA ** (jnp.arange(0, dim, 2, dtype=F32) / dim))
    ang = jnp.arange(seq, dtype=F32)[:, None] * inv[None, :]
    return jnp.cos(ang), jnp.sin(ang)


def apply_rope(x, cos, sin):
    x1, x2 = jnp.split(x, 2, axis=-1)
    c = cos.astype(x.dtype)
    s = sin.astype(x.dtype)
    return jnp.concatenate([x1 * c - x2 * s, x2 * c + x1 * s], axis=-1)


def layer_norm(x, g, b):
    xf = x.astype(F32)
    mu = jnp.mean(xf, axis=-1, keepdims=True)
    var = jnp.mean(jnp.square(xf - mu), axis=-1, keepdims=True)
    y = (xf - mu) * lax.rsqrt(var + LN_EPS) * g.astype(F32) + b.astype(F32)
    return y.astype(x.dtype)


def neighborhood_attention(q, k, v, rpb):
    B, H, S, hd = q.shape
    rows = S // GRID_W
    kh = min(NA_KH, rows)
    ncb = GRID_W // NA_QC
    kbw = NA_QC + NA_KW
    r = np.arange(rows)
    row_idx = np.clip(r - kh // 2, 0, rows - kh)[:, None] + np.arange(kh)[None, :]
    cb = np.arange(ncb)
    col_idx = np.clip(cb * NA_QC - NA_KW // 2, 0, GRID_W - kbw)[:, None] + np.arange(kbw)[None, :]
    q_col = cb[:, None] * NA_QC + np.arange(NA_QC)[None, :]
    c_start = np.clip(q_col - NA_KW // 2, 0, GRID_W - NA_KW)
    kc = col_idx[:, None, :]
    col_ok = (kc >= c_start[:, :, None]) & (kc < c_start[:, :, None] + NA_KW)
    row_off = row_idx - r[:, None] + NA_KH - 1
    col_off = np.clip(kc - q_col[:, :, None] + NA_KW - 1, 0, 2 * NA_KW - 2)
    bias = rpb[:, row_off[:, None, None, :, None], col_off[None, :, :, None, :]]
    qg = q.reshape(B, H, rows, ncb, NA_QC, hd)
    ridx = row_idx[:, :, None, None]
    cidx = col_idx[None, None, :, :]
    kg = k.reshape(B, H, rows, GRID_W, hd)[:, :, ridx, cidx]
    vg = v.reshape(B, H, rows, GRID_W, hd)[:, :, ridx, cidx]
    s = jnp.einsum('bhrnqd,bhrinkd->bhrnqik', qg, kg, preferred_element_type=F32) * (hd ** -0.5)
    s = s + bias.astype(F32)
    s = jnp.where(col_ok[:, :, None, :], s, NEG_INF)
    p = jax.nn.softmax(s.reshape(s.shape[:-2] + (kh * kbw,)), axis=-1).reshape(s.shape)
    o = jnp.einsum('bhrnqik,bhrinkd->bhrnqd', p.astype(v.dtype), vg)
    return o.reshape(B, H, S, hd)


def dilated_window_attention(q, k, v, window, dil):
    B, H, S, hd = q.shape
    n = window // (2 * dil)
    L = S // dil
    qb = min(DIL_QB, L)
    nb = -(-L // qb)
    Lp = nb * qb
    kbw = qb + 2 * n

    def to_res(t):
        return t.reshape(B, H, L, dil, hd).transpose(0, 1, 3, 2, 4)

    qr = jnp.pad(to_res(q), ((0, 0), (0, 0), (0, 0), (0, Lp - L), (0, 0)))
    kv_pad = ((0, 0), (0, 0), (0, 0), (n, n + Lp - L), (0, 0))
    kr = jnp.pad(to_res(k), kv_pad)
    vr = jnp.pad(to_res(v), kv_pad)
    idx = np.arange(nb)[:, None] * qb + np.arange(kbw)[None, :]
    kb = kr[:, :, :, idx]
    vb = vr[:, :, :, idx]
    qs = qr.reshape(B, H, dil, nb, qb, hd)
    s = jnp.einsum('bhrnqd,bhrnkd->bhrnqk', qs, kb, preferred_element_type=F32) * (hd ** -0.5)
    m_q = np.arange(nb)[:, None] * qb + np.arange(qb)[None, :]
    m_k = (idx - n)[:, None, :]
    valid = (np.abs(m_k - m_q[:, :, None]) <= n) & (m_k >= 0) & (m_k < L)
    s = jnp.where(valid, s, NEG_INF)
    mx = jnp.max(s, axis=-1, keepdims=True)
    e = jnp.exp(s - mx)
    den = jnp.sum(e, axis=-1, keepdims=True)
    o = jnp.einsum('bhrnqk,bhrnkd->bhrnqd', (e / den).astype(v.dtype), vb)
    lse = (mx + jnp.log(den))[..., 0]
    o = o.reshape(B, H, dil, Lp, hd)[:, :, :, :L].transpose(0, 1, 3, 2, 4).reshape(B, H, S, hd)
    lse = lse.reshape(B, H, dil, Lp)[..., :L].transpose(0, 1, 3, 2).reshape(B, H, S)
    return o, lse


def dilated_mixture(q, k, v):
    outs, lses = [], []
    for g, (window, dil) in enumerate(DIL_GROUPS):
        sl = slice(g * DIL_HEADS_PER_GROUP, (g + 1) * DIL_HEADS_PER_GROUP)
        o, lse = dilated_window_attention(q[:, sl], k[:, sl], v[:, sl], window, dil)
        outs.append(o)
        lses.append(lse)
    w = jax.nn.softmax(jnp.stack(lses), axis=0)
    o = jnp.stack(outs)
    return jnp.sum(w[..., None].astype(o.dtype) * o, axis=0)


def diff_attention(q, k, v, lam, lam_init, norm_g):
    B, H, _, S, d = q.shape
    qb = min(DIFF_QB, S)
    nq = S // qb
    qs = q.reshape(B, H, 2, nq, qb, d).transpose(3, 0, 1, 2, 4, 5)

    def block(qblk):
        s = jnp.einsum('bhcqd,bhckd->bhcqk', qblk, k, preferred_element_type=F32) * (d ** -0.5)
        p = jax.nn.softmax(s, axis=-1)
        a = p[:, :, 0] - lam * p[:, :, 1]
        return jnp.einsum('bhqk,bhke->bhqe', a.astype(v.dtype), v)

    o = lax.map(block, qs)
    o = o.transpose(1, 2, 0, 3, 4).reshape(B, H, S, 2 * d)
    of = o.astype(F32)
    of = of * lax.rsqrt(jnp.mean(jnp.square(of), axis=-1, keepdims=True) + LN_EPS) * norm_g.astype(F32)
    return (of * (1.0 - lam_init)).astype(o.dtype)


def token_mixer(x, lam_init, w_in, b_gates, w_proj_a, w_proj_b, w_proj_c, w_out, rpb, lam_vec, norm_g, rope64, rope_c):
    B, S, D = x.shape
    h = x @ w_in
    sizes = [A_W, A_W, A_W, B_W, B_W, B_W, C_W, C_W, C_W]
    splits = [int(v) for v in np.cumsum(sizes)]
    qa, ka, va, qb, kb, vb, qc, kc, vc, gp = jnp.split(h, splits, axis=-1)

    def heads(t, nh):
        return t.reshape(B, S, nh, -1).transpose(0, 2, 1, 3)

    ya = neighborhood_attention(heads(qa, NA_HEADS), heads(ka, NA_HEADS), heads(va, NA_HEADS), rpb)
    ya = ya.transpose(0, 2, 1, 3).reshape(B, S, A_W)
    cos64, sin64 = rope64
    yb = dilated_mixture(apply_rope(heads(qb, DIL_HEADS), cos64, sin64),
                         apply_rope(heads(kb, DIL_HEADS), cos64, sin64),
                         heads(vb, DIL_HEADS))
    yb = yb.transpose(0, 2, 1, 3).reshape(B, S, B_OUT)
    cosc, sinc = rope_c

    def diff_heads(t):
        return t.reshape(B, S, DIFF_HEADS, 2, DIFF_DIM).transpose(0, 2, 3, 1, 4)

    lv = lam_vec.astype(F32)
    lam = jnp.exp(jnp.sum(lv[0] * lv[1])) - jnp.exp(jnp.sum(lv[2] * lv[3])) + lam_init
    yc = diff_attention(apply_rope(diff_heads(qc), cosc, sinc), apply_rope(diff_heads(kc), cosc, sinc),
                        heads(vc, DIFF_HEADS), lam, lam_init, norm_g)
    yc = yc.transpose(0, 2, 1, 3).reshape(B, S, C_W)
    gates = jax.nn.sigmoid((gp + b_gates).reshape(B, S, N_BRANCH, D))
    merged = gates[:, :, 0] * (ya @ w_proj_a) + gates[:, :, 1] * (yb @ w_proj_b) + gates[:, :, 2] * (yc @ w_proj_c)
    return merged @ w_out


def moe_ffn(x, w_router, b_router, w_gate, b_gate, w_up, b_up, w_down, b_down):
    B, S, D = x.shape
    xt = x.reshape(B * S, D)
    logits = (xt @ w_router).astype(F32) + b_router.astype(F32)
    top_v, top_i = lax.top_k(logits, TOP_K)
    top_w = jax.nn.softmax(top_v, axis=-1)
    gates = jnp.sum(jax.nn.one_hot(top_i, N_EXPERTS, dtype=F32) * top_w[..., None], axis=1).astype(xt.dtype)
    y = jnp.zeros_like(xt)
    for e in range(N_EXPERTS):
        hg = jnp.minimum(xt @ w_gate[e] + b_gate[e], SWIGLU_LIMIT)
        hu = jnp.clip(xt @ w_up[e] + b_up[e], -SWIGLU_LIMIT, SWIGLU_LIMIT)
        hh = hg * jax.nn.sigmoid(SWIGLU_ALPHA * hg) * (hu + 1.0)
        y = y + gates[:, e:e + 1] * (hh @ w_down[e] + b_down[e])
    return y.reshape(B, S, D)


def setup_inputs(seed: int = 0) -> dict:
    key = jax.random.key(seed)
    ks = jax.random.split(key, 24)

    def nrm(k, shape, scale):
        return jax.random.normal(k, shape, F32) * scale

    Lr = DEPTH
    return {
        'x': nrm(ks[0], (BATCH, SEQ, D_MODEL), 1.0),
        'w_in': nrm(ks[1], (Lr, D_MODEL, IN_COLS), D_MODEL ** -0.5),
        'b_gates': nrm(ks[2], (Lr, N_BRANCH * D_MODEL), 0.02),
        'w_proj_a': nrm(ks[3], (Lr, A_W, D_MODEL), A_W ** -0.5),
        'w_proj_b': nrm(ks[4], (Lr, B_OUT, D_MODEL), B_OUT ** -0.5),
        'w_proj_c': nrm(ks[5], (Lr, C_W, D_MODEL), C_W ** -0.5),
        'w_out': nrm(ks[6], (Lr, D_MODEL, D_MODEL), DEEPNORM_BETA * D_MODEL ** -0.5),
        'na_rpb': nrm(ks[7], (Lr, NA_HEADS, 2 * NA_KH - 1, 2 * NA_KW - 1), 0.02),
        'diff_lambda': nrm(ks[8], (Lr, 4, DIFF_DIM), 0.1),
        'diff_norm_g': 1.0 + nrm(ks[9], (Lr, 2 * DIFF_DIM), 0.02),
        'ln1_g': 1.0 + nrm(ks[10], (Lr, D_MODEL), 0.02),
        'ln1_b': nrm(ks[11], (Lr, D_MODEL), 0.02),
        'w_router': nrm(ks[12], (Lr, D_MODEL, N_EXPERTS), D_MODEL ** -0.5),
        'b_router': nrm(ks[13], (Lr, N_EXPERTS), 0.01),
        'w_exp_gate': nrm(ks[14], (Lr, N_EXPERTS, D_MODEL, D_EXPERT), D_MODEL ** -0.5),
        'b_exp_gate': nrm(ks[15], (Lr, N_EXPERTS, D_EXPERT), 0.02),
        'w_exp_up': nrm(ks[16], (Lr, N_EXPERTS, D_MODEL, D_EXPERT), D_MODEL ** -0.5),
        'b_exp_up': nrm(ks[17], (Lr, N_EXPERTS, D_EXPERT), 0.02),
        'w_exp_down': nrm(ks[18], (Lr, N_EXPERTS, D_EXPERT, D_MODEL), DEEPNORM_BETA * D_EXPERT ** -0.5),
        'b_exp_down': nrm(ks[19], (Lr, N_EXPERTS, D_MODEL), 0.02),
        'ln2_g': 1.0 + nrm(ks[20], (Lr, D_MODEL), 0.02),
        'ln2_b': nrm(ks[21], (Lr, D_MODEL), 0.02),
    }


def reference(x, w_in, b_gates, w_proj_a, w_proj_b, w_proj_c, w_out, na_rpb, diff_lambda, diff_norm_g,
              ln1_g, ln1_b, w_router, b_router, w_exp_gate, b_exp_gate, w_exp_up, b_exp_up,
              w_exp_down, b_exp_down, ln2_g, ln2_b):
    S = x.shape[1]
    rope64 = rope_tables(S, HEAD_DIM)
    rope_c = rope_tables(S, DIFF_DIM)
    for l in range(DEPTH):
        lam_init = 0.8 - 0.6 * math.exp(-0.3 * l)
        mix = token_mixer(x, lam_init, w_in[l], b_gates[l], w_proj_a[l], w_proj_b[l], w_proj_c[l], w_out[l],
                          na_rpb[l], diff_lambda[l], diff_norm_g[l], rope64, rope_c)
        x = layer_norm(DEEPNORM_ALPHA * x + mix, ln1_g[l], ln1_b[l])
        ffn = moe_ffn(x, w_router[l], b_router[l], w_exp_gate[l], b_exp_gate[l], w_exp_up[l], b_exp_up[l],
                      w_exp_down[l], b_exp_down[l])
        x = layer_norm(DEEPNORM_ALPHA * x + ffn, ln2_g[l], ln2_b[l])
    return x
```

```python
import math
from contextlib import ExitStack
import numpy as np
import ml_dtypes
import concourse.bass as bass
import concourse.mybir as mybir
from concourse.bass_utils import run_bass_kernel_spmd

F32 = mybir.dt.float32
BF16 = mybir.dt.bfloat16
AF = mybir.ActivationFunctionType
ALU = mybir.AluOpType
AX = mybir.AxisListType

S = 4096
D = 1024
NTB = 8
LN_EPS = 1e-5
NEG = -30000.0
SW_ALPHA = 1.702
SW_LIM = 7.0
B_JB = [(-1, 4), (-2, 5), (-8, 11)]
B_OFF = [0, 6, 14]


class Trk:
    def __init__(s, nc, es):
        s.nc = nc
        s.es = es
        s.eng = {'pe': nc.tensor, 'act': nc.scalar, 'dve': nc.vector, 'pool': nc.gpsimd, 'sp': nc.sync}
        s.esem = {k: es.enter_context(nc.semaphore('e_' + k)) for k in ('pe', 'act', 'dve', 'pool')}
        s.ecnt = {k: 0 for k in s.esem}
        s.dsem = {}
        s.dcnt = {}
        s.waited = {k: {} for k in s.eng}
        s.lastw = {}
        s.rd = {}

    def _wait(s, e, tok):
        kind, key, val = tok
        if kind == 'e':
            if key == 'pe' and e == 'pe':
                return
            if val <= 0 or s.waited[e].get(('e', key), 0) >= val:
                return
            s.eng[e].wait_ge(s.esem[key], val)
            s.waited[e][('e', key)] = val
        else:
            tgt = s.dcnt[key]
            if tgt <= 0 or s.waited[e].get(('d', key), 0) >= tgt:
                return
            s.eng[e].wait_ge(s.dsem[key], tgt)
            s.waited[e][('d', key)] = tgt

    def _deps(s, e, reads, writes):
        for r in reads:
            t = s.lastw.get(r)
            if t:
                s._wait(e, t)
        for w in writes:
            t = s.lastw.get(w)
            if t:
                s._wait(e, t)
            for t in s.rd.get(w, {}).values():
                s._wait(e, t)

    def _record(s, tok, reads, writes):
        for r in reads:
            s.rd.setdefault(r, {})[(tok[0], tok[1])] = tok
        for w in writes:
            s.lastw[w] = tok
            s.rd[w] = {}

    def op(s, e, fn, reads=(), writes=()):
        s._deps(e, reads, writes)
        ins = fn(s.eng[e])
        s.ecnt[e] += 1
        ins.then_inc(s.esem[e], 1)
        s._record(('e', e, s.ecnt[e]), reads, writes)

    def dma(s, q, out, in_, reads=(), writes=(), key=None):
        s._deps(q, reads, writes)
        if key not in s.dsem:
            s.dsem[key] = s.es.enter_context(s.nc.semaphore('d_%d' % len(s.dsem)))
            s.dcnt[key] = 0
        s.eng[q].dma_start(out=out, in_=in_).then_inc(s.dsem[key], 16)
        s.dcnt[key] += 16
        s._record(('d', key, s.dcnt[key]), reads, writes)

    def barrier(s):
        for e in s.eng:
            for k in s.esem:
                s._wait(e, ('e', k, s.ecnt[k]))
            for k in s.dsem:
                s._wait(e, ('d', k, 0))
        s.lastw = {}
        s.rd = {}


class Rot:
    def __init__(s, name, views):
        s.name = name
        s.views = views
        s.i = -1

    def next(s):
        s.i = (s.i + 1) % len(s.views)
        return s.views[s.i], (s.name, s.i)


def build_program(depth, n_exp, debug=False):
    nc = bass.Bass("TRN2", target_bir_lowering=False)
    es = ExitStack()
    T = Trk(nc, es)
    E = n_exp

    def din(name, shape, dt=F32):
        return nc.dram_tensor(name, list(shape), dt, kind="ExternalInput").ap()

    def dscr(name, shape, dt):
        kind = "ExternalOutput" if debug else "Internal"
        return nc.dram_tensor(name, list(shape), dt, kind=kind).ap()

    x_in = din("x", [S, D])
    w_in = din("w_in", [depth * D, 6144])
    bgT = din("bgT", [depth * 128, 24])
    w_pa = din("w_proj_a", [depth * 256, D])
    w_pb = din("w_proj_b", [depth * 128, D])
    w_pc = din("w_proj_c", [depth * 384, D])
    w_out = din("w_out", [depth * D, D])
    biasA = din("biasA", [depth * 4 * 128, 8 * 512])
    dlam = din("diff_lambda", [depth, 192])
    dng = din("diff_norm_g", [depth * 96, 1])
    ln1g = din("ln1_g", [depth, D])
    ln1b = din("ln1_b", [depth, D])
    ln2g = din("ln2_g", [depth, D])
    ln2b = din("ln2_b", [depth, D])
    w_r = din("w_router", [depth * D, E])
    b_r = din("b_router", [depth, E])
    w_eg = din("w_exp_gate", [depth * E * D, D])
    w_eu = din("w_exp_up", [depth * E * D, D])
    w_ed = din("w_exp_down", [depth * E * D, D])
    begT = din("begT", [depth * 128, E * 8])
    beuT = din("beuT", [depth * 128, E * 8])
    b_ed = din("b_exp_down", [depth * E, D])
    c_ident = din("c_ident", [128, 128], BF16)
    c_cosB = din("c_cosB", [128, S])
    c_sinB = din("c_sinB", [128, S])
    c_cosC = din("c_cosC", [96, S])
    c_sinC = din("c_sinC", [96, S])
    c_maskA = din("c_maskA", [128, 24 * 512], BF16)
    c_maskB = din("c_maskB", [128, 34 * 512], BF16)
    y_out = nc.dram_tensor("y", [S, D], F32, kind="ExternalOutput").ap()

    xT_d = dscr("xT_d", [D, S], BF16)
    x1T_d = dscr("x1T_d", [D, S], BF16)
    QT_d = dscr("QT_d", [D, S], BF16)
    KT_d = dscr("KT_d", [D, S], BF16)
    V_d = dscr("V_d", [S, D], BF16)
    YT_d = dscr("YT_d", [768, S], BF16)
    x1_d = dscr("x1_d", [S, D], F32)
    x2_d = dscr("x2_d", [S, D], F32)
    lam_d = dscr("lam_d", [1, 8], F32)

    sb_n = [0]

    def sb(stack, name, shape, dt):
        sb_n[0] += 1
        return stack.enter_context(nc.sbuf_tensor("%s_%d" % (name, sb_n[0]), list(shape), dt))

    ident = sb(es, "ident", [128, 128], BF16)
    onesb = sb(es, "onesb", [128, 128], BF16)
    psS = [es.enter_context(nc.psum_tensor("psS%d" % i, [128, 512], F32)) for i in range(3)]
    psA = [es.enter_context(nc.psum_tensor("psA%d" % i, [128, 512], F32)) for i in range(4)]
    psT = es.enter_context(nc.psum_tensor("psT", [128, 1024], BF16))
    Spool = Rot("psS", psS)
    Apool = Rot("psA", psA)

    T.dma('sp', ident[:], c_ident[:, :], writes=[("ident",)], key="const")
    T.op('pool', lambda e: e.memset(onesb[:], 1.0), writes=[("onesb",)])

    def ln_epilogue(st, z, zres, g_t, b_t, row0, dst_f32, dstT, pools):
        stt, mv, xo_p, xb_p, xt_p = pools
        st6, st6r = stt.next()
        T.op('dve', lambda e: e.bn_stats(st6[:, 0:6], z[:, 0:512]), reads=zres, writes=[st6r + ("a",)])
        T.op('dve', lambda e: e.bn_stats(st6[:, 6:12], z[:, 512:1024]), reads=zres, writes=[st6r + ("b",)])
        m, mr = mv.next()
        T.op('dve', lambda e: e.bn_aggr(m[:, 0:2], st6[:, 0:12]), reads=[st6r + ("a",), st6r + ("b",)], writes=[mr])
        T.op('act', lambda e: e.activation(out=m[:, 3:4], in_=m[:, 1:2], func=AF.Ln, bias=LN_EPS),
             reads=[mr], writes=[mr + ("l",)])
        T.op('act', lambda e: e.activation(out=m[:, 2:3], in_=m[:, 3:4], func=AF.Exp, scale=-0.5),
             reads=[mr + ("l",)], writes=[mr + ("r",)])
        xo, xor_ = xo_p.next()
        T.op('dve', lambda e: e.tensor_scalar(xo[:], z[:], m[:, 0:1], m[:, 2:3], ALU.subtract, ALU.mult),
             reads=zres + [mr, mr + ("r",)], writes=[xor_])
        T.op('pool', lambda e: e.tensor_tensor(xo[:], xo[:], g_t[:], ALU.mult), reads=[xor_, ("lng",)], writes=[xor_])
        T.op('pool', lambda e: e.tensor_tensor(xo[:], xo[:], b_t[:], ALU.add), reads=[xor_, ("lnb",)], writes=[xor_])
        T.dma('sp', dst_f32[row0:row0 + 128, :], xo[:], reads=[xor_], key="st_x")
        if dstT is not None:
            xb, xbr = xb_p.next()
            T.op('act', lambda e: e.activation(out=xb[:], in_=xo[:], func=AF.Copy), reads=[xor_], writes=[xbr])
            transpose_store(xb, xbr, row0, dstT, xt_p)

    def transpose_store(xb, xbr, row0, dstT, xt_p):
        for fc in range(8):
            T.op('pe', lambda e, fc=fc: e.transpose(psT[:, fc * 128:(fc + 1) * 128], xb[:, fc * 128:(fc + 1) * 128], ident[:]),
                 reads=[xbr, ("ident",)], writes=[("psT", fc)])
        xt, xtr = xt_p.next()
        T.op('act', lambda e: e.activation(out=xt[:], in_=psT[:], func=AF.Copy),
             reads=[("psT", fc) for fc in range(8)], writes=[xtr])
        T.dma('sp', dstT[:, row0:row0 + 128].rearrange("(fc p) t -> p fc t", p=128),
              xt[:].rearrange("p (fc t) -> p fc t", t=128), reads=[xtr], key="st_xT")

    with ExitStack() as st:
        xl = [sb(st, "p_xl%d" % i, [128, D], F32) for i in range(2)]
        xbs = [sb(st, "p_xb%d" % i, [128, D], BF16) for i in range(2)]
        xts = [sb(st, "p_xt%d" % i, [128, D], BF16) for i in range(2)]
        xl_p, xb_p, xt_p = Rot("p_xl", xl), Rot("p_xb", xbs), Rot("p_xt", xts)
        for tt in range(32):
            xt_, xr = xl_p.next()
            T.dma('sp', xt_[:], x_in[tt * 128:(tt + 1) * 128, :], writes=[xr], key=("ldx", xr))
            xb, xbr = xb_p.next()
            T.op('act', lambda e: e.activation(out=xb[:], in_=xt_[:], func=AF.Copy), reads=[xr], writes=[xbr])
            transpose_store(xb, xbr, tt * 128, xT_d, xt_p)
        T.barrier()

    x_cur = x_in
    for l in range(depth):
        lam_init = 0.8 - 0.6 * math.exp(-0.3 * l)
        last = (l == depth - 1)
        with ExitStack() as st:
            wq = sb(st, "a_wq", [128, 8, 3072], BF16)
            wrot = sb(st, "a_wrot", [128, 8, 1536], BF16)
            for kc in range(8):
                T.dma('pool', wq[:, kc, :], w_in[l * D + kc * 128:l * D + (kc + 1) * 128, 0:3072],
                      writes=[("wq", kc)], key=("wq", kc % 2))
            for kc in range(8):
                for (sbase, dbase, nu, w) in ((768, 0, 12, 64), (1920, 768, 16, 48)):
                    h = w // 2
                    src = wq[:, kc, sbase:sbase + nu * w].rearrange("p (u j) -> p u j", j=w)
                    dst = wrot[:, kc, dbase:dbase + nu * w].rearrange("p (u j) -> p u j", j=w)
                    T.op('pool', lambda e, src=src, dst=dst, h=h, w=w: e.tensor_scalar(
                        dst[:, :, 0:h], src[:, :, h:w], -1.0, None, ALU.mult),
                        reads=[("wq", kc)], writes=[("wrot", kc, dbase, 0)])
                    T.op('pool', lambda e, src=src, dst=dst, h=h, w=w: e.tensor_copy(dst[:, :, h:w], src[:, :, 0:h]),
                         reads=[("wq", kc)], writes=[("wrot", kc, dbase, 1)])
            wq_res = [("wq", kc) for kc in range(8)]
            wrot_res = [("wrot", kc, db, hh) for kc in range(8) for db in (0, 768) for hh in (0, 1)]
            xts = [sb(st, "a_xt%d" % i, [128, 8, 512], BF16) for i in range(2)]
            tabs = [sb(st, "a_tab%d" % i, [128, 4, 512], F32) for i in range(2)]
            t1s = [sb(st, "a_t1%d" % i, [128, 512], F32) for i in range(2)]
            t2s = [sb(st, "a_t2%d" % i, [128, 512], F32) for i in range(2)]
            obs = [sb(st, "a_ob%d" % i, [128, 512], BF16) for i in range(3)]
            vts = [sb(st, "a_vt%d" % i, [128, 1024], BF16) for i in range(2)]
            xt_p, tab_p, t1_p, t2_p, ob_p, vt_p = (Rot("a_xt", xts), Rot("a_tab", tabs), Rot("a_t1", t1s),
                                                   Rot("a_t2", t2s), Rot("a_ob", obs), Rot("a_vt", vts))
            chunks = []
            for i in range(2):
                chunks.append((i * 128, 128, QT_d, i * 128, None, 0, 0.125))
                chunks.append((256 + i * 128, 128, KT_d, i * 128, None, 0, 1.0))
            for i in range(3):
                chunks.append((768 + i * 128, 128, QT_d, 256 + i * 128, i * 128, 0, 1.0))
                chunks.append((1152 + i * 128, 128, KT_d, 256 + i * 128, 384 + i * 128, 0, 1.0))
            for i in range(4):
                chunks.append((1920 + i * 96, 96, QT_d, 640 + i * 96, 768 + i * 96, 1, 1.0))
                chunks.append((2304 + i * 96, 96, KT_d, 640 + i * 96, 1152 + i * 96, 1, 1.0))
            for tb in range(NTB):
                c0 = tb * 512
                xt, xtr = xt_p.next()
                T.dma('sp', xt[:], xT_d[:, c0:c0 + 512].rearrange("(kc p) t -> p kc t", p=128), writes=[xtr], key=("a_xt", xtr))
                tab, tabr = tab_p.next()
                T.dma('sp', tab[:, 0, :], c_cosB[:, c0:c0 + 512], writes=[tabr + (0,)], key=("a_tab", tabr))
                T.dma('sp', tab[:, 1, :], c_sinB[:, c0:c0 + 512], writes=[tabr + (1,)], key=("a_tab", tabr))
                T.dma('sp', tab[0:96, 2, :], c_cosC[:, c0:c0 + 512], writes=[tabr + (2,)], key=("a_tab", tabr))
                T.dma('sp', tab[0:96, 3, :], c_sinC[:, c0:c0 + 512], writes=[tabr + (3,)], key=("a_tab", tabr))
                for (col0, M, dst, drow, rc0, ti, scale) in chunks:
                    p1, p1r = Spool.next()
                    for kc in range(8):
                        T.op('pe', lambda e, kc=kc, p1=p1: e.matmul(p1[0:M, :], wq[:, kc, col0:col0 + M], xt[:, kc, :],
                                                                 start=(kc == 0), stop=(kc == 7)),
                             reads=[("wq", kc), xtr], writes=[p1r])
                    ob, obr = ob_p.next()
                    if rc0 is None:
                        T.op('act', lambda e, p1=p1, ob=ob: e.activation(out=ob[0:M, :], in_=p1[0:M, :], func=AF.Copy, scale=scale),
                             reads=[p1r], writes=[obr])
                    else:
                        p2, p2r = Spool.next()
                        for kc in range(8):
                            T.op('pe', lambda e, kc=kc, p2=p2: e.matmul(p2[0:M, :], wrot[:, kc, rc0:rc0 + M], xt[:, kc, :],
                                                                     start=(kc == 0), stop=(kc == 7)),
                                 reads=wrot_res[kc * 4:(kc + 1) * 4] + [xtr], writes=[p2r])
                        t1, t1r = t1_p.next()
                        t2, t2r = t2_p.next()
                        T.op('dve', lambda e, p1=p1, t1=t1: e.tensor_tensor(t1[0:M, :], p1[0:M, :], tab[0:M, 2 * ti, :], ALU.mult),
                             reads=[p1r, tabr + (2 * ti,)], writes=[t1r])
                        T.op('dve', lambda e, p2=p2, t2=t2: e.tensor_tensor(t2[0:M, :], p2[0:M, :], tab[0:M, 2 * ti + 1, :], ALU.mult),
                             reads=[p2r, tabr + (2 * ti + 1,)], writes=[t2r])
                        T.op('pool', lambda e, t1=t1, t2=t2, ob=ob: e.tensor_tensor(ob[0:M, :], t1[0:M, :], t2[0:M, :], ALU.add),
                             reads=[t1r, t2r], writes=[obr])
                    T.dma('sp', dst[drow:drow + M, c0:c0 + 512], ob[0:M, :], reads=[obr], key="st_a")
                for tt in range(4):
                    vt, vtr = vt_p.next()
                    for (vc0, n, dcol) in ((512, 256, 0), (1536, 384, 256), (2688, 384, 640)):
                        pv, pvr = Spool.next()
                        for kc in range(8):
                            T.op('pe', lambda e, kc=kc, pv=pv: e.matmul(pv[:, 0:n], xt[:, kc, tt * 128:(tt + 1) * 128],
                                                                     wq[:, kc, vc0:vc0 + n], start=(kc == 0), stop=(kc == 7)),
                                 reads=[("wq", kc), xtr], writes=[pvr])
                        T.op('act', lambda e, pv=pv, vt=vt: e.activation(out=vt[:, dcol:dcol + n], in_=pv[:, 0:n], func=AF.Copy),
                             reads=[pvr], writes=[vtr + (dcol,)])
                    T.dma('sp', V_d[c0 + tt * 128:c0 + (tt + 1) * 128, :], vt[:],
                          reads=[vtr + (0,), vtr + (256,), vtr + (640,)], key="st_v")
            T.barrier()

        with ExitStack() as st:
            kts = [sb(st, "b_kt%d" % i, [64, S], BF16) for i in range(6)]
            vhs = [sb(st, "b_vh%d" % i, [128, 32, 96], BF16) for i in range(6)]
            maskt = sb(st, "b_mask", [128, 34, 512], BF16)
            biast = sb(st, "b_bias", [128, 8, 512], BF16)
            qts = [sb(st, "b_q%d" % i, [64, 512], BF16) for i in range(4)]
            ets = [sb(st, "b_e%d" % i, [128, 512], BF16) for i in range(4)]
            rts = [sb(st, "b_r%d" % i, [96, 512], F32) for i in range(2)]
            fts = [sb(st, "b_f%d" % i, [96, 512], F32) for i in range(4)]
            sqs = [sb(st, "b_sq%d" % i, [96, 512], BF16) for i in range(2)]
            yos = [sb(st, "b_yo%d" % i, [96, 512], BF16) for i in range(2)]
            lamt = sb(st, "b_lam", [1, 200], F32)
            nlam = sb(st, "b_nlam", [96, 1], F32)
            gpr = sb(st, "b_gpr", [96, 1], F32)
            q_p, e_p, r_p, f_p, sq_p, yo_p = (Rot("b_q", qts), Rot("b_e", ets), Rot("b_r", rts), Rot("b_f", fts),
                                              Rot("b_sq", sqs), Rot("b_yo", yos))

            def load_k(slot, row0, d):
                T.dma('sp', kts[slot][0:d, :], KT_d[row0:row0 + d, :], writes=[("kt", slot)], key=("kt", slot))

            def load_v(slot, col0, e_):
                T.dma('sp', vhs[slot][:, :, 0:e_], V_d[:, col0:col0 + e_].rearrange("(kb p) e -> p kb e", p=128),
                      writes=[("vh", slot)], key=("vh", slot))

            def load_q(row0, d, qb):
                q, qr = q_p.next()
                T.dma('sp', q[0:d, :], QT_d[row0:row0 + d, qb * 512:(qb + 1) * 512], writes=[qr], key=("q", qr))
                return q, qr

            def attn_tiles(items, e_, accs, scale):
                num, numr, den, denr = accs
                n = len(items)
                for i, (ks, d, q, qr, kb, biases, vs) in enumerate(items):
                    sp_, spr = Spool.next()
                    T.op('pe', lambda e, sp_=sp_, ks=ks, d=d, q=q, kb=kb: e.matmul(
                        sp_[:, :], kts[ks][0:d, kb * 128:(kb + 1) * 128], q[0:d, :], start=True, stop=(len(biases) == 0)),
                        reads=[("kt", ks), qr], writes=[spr])
                    for bi, (bap, bres) in enumerate(biases):
                        T.op('pe', lambda e, sp_=sp_, bap=bap, bi=bi: e.matmul(sp_[:, :], ident[:], bap, start=False,
                                                                             stop=(bi == len(biases) - 1)),
                             reads=[("ident",), bres], writes=[spr])
                    et, etr = e_p.next()
                    T.op('act', lambda e, sp_=sp_, et=et: e.activation(out=et[:], in_=sp_[:, :], func=AF.Exp, scale=scale),
                         reads=[spr], writes=[etr])
                    T.op('pe', lambda e, et=et, vs=vs, kb=kb, i=i: e.matmul(num[0:e_, :], vhs[vs][:, kb, 0:e_], et[:],
                                                                          start=(i == 0), stop=(i == n - 1)),
                         reads=[("vh", vs), etr], writes=[numr])
                    T.op('pe', lambda e, et=et, i=i: e.matmul(den[0:e_, :], onesb[:, 0:e_], et[:],
                                                            start=(i == 0), stop=(i == n - 1)),
                         reads=[("onesb",), etr], writes=[denr])

            def finalize_simple(accs, e_, yrow, qb):
                num, numr, den, denr = accs
                r, rr = r_p.next()
                T.op('dve', lambda e: e.reciprocal(r[0:e_, :], den[0:e_, :]), reads=[denr], writes=[rr])
                yo, yor = yo_p.next()
                T.op('dve', lambda e: e.tensor_tensor(yo[0:e_, :], num[0:e_, :], r[0:e_, :], ALU.mult),
                     reads=[numr, rr], writes=[yor])
                T.dma('sp', YT_d[yrow:yrow + e_, qb * 512:(qb + 1) * 512], yo[0:e_, :], reads=[yor], key="st_y")

            T.dma('sp', maskt[:, 0:24, :], c_maskA[:, :].rearrange("p (j q) -> p j q", q=512), writes=[("mask",)], key="mask")
            for h in range(4):
                load_k(0, h * 64, 64)
                load_v(0, h * 64, 64)
                r0 = (l * 4 + h) * 128
                T.dma('pool', biast[:], biasA[r0:r0 + 128, :].rearrange("p (j q) -> p j q", q=512), writes=[("bias",)], key="bias")
                for qb in range(8):
                    q, qr = load_q(h * 64, 64, qb)
                    var = 0 if qb == 0 else (2 if qb == 7 else 1)
                    items = []
                    for j in range(8):
                        kb = 4 * qb - 2 + j
                        if 0 <= kb < 32:
                            items.append((0, 64, q, qr, kb, [(biast[:, j, :], ("bias",)), (maskt[:, var * 8 + j, :], ("mask",))], 0))
                    num, numr = Apool.next()
                    den, denr = Apool.next()
                    attn_tiles(items, 64, (num, numr, den, denr), 1.0)
                    finalize_simple((num, numr, den, denr), 64, h * 64, qb)
            T.dma('sp', maskt[:, :, :], c_maskB[:, :].rearrange("p (j q) -> p j q", q=512), writes=[("mask",)], key="mask")
            for u in range(6):
                load_k(u, 256 + u * 64, 64)
                load_v(u, 256 + u * 64, 64)
            for jo in range(2):
                for qb in range(8):
                    items = []
                    for g in range(3):
                        u = 2 * g + jo
                        q, qr = load_q(256 + u * 64, 64, qb)
                        for jb in range(B_JB[g][0], B_JB[g][1] + 1):
                            kb = 4 * qb + jb
                            if 0 <= kb < 32:
                                mi = B_OFF[g] + jb - B_JB[g][0]
                                items.append((u, 64, q, qr, kb, [(maskt[:, mi, :], ("mask",))], u))
                    num, numr = Apool.next()
                    den, denr = Apool.next()
                    attn_tiles(items, 64, (num, numr, den, denr), 0.125)
                    finalize_simple((num, numr, den, denr), 64, 256 + jo * 64, qb)
            T.dma('sp', lamt[:, 0:192], dlam[l:l + 1, :], writes=[("lamt",)], key="lam")
            T.op('dve', lambda e: e.tensor_tensor(lamt[:, 0:48], lamt[:, 0:48], lamt[:, 48:96], ALU.mult),
                 reads=[("lamt",)], writes=[("lamt", 0)])
            T.op('dve', lambda e: e.tensor_tensor(lamt[:, 48:96], lamt[:, 96:144], lamt[:, 144:192], ALU.mult),
                 reads=[("lamt",)], writes=[("lamt", 1)])
            T.op('dve', lambda e: e.reduce_sum(lamt[:, 192:194], lamt[:, 0:96].rearrange("p (a b) -> p a b", b=48), AX.X),
                 reads=[("lamt", 0), ("lamt", 1)], writes=[("lamt", 2)])
            T.op('act', lambda e: e.activation(out=lamt[:, 194:196], in_=lamt[:, 192:194], func=AF.Exp),
                 reads=[("lamt", 2)], writes=[("lamt", 3)])
            T.op('dve', lambda e: e.tensor_tensor(lamt[:, 196:197], lamt[:, 194:195], lamt[:, 195:196], ALU.subtract),
                 reads=[("lamt", 3)], writes=[("lamt", 4)])
            T.op('dve', lambda e: e.tensor_scalar(lamt[:, 197:198], lamt[:, 196:197], -1.0, -lam_init, ALU.mult, ALU.add),
                 reads=[("lamt", 4)], writes=[("lamt", 5)])
            T.dma('sp', lam_d[0:1, 0:1], lamt[:, 197:198], reads=[("lamt", 5)], writes=[("lam_d",)], key="lam2")
            T.dma('sp', nlam[:], lam_d[0, 0:1].partition_broadcast(96), reads=[("lam_d",)], writes=[("nlam",)], key="lam3")
            T.dma('sp', gpr[:], dng[l * 96:(l + 1) * 96, :], writes=[("gpr",)], key="lam4")
            T.op('dve', lambda e: e.tensor_scalar(gpr[:], gpr[:], 1.0 - lam_init, None, ALU.mult),
                 reads=[("gpr",)], writes=[("gpr",)])
            for h in range(4):
                for c in range(2):
                    load_k(c, 640 + h * 96 + c * 48, 48)
                load_v(0, 640 + h * 96, 96)
                for qb in range(8):
                    accs = []
                    for c in range(2):
                        q, qr = load_q(640 + h * 96 + c * 48, 48, qb)
                        num, numr = Apool.next()
                        den, denr = Apool.next()
                        items = [(c, 48, q, qr, kb, [], 0) for kb in range(32)]
                        attn_tiles(items, 96, (num, numr, den, denr), 48 ** -0.5)
                        accs.append((num, numr, den, denr))
                    fs = []
                    for c in range(2):
                        num, numr, den, denr = accs[c]
                        r, rr = r_p.next()
                        T.op('dve', lambda e, r=r, den=den: e.reciprocal(r[:, :], den[0:96, :]), reads=[denr], writes=[rr])
                        f, fr = f_p.next()
                        T.op('dve', lambda e, f=f, num=num, r=r: e.tensor_tensor(f[:, :], num[0:96, :], r[:, :], ALU.mult),
                             reads=[numr, rr], writes=[fr])
                        fs.append((f, fr))
                    o, orr = f_p.next()
                    T.op('dve', lambda e: e.scalar_tensor_tensor(o[:, :], fs[1][0][:, :], nlam[:, 0:1], fs[0][0][:, :], ALU.mult, ALU.add),
                         reads=[fs[0][1], fs[1][1], ("nlam",)], writes=[orr])
                    sq, sqr = sq_p.next()
                    T.op('pool', lambda e: e.tensor_tensor(sq[:, :], o[:, :], o[:, :], ALU.mult), reads=[orr], writes=[sqr])
                    ms, msr = Spool.next()
                    T.op('pe', lambda e: e.matmul(ms[0:96, :], onesb[0:96, 0:96], sq[:, :], start=True, stop=True),
                         reads=[("onesb",), sqr], writes=[msr])
                    lnv, lnr = f_p.next()
                    T.op('act', lambda e: e.activation(out=lnv[:, :], in_=ms[0:96, :], func=AF.Ln, scale=1.0 / 96.0, bias=LN_EPS),
                         reads=[msr], writes=[lnr])
                    T.op('act', lambda e: e.activation(out=lnv[:, :], in_=lnv[:, :], func=AF.Exp, scale=-0.5),
                         reads=[lnr], writes=[lnr])
                    yo, yor = yo_p.next()
                    T.op('dve', lambda e: e.scalar_tensor_tensor(yo[:, :], o[:, :], gpr[:, 0:1], lnv[:, :], ALU.mult, ALU.mult),
                         reads=[orr, lnr, ("gpr",)], writes=[yor])
                    T.dma('sp', YT_d[384 + h * 96:384 + (h + 1) * 96, qb * 512:(qb + 1) * 512], yo[:, :], reads=[yor], key="st_y")
            T.barrier()

        with ExitStack() as st:
            wg = sb(st, "c_wg", [128, 8, 3072], BF16)
            wp = sb(st, "c_wp", [128, 6, D], BF16)
            wo = sb(st, "c_wo", [128, 8, D], BF16)
            bgt = sb(st, "c_bg", [128, 24], F32)
            g_t = sb(st, "c_lng", [128, D], F32)
            b_t = sb(st, "c_lnb", [128, D], F32)
            for kc in range(8):
                T.dma('pool', wg[:, kc, :], w_in[l * D + kc * 128:l * D + (kc + 1) * 128, 3072:6144],
                      writes=[("wg", kc)], key=("wg", kc % 2))
                T.dma('pool', wo[:, kc, :], w_out[l * D + kc * 128:l * D + (kc + 1) * 128, :], writes=[("wo", kc)], key=("wo", kc % 2))
            for c in range(2):
                T.dma('pool', wp[:, c, :], w_pa[l * 256 + c * 128:l * 256 + (c + 1) * 128, :], writes=[("wp", c)], key="wp")
            T.dma('pool', wp[:, 2, :], w_pb[l * 128:(l + 1) * 128, :], writes=[("wp", 2)], key="wp")
            for c in range(3):
                T.dma('pool', wp[:, 3 + c, :], w_pc[l * 384 + c * 128:l * 384 + (c + 1) * 128, :], writes=[("wp", 3 + c)], key="wp")
            T.dma('sp', bgt[:], bgT[l * 128:(l + 1) * 128, :], writes=[("bgt",)], key="cmisc")
            T.dma('sp', g_t[:], ln1g[l, :].partition_broadcast(128), writes=[("lng",)], key="cmisc")
            T.dma('sp', b_t[:], ln1b[l, :].partition_broadcast(128), writes=[("lnb",)], key="cmisc")
            xts = [sb(st, "c_xt%d" % i, [128, 8, 512], BF16) for i in range(2)]
            yts = [sb(st, "c_yt%d" % i, [128, 6, 512], BF16) for i in range(2)]
            gts = [sb(st, "c_g%d" % i, [128, 512], F32) for i in range(3)]
            mts = [sb(st, "c_m%d" % i, [128, 512], F32) for i in range(4)]
            mgs = [sb(st, "c_mg%d" % i, [128, 8, 512], BF16) for i in range(2)]
            xrs = [sb(st, "c_xr%d" % i, [128, D], F32) for i in range(2)]
            zs = [sb(st, "c_z%d" % i, [128, D], F32) for i in range(2)]
            lnpools = (Rot("c_st", [sb(st, "c_st%d" % i, [128, 12], F32) for i in range(2)]),
                       Rot("c_mv", [sb(st, "c_mv%d" % i, [128, 4], F32) for i in range(2)]),
                       Rot("c_xo", [sb(st, "c_xo%d" % i, [128, D], F32) for i in range(2)]),
                       Rot("c_xb", [sb(st, "c_xb%d" % i, [128, D], BF16) for i in range(2)]),
                       Rot("c_xT", [sb(st, "c_xT%d" % i, [128, D], BF16) for i in range(2)]))
            xt_p, yt_p, g_p, m_p, mg_p, xr_p, z_p = (Rot("c_xt", xts), Rot("c_yt", yts), Rot("c_g", gts), Rot("c_m", mts),
                                                     Rot("c_mg", mgs), Rot("c_xr", xrs), Rot("c_z", zs))
            br_chunks = [(0, 2), (2, 3), (3, 6)]
            alpha = (2 * depth) ** 0.25
            for tb in range(NTB):
                c0 = tb * 512
                xt, xtr = xt_p.next()
                T.dma('sp', xt[:], xT_d[:, c0:c0 + 512].rearrange("(kc p) t -> p kc t", p=128), writes=[xtr], key=("c_xt", xtr))
                yt, ytr = yt_p.next()
                T.dma('sp', yt[:], YT_d[:, c0:c0 + 512].rearrange("(c p) t -> p c t", p=128), writes=[ytr], key=("c_yt", ytr))
                mg, mgr = mg_p.next()
                for fc in range(8):
                    ms_ = []
                    for i in range(3):
                        pg, pgr = Spool.next()
                        for kc in range(8):
                            T.op('pe', lambda e, kc=kc, pg=pg, i=i: e.matmul(
                                pg[:, :], wg[:, kc, i * D + fc * 128:i * D + (fc + 1) * 128], xt[:, kc, :],
                                start=(kc == 0), stop=(kc == 7)), reads=[("wg", kc), xtr], writes=[pgr])
                        g, gr = g_p.next()
                        T.op('act', lambda e, pg=pg, g=g, i=i: e.activation(out=g[:], in_=pg[:, :], func=AF.Sigmoid,
                                                                         bias=bgt[:, i * 8 + fc:i * 8 + fc + 1]),
                             reads=[pgr, ("bgt",)], writes=[gr])
                        pp, ppr = Spool.next()
                        cs = list(range(*br_chunks[i]))
                        for ci, c in enumerate(cs):
                            T.op('pe', lambda e, c=c, ci=ci, pp=pp, cs=cs: e.matmul(
                                pp[:, :], wp[:, c, fc * 128:(fc + 1) * 128], yt[:, c, :],
                                start=(ci == 0), stop=(ci == len(cs) - 1)), reads=[("wp", c), ytr], writes=[ppr])
                        m, mr = m_p.next()
                        T.op('dve', lambda e, m=m, g=g, pp=pp: e.tensor_tensor(m[:], pp[:, :], g[:], ALU.mult),
                             reads=[ppr, gr], writes=[mr])
                        ms_.append((m, mr))
                    T.op('pool', lambda e, ms_=ms_: e.tensor_tensor(ms_[0][0][:], ms_[0][0][:], ms_[1][0][:], ALU.add),
                         reads=[ms_[0][1], ms_[1][1]], writes=[ms_[0][1]])
                    T.op('pool', lambda e, ms_=ms_, fc=fc: e.tensor_tensor(mg[:, fc, :], ms_[0][0][:], ms_[2][0][:], ALU.add),
                         reads=[ms_[0][1], ms_[2][1]], writes=[mgr + (fc,)])
                for tt in range(4):
                    row0 = c0 + tt * 128
                    xr, xrr = xr_p.next()
                    T.dma('sp', xr[:], x_cur[row0:row0 + 128, :], writes=[xrr], key=("c_xr", xrr))
                    z, zr = z_p.next()
                    for half in range(2):
                        py, pyr = Apool.next()
                        for fc in range(8):
                            T.op('pe', lambda e, fc=fc, py=py, half=half: e.matmul(
                                py[:, :], mg[:, fc, tt * 128:(tt + 1) * 128], wo[:, fc, half * 512:(half + 1) * 512],
                                start=(fc == 0), stop=(fc == 7)), reads=[mgr + (fc,), ("wo", fc)], writes=[pyr])
                        T.op('dve', lambda e, py=py, half=half: e.scalar_tensor_tensor(
                            z[:, half * 512:(half + 1) * 512], xr[:, half * 512:(half + 1) * 512], alpha, py[:, :], ALU.mult, ALU.add),
                            reads=[pyr, xrr], writes=[zr + (half,)])
                    ln_epilogue(st, z, [zr + (0,), zr + (1,)], g_t, b_t, row0, x1_d, x1T_d, lnpools)
            T.barrier()

        with ExitStack() as st:
            wr = sb(st, "e_wr", [128, 8, E], BF16)
            brt = sb(st, "e_br", [128, E], F32)
            bdn = sb(st, "e_bdn", [E, D], BF16)
            begt = sb(st, "e_beg", [128, E * 8], F32)
            beut = sb(st, "e_beu", [128, E * 8], F32)
            g_t = sb(st, "e_lng", [128, D], F32)
            b_t = sb(st, "e_lnb", [128, D], F32)
            T.dma('pool', wr[:], w_r[l * D:(l + 1) * D, :].rearrange("(kc p) e -> p kc e", p=128), writes=[("wr",)], key="emisc_p")
            T.dma('pool', bdn[:], b_ed[l * E:(l + 1) * E, :], writes=[("bdn",)], key="emisc_p")
            T.dma('sp', brt[:], b_r[l, :].partition_broadcast(128), writes=[("brt",)], key="emisc")
            T.dma('sp', begt[:], begT[l * 128:(l + 1) * 128, :], writes=[("begt",)], key="emisc")
            T.dma('sp', beut[:], beuT[l * 128:(l + 1) * 128, :], writes=[("beut",)], key="emisc")
            T.dma('sp', g_t[:], ln2g[l, :].partition_broadcast(128), writes=[("lng",)], key="emisc")
            T.dma('sp', b_t[:], ln2b[l, :].partition_broadcast(128), writes=[("lnb",)], key="emisc")
            xq = sb(st, "e_xq", [128, 8, 1024], BF16)
            yacc = sb(st, "e_yacc", [128, 8, D], F32)
            wsl = [sb(st, "e_w%d" % i, [128, 8, D], BF16) for i in range(4)]
            hh = sb(st, "e_hh", [128, 8, 1024], BF16)
            gates = sb(st, "e_gates", [128, 8, E], F32)
            gbf = sb(st, "e_gbf", [128, 8, E], BF16)
            gT = sb(st, "e_gT", [E, 1024], BF16)
            lg = sb(st, "e_lg", [128, 8, E], F32)
            mx8 = sb(st, "e_mx8", [128, 8, 8], F32)
            msk = sb(st, "e_msk", [128, 8, E], F32)
            sm = sb(st, "e_sm", [128, 8, 4], F32)
            hgs = [sb(st, "e_hg%d" % i, [128, 512], F32) for i in range(2)]
            sgs = [sb(st, "e_sg%d" % i, [128, 512], F32) for i in range(2)]
            hus = [sb(st, "e_hu%d" % i, [128, 512], F32) for i in range(2)]
            xrs = [sb(st, "e_xr%d" % i, [128, D], F32) for i in range(2)]
            zs = [sb(st, "e_z%d" % i, [128, D], F32) for i in range(2)]
            lnpools = (Rot("e_st", [sb(st, "e_st%d" % i, [128, 12], F32) for i in range(2)]),
                       Rot("e_mv", [sb(st, "e_mv%d" % i, [128, 4], F32) for i in range(2)]),
                       Rot("e_xo", [sb(st, "e_xo%d" % i, [128, D], F32) for i in range(2)]),
                       Rot("e_xb", [sb(st, "e_xb%d" % i, [128, D], BF16) for i in range(2)]),
                       Rot("e_xT", [sb(st, "e_xT%d" % i, [128, D], BF16) for i in range(2)]))
            w_p, hg_p, sg_p, hu_p, xr_p, z_p = (Rot("e_w", wsl), Rot("e_hg", hgs), Rot("e_sg", sgs), Rot("e_hu", hus),
                                                Rot("e_xr", xrs), Rot("e_z", zs))
            alpha = (2 * depth) ** 0.25
            dst_f32 = y_out if last else x2_d
            dstT = None if last else xT_d

            def load_w(src, e_):
                w, wres = w_p.next()
                base = (l * E + e_) * D
                for half in range(2):
                    T.dma('pool', w[:, half * 4:(half + 1) * 4, :],
                          src[base + half * 512:base + (half + 1) * 512, :].rearrange("(kc p) f -> p kc f", p=128),
                          writes=[wres + (half,)], key=("ew", wres))
                return w, wres

            for qi in range(4):
                t0 = qi * 1024
                T.dma('sp', xq[:], x1T_d[:, t0:t0 + 1024].rearrange("(kc p) t -> p kc t", p=128), writes=[("xq",)], key="xq")
                for tt in range(8):
                    pl, plr = Spool.next()
                    for kc in range(8):
                        T.op('pe', lambda e, kc=kc, pl=pl: e.matmul(pl[:, 0:E], xq[:, kc, tt * 128:(tt + 1) * 128], wr[:, kc, :],
                                                                 start=(kc == 0), stop=(kc == 7)),
                             reads=[("xq",), ("wr",)], writes=[plr])
                    R = lambda n: ("rt", n, tt)
                    T.op('dve', lambda e, pl=pl: e.tensor_tensor(lg[:, tt, :], pl[:, 0:E], brt[:], ALU.add),
                         reads=[plr, ("brt",)], writes=[R("lg")])
                    T.op('dve', lambda e: e.max(mx8[:, tt, :], lg[:, tt, :]), reads=[R("lg")], writes=[R("mx")])
                    T.op('dve', lambda e: e.tensor_scalar(msk[:, tt, :], lg[:, tt, :], mx8[:, tt, 3:4], None, ALU.is_ge),
                         reads=[R("lg"), R("mx")], writes=[R("msk")])
                    T.op('dve', lambda e: e.tensor_scalar(sm[:, tt, 0:1], mx8[:, tt, 0:1], -1.0, None, ALU.mult),
                         reads=[R("mx")], writes=[R("nm")])
                    T.op('act', lambda e: e.activation(out=lg[:, tt, :], in_=lg[:, tt, :], func=AF.Exp, bias=sm[:, tt, 0:1]),
                         reads=[R("lg"), R("nm"), R("msk")], writes=[R("lg")])
                    T.op('dve', lambda e: e.tensor_tensor(msk[:, tt, :], msk[:, tt, :], lg[:, tt, :], ALU.mult),
                         reads=[R("lg"), R("msk")], writes=[R("msk")])
                    T.op('dve', lambda e: e.reduce_sum(sm[:, tt, 1:2], msk[:, tt, :], AX.X), reads=[R("msk")], writes=[R("ss")])
                    T.op('dve', lambda e: e.reciprocal(sm[:, tt, 2:3], sm[:, tt, 1:2]), reads=[R("ss")], writes=[R("rs")])
                    T.op('dve', lambda e: e.tensor_scalar(gates[:, tt, :], msk[:, tt, :], sm[:, tt, 2:3], None, ALU.mult),
                         reads=[R("msk"), R("rs")], writes=[("gates", tt)])
                    T.op('act', lambda e: e.activation(out=gbf[:, tt, :], in_=gates[:, tt, :], func=AF.Copy),
                         reads=[("gates", tt)], writes=[("gbf", tt)])
                    T.op('pe', lambda e: e.transpose(psT[0:E, tt * 128:(tt + 1) * 128], gbf[:, tt, :], ident[:]),
                         reads=[("gbf", tt), ("ident",)], writes=[("psT", tt)])
                T.op('act', lambda e: e.activation(out=gT[:, :], in_=psT[0:E, :], func=AF.Copy),
                     reads=[("psT", tt) for tt in range(8)], writes=[("gT",)])
                for tt in range(8):
                    for half in range(2):
                        py, pyr = Apool.next()
                        T.op('pe', lambda e, py=py, half=half: e.matmul(py[:, :], gT[:, tt * 128:(tt + 1) * 128],
                                                                      bdn[:, half * 512:(half + 1) * 512], start=True, stop=True),
                             reads=[("gT",), ("bdn",)], writes=[pyr])
                        T.op('act', lambda e, py=py, half=half: e.activation(out=yacc[:, tt, half * 512:(half + 1) * 512],
                                                                           in_=py[:, :], func=AF.Copy),
                             reads=[pyr], writes=[("yacc", tt, half)])
                nxt = None
                for ex in range(E):
                    if nxt is None:
                        wgt = load_w(w_eg, ex)
                        wut = load_w(w_eu, ex)
                    else:
                        wgt, wut = nxt
                    wdt = load_w(w_ed, ex)
                    for tb2 in range(2):
                        for fc in range(8):
                            pg, pgr = Spool.next()
                            pu, pur = Spool.next()
                            for (pt, ptr, wt) in ((pg, pgr, wgt), (pu, pur, wut)):
                                for kc in range(8):
                                    T.op('pe', lambda e, kc=kc, pt=pt, wt=wt: e.matmul(
                                        pt[:, :], wt[0][:, kc, fc * 128:(fc + 1) * 128], xq[:, kc, tb2 * 512:(tb2 + 1) * 512],
                                        start=(kc == 0), stop=(kc == 7)), reads=[wt[1] + (kc // 4,), ("xq",)], writes=[ptr])
                            hg, hgr = hg_p.next()
                            sg, sgr = sg_p.next()
                            hu, hur = hu_p.next()
                            bi = ex * 8 + fc
                            T.op('dve', lambda e, pg=pg, hg=hg, bi=bi: e.tensor_scalar(hg[:], pg[:, :], begt[:, bi:bi + 1], SW_LIM, ALU.add, ALU.min),
                                 reads=[pgr, ("begt",)], writes=[hgr])
                            T.op('act', lambda e, hg=hg, sg=sg: e.activation(out=sg[:], in_=hg[:], func=AF.Sigmoid, scale=SW_ALPHA),
                                 reads=[hgr], writes=[sgr])
                            T.op('dve', lambda e, pu=pu, hu=hu, bi=bi: e.tensor_scalar(hu[:], pu[:, :], beut[:, bi:bi + 1], SW_LIM, ALU.add, ALU.min),
                                 reads=[pur, ("beut",)], writes=[hur])
                            T.op('pool', lambda e, hu=hu: e.tensor_scalar(hu[:], hu[:], -SW_LIM, 1.0, ALU.max, ALU.add),
                                 reads=[hur], writes=[hur])
                            T.op('pool', lambda e, hg=hg, sg=sg: e.tensor_tensor(hg[:], hg[:], sg[:], ALU.mult),
                                 reads=[hgr, sgr], writes=[hgr])
                            T.op('pool', lambda e, hg=hg, hu=hu, fc=fc: e.tensor_tensor(hh[:, fc, tb2 * 512:(tb2 + 1) * 512], hg[:], hu[:], ALU.mult),
                                 reads=[hgr, hur], writes=[("hh", fc, tb2)])
                    if ex + 1 < E:
                        nxt = (load_w(w_eg, ex + 1), load_w(w_eu, ex + 1))
                    else:
                        nxt = None
                    for tt in range(8):
                        for half in range(2):
                            py, pyr = Apool.next()
                            for fc in range(8):
                                T.op('pe', lambda e, fc=fc, py=py, half=half: e.matmul(
                                    py[:, :], hh[:, fc, tt * 128:(tt + 1) * 128], wdt[0][:, fc, half * 512:(half + 1) * 512],
                                    start=(fc == 0), stop=(fc == 7)),
                                    reads=[("hh", fc, tt // 4), wdt[1] + (fc // 4,)], writes=[pyr])
                            T.op('dve', lambda e, py=py, half=half: e.scalar_tensor_tensor(
                                yacc[:, tt, half * 512:(half + 1) * 512], py[:, :], gates[:, tt, ex:ex + 1],
                                yacc[:, tt, half * 512:(half + 1) * 512], ALU.mult, ALU.add),
                                reads=[pyr, ("gates", tt), ("yacc", tt, half)], writes=[("yacc", tt, half)])
                for tt in range(8):
                    row0 = t0 + tt * 128
                    xr, xrr = xr_p.next()
                    T.dma('sp', xr[:], x1_d[row0:row0 + 128, :], writes=[xrr], key=("e_xr", xrr))
                    z, zr = z_p.next()
                    T.op('dve', lambda e: e.scalar_tensor_tensor(z[:], xr[:], alpha, yacc[:, tt, :], ALU.mult, ALU.add),
                         reads=[xrr, ("yacc", tt, 0), ("yacc", tt, 1)], writes=[zr])
                    ln_epilogue(st, z, [zr], g_t, b_t, row0, dst_f32, dstT, lnpools)
            T.barrier()
        x_cur = x2_d

    T.barrier()
    es.close()
    return nc


def _consts():
    bf = ml_dtypes.bfloat16
    c = {}
    c["c_ident"] = np.eye(128, dtype=np.float32).astype(bf)
    t = np.arange(S, dtype=np.float32)

    def tab(dim, rows):
        inv = (1.0 / (np.float32(10000.0) ** (np.arange(0, dim, 2, dtype=np.float32) / np.float32(dim)))).astype(np.float32)
        ang = (t[:, None] * inv[None, :]).astype(np.float32)
        cs, sn = np.cos(ang).astype(np.float32), np.sin(ang).astype(np.float32)
        idx = (np.arange(rows) % dim) % (dim // 2)
        return np.ascontiguousarray(cs[:, idx].T), np.ascontiguousarray(sn[:, idx].T)

    c["c_cosB"], c["c_sinB"] = tab(64, 128)
    c["c_cosC"], c["c_sinC"] = tab(48, 96)
    k = np.arange(128)
    q = np.arange(512)
    mA = np.zeros((3, 8, 128, 512), np.float32)
    for var, qb in ((0, 0), (1, 3), (2, 7)):
        r0 = 8 * qb
        r = r0 + q // 64
        cc = q % 64
        rs = np.clip(r - 4, 0, 56)
        cs_ = np.clip(cc - 8, 0, 48)
        for j in range(8):
            kr = r0 - 4 + 2 * j + k // 64
            kc = k % 64
            ok = ((kr[:, None] >= rs[None, :]) & (kr[:, None] < rs[None, :] + 8) &
                  (kc[:, None] >= cs_[None, :]) & (kc[:, None] < cs_[None, :] + 16))
            mA[var, j] = np.where(ok, 0.0, NEG)
    c["c_maskA"] = np.ascontiguousarray(mA.reshape(24, 128, 512).transpose(1, 0, 2)).reshape(128, 24 * 512).astype(bf)
    mB = np.zeros((34, 128, 512), np.float32)
    for g, d in enumerate((1, 4, 16)):
        for jb in range(B_JB[g][0], B_JB[g][1] + 1):
            kk = jb * 128 + k
            diff = kk[:, None] - q[None, :]
            ok = (np.abs(diff) <= 64 * d) & (diff % d == 0)
            mB[B_OFF[g] + jb - B_JB[g][0]] = np.where(ok, 0.0, NEG)
    c["c_maskB"] = np.ascontiguousarray(mB.transpose(1, 0, 2)).reshape(128, 34 * 512).astype(bf)
    return c


def _bias_gather(na_rpb):
    L, H = na_rpb.shape[0], na_rpb.shape[1]
    k = np.arange(128)
    q = np.arange(512)
    j = np.arange(8)
    dr = (-4 + 2 * j[None, :, None] + k[:, None, None] // 64) - (q[None, None, :] // 64)
    dc = (k[:, None, None] % 64) - (q[None, None, :] % 64) + 0 * j[None, :, None]
    ri = np.clip(dr + 7, 0, 14)
    ci = np.clip(dc + 15, 0, 30)
    out = na_rpb[:, :, ri, ci]
    return np.ascontiguousarray(out).reshape(L * H * 128, 8 * 512)


def prepare_inputs(inp, depth, n_exp):
    f = lambda a: np.ascontiguousarray(np.asarray(a, dtype=np.float32))
    E = n_exp
    m = {}
    m["w_in"] = f(inp["w_in"]).reshape(depth * D, 6144)
    m["bgT"] = np.ascontiguousarray(f(inp["b_gates"]).reshape(depth, 24, 128).transpose(0, 2, 1)).reshape(depth * 128, 24)
    m["w_proj_a"] = f(inp["w_proj_a"]).reshape(depth * 256, D)
    m["w_proj_b"] = f(inp["w_proj_b"]).reshape(depth * 128, D)
    m["w_proj_c"] = f(inp["w_proj_c"]).reshape(depth * 384, D)
    m["w_out"] = f(inp["w_out"]).reshape(depth * D, D)
    m["biasA"] = _bias_gather(f(inp["na_rpb"]))
    m["diff_lambda"] = f(inp["diff_lambda"]).reshape(depth, 192)
    m["diff_norm_g"] = f(inp["diff_norm_g"]).reshape(depth * 96, 1)
    for k_ in ("ln1_g", "ln1_b", "ln2_g", "ln2_b"):
        m[k_] = f(inp[k_]).reshape(depth, D)
    m["w_router"] = f(inp["w_router"]).reshape(depth * D, E)
    m["b_router"] = f(inp["b_router"]).reshape(depth, E)
    m["w_exp_gate"] = f(inp["w_exp_gate"]).reshape(depth * E * D, D)
    m["w_exp_up"] = f(inp["w_exp_up"]).reshape(depth * E * D, D)
    m["w_exp_down"] = f(inp["w_exp_down"]).reshape(depth * E * D, D)
    m["begT"] = np.ascontiguousarray(f(inp["b_exp_gate"]).reshape(depth, E * 8, 128).transpose(0, 2, 1)).reshape(depth * 128, E * 8)
    m["beuT"] = np.ascontiguousarray(f(inp["b_exp_up"]).reshape(depth, E * 8, 128).transpose(0, 2, 1)).reshape(depth * 128, E * 8)
    m["b_exp_down"] = f(inp["b_exp_down"]).reshape(depth * E, D)
    m.update(_consts())
    return m


def kernel(**inputs):
    depth, n_exp, n_cores = 4, 32, 8
    x = np.asarray(inputs["x"], dtype=np.float32)
    shared = prepare_inputs(inputs, depth, n_exp)
    nc = build_program(depth, n_exp)
    in_maps = []
    for c in range(n_cores):
        m = dict(shared)
        m["x"] = np.ascontiguousarray(x[c])
        in_maps.append(m)
    res = run_bass_kernel_spmd(nc, in_maps, core_ids=list(range(n_cores)))
    return np.stack([np.asarray(res.results[c]["y"], dtype=np.float32) for c in range(n_cores)], axis=0)
```

```python
import math
from contextlib import ExitStack
import numpy as np
import ml_dtypes
import concourse.bass as bass
import concourse.mybir as mybir
from concourse.bass_utils import run_bass_kernel_spmd

F32 = mybir.dt.float32
BF16 = mybir.dt.bfloat16
I32 = mybir.dt.int32
AF = mybir.ActivationFunctionType
ALU = mybir.AluOpType
AX = mybir.AxisListType

S = 4096
D = 1024
NTB = 8
LN_EPS = 1e-5
NEG = -30000.0
SW_ALPHA = 1.702
SW_LIM = 7.0
B_JB = [(-1, 4), (-2, 5), (-8, 11)]
B_OFF = [0, 6, 14]
TS = 512
NT = 63
R_XS = NT * TS


class Trk:
    def __init__(s, nc, es):
        s.nc = nc
        s.es = es
        s.eng = {'pe': nc.tensor, 'act': nc.scalar, 'dve': nc.vector, 'pool': nc.gpsimd, 'sp': nc.sync}
        s.esem = {k: es.enter_context(nc.semaphore('e_' + k)) for k in ('pe', 'act', 'dve', 'pool')}
        s.ecnt = {k: 0 for k in s.esem}
        s.dsem = {}
        s.dcnt = {}
        s.waited = {k: {} for k in s.eng}
        s.lastw = {}
        s.rd = {}

    def _wait(s, e, tok):
        kind, key, val = tok
        if kind == 'e':
            if key == 'pe' and e == 'pe':
                return
            if val <= 0 or s.waited[e].get(('e', key), 0) >= val:
                return
            s.eng[e].wait_ge(s.esem[key], val)
            s.waited[e][('e', key)] = val
        else:
            tgt = s.dcnt[key]
            if tgt <= 0 or s.waited[e].get(('d', key), 0) >= tgt:
                return
            s.eng[e].wait_ge(s.dsem[key], tgt)
            s.waited[e][('d', key)] = tgt

    def _deps(s, e, reads, writes):
        for r in reads:
            t = s.lastw.get(r)
            if t:
                s._wait(e, t)
        for w in writes:
            t = s.lastw.get(w)
            if t:
                s._wait(e, t)
            for t in s.rd.get(w, {}).values():
                s._wait(e, t)

    def _record(s, tok, reads, writes):
        for r in reads:
            s.rd.setdefault(r, {})[(tok[0], tok[1])] = tok
        for w in writes:
            s.lastw[w] = tok
            s.rd[w] = {}

    def op(s, e, fn, reads=(), writes=()):
        s._deps(e, reads, writes)
        ins = fn(s.eng[e])
        s.ecnt[e] += 1
        ins.then_inc(s.esem[e], 1)
        s._record(('e', e, s.ecnt[e]), reads, writes)

    def dma(s, q, out, in_, reads=(), writes=(), key=None):
        s._deps(q, reads, writes)
        if key not in s.dsem:
            s.dsem[key] = s.es.enter_context(s.nc.semaphore('d_%d' % len(s.dsem)))
            s.dcnt[key] = 0
        s.eng[q].dma_start(out=out, in_=in_).then_inc(s.dsem[key], 16)
        s.dcnt[key] += 16
        s._record(('d', key, s.dcnt[key]), reads, writes)

    def idma(s, out, in_, out_off=None, in_off=None, bound=None, reads=(), writes=(), key=None):
        s._deps('pool', reads, writes)
        if key not in s.dsem:
            s.dsem[key] = s.es.enter_context(s.nc.semaphore('d_%d' % len(s.dsem)))
            s.dcnt[key] = 0
        s.nc.gpsimd.indirect_dma_start(out=out, out_offset=out_off, in_=in_, in_offset=in_off).then_inc(s.dsem[key], 16)
        s.dcnt[key] += 16
        s._record(('d', key, s.dcnt[key]), reads, writes)

    def barrier(s):
        for e in s.eng:
            for k in s.esem:
                s._wait(e, ('e', k, s.ecnt[k]))
            for k in s.dsem:
                s._wait(e, ('d', k, 0))
        s.lastw = {}
        s.rd = {}


class Rot:
    def __init__(s, name, views):
        s.name = name
        s.views = views
        s.i = -1

    def next(s):
        s.i = (s.i + 1) % len(s.views)
        return s.views[s.i], (s.name, s.i)


def build_program(depth, n_exp, debug=False):
    nc = bass.Bass("TRN2", target_bir_lowering=False)
    es = ExitStack()
    T = Trk(nc, es)
    E = n_exp

    def din(name, shape, dt=F32):
        return nc.dram_tensor(name, list(shape), dt, kind="ExternalInput").ap()

    def dscr(name, shape, dt):
        kind = "ExternalOutput" if debug else "Internal"
        return nc.dram_tensor(name, list(shape), dt, kind=kind).ap()

    x_in = din("x", [S, D])
    w_in = din("w_in", [depth * D, 6144])
    bgT = din("bgT", [depth * 128, 24])
    w_pa = din("w_proj_a", [depth * 256, D])
    w_pb = din("w_proj_b", [depth * 128, D])
    w_pc = din("w_proj_c", [depth * 384, D])
    w_out = din("w_out", [depth * D, D])
    biasA = din("biasA", [depth * 4 * 128, 8 * 512])
    dlam = din("diff_lambda", [depth, 192])
    dng = din("diff_norm_g", [depth * 96, 1])
    ln1g = din("ln1_g", [depth, D])
    ln1b = din("ln1_b", [depth, D])
    ln2g = din("ln2_g", [depth, D])
    ln2b = din("ln2_b", [depth, D])
    w_r = din("w_router", [depth * D, E])
    b_r = din("b_router", [depth, E])
    w_eg = din("w_exp_gate", [depth * E * D, D])
    w_eu = din("w_exp_up", [depth * E * D, D])
    w_ed = din("w_exp_down", [depth * E * D, D])
    begP = din("begP", [depth * E * 128, 8])
    beuP = din("beuP", [depth * E * 128, 8])
    c_tri = din("c_tri", [128, 128], BF16)
    c_iop = din("c_iop", [128, 1])
    b_ed = din("b_exp_down", [depth * E, D])
    c_ident = din("c_ident", [128, 128], BF16)
    c_cosB = din("c_cosB", [128, S])
    c_sinB = din("c_sinB", [128, S])
    c_cosC = din("c_cosC", [96, S])
    c_sinC = din("c_sinC", [96, S])
    c_maskA = din("c_maskA", [128, 24 * 512], BF16)
    c_maskB = din("c_maskB", [128, 34 * 512], BF16)
    y_out = nc.dram_tensor("y", [S, D], F32, kind="ExternalOutput").ap()

    xT_d = dscr("xT_d", [D, S], BF16)
    x1T_d = dscr("x1T_d", [D, S], BF16)
    QT_d = dscr("QT_d", [D, S], BF16)
    KT_d = dscr("KT_d", [D, S], BF16)
    V_d = dscr("V_d", [S, D], BF16)
    YT_d = dscr("YT_d", [768, S], BF16)
    x1_d = dscr("x1_d", [S, D], F32)
    x2_d = dscr("x2_d", [S, D], F32)
    lam_d = dscr("lam_d", [1, 8], F32)
    x1b_d = dscr("x1b_d", [S, D], BF16)
    XS_d = dscr("XS_d", [R_XS, D], BF16)
    YS_d = dscr("YS_d", [R_XS, D], BF16)

    sb_n = [0]

    def sb(stack, name, shape, dt):
        sb_n[0] += 1
        return stack.enter_context(nc.sbuf_tensor("%s_%d" % (name, sb_n[0]), list(shape), dt))

    ident = sb(es, "ident", [128, 128], BF16)
    onesb = sb(es, "onesb", [128, 128], BF16)
    psS = [es.enter_context(nc.psum_tensor("psS%d" % i, [128, 512], F32)) for i in range(3)]
    psA = [es.enter_context(nc.psum_tensor("psA%d" % i, [128, 512], F32)) for i in range(4)]
    psT = es.enter_context(nc.psum_tensor("psT", [128, 1024], BF16))
    Spool = Rot("psS", psS)
    Apool = Rot("psA", psA)

    T.dma('sp', ident[:], c_ident[:, :], writes=[("ident",)], key="const")
    T.op('pool', lambda e: e.memset(onesb[:], 1.0), writes=[("onesb",)])

    def ln_epilogue(st, z, zres, g_t, b_t, row0, dst_f32, dstT, pools, dst_b16=None):
        stt, mv, xo_p, xb_p, xt_p = pools
        st6, st6r = stt.next()
        T.op('dve', lambda e: e.bn_stats(st6[:, 0:6], z[:, 0:512]), reads=zres, writes=[st6r + ("a",)])
        T.op('dve', lambda e: e.bn_stats(st6[:, 6:12], z[:, 512:1024]), reads=zres, writes=[st6r + ("b",)])
        m, mr = mv.next()
        T.op('dve', lambda e: e.bn_aggr(m[:, 0:2], st6[:, 0:12]), reads=[st6r + ("a",), st6r + ("b",)], writes=[mr])
        T.op('act', lambda e: e.activation(out=m[:, 3:4], in_=m[:, 1:2], func=AF.Ln, bias=LN_EPS),
             reads=[mr], writes=[mr + ("l",)])
        T.op('act', lambda e: e.activation(out=m[:, 2:3], in_=m[:, 3:4], func=AF.Exp, scale=-0.5),
             reads=[mr + ("l",)], writes=[mr + ("r",)])
        xo, xor_ = xo_p.next()
        T.op('dve', lambda e: e.tensor_scalar(xo[:], z[:], m[:, 0:1], m[:, 2:3], ALU.subtract, ALU.mult),
             reads=zres + [mr, mr + ("r",)], writes=[xor_])
        T.op('pool', lambda e: e.tensor_tensor(xo[:], xo[:], g_t[:], ALU.mult), reads=[xor_, ("lng",)], writes=[xor_])
        T.op('pool', lambda e: e.tensor_tensor(xo[:], xo[:], b_t[:], ALU.add), reads=[xor_, ("lnb",)], writes=[xor_])
        T.dma('sp', dst_f32[row0:row0 + 128, :], xo[:], reads=[xor_], key="st_x")
        if dstT is not None:
            xb, xbr = xb_p.next()
            T.op('act', lambda e: e.activation(out=xb[:], in_=xo[:], func=AF.Copy), reads=[xor_], writes=[xbr])
            if dst_b16 is not None:
                T.dma('sp', dst_b16[row0:row0 + 128, :], xb[:], reads=[xbr], key="st_xb")
            transpose_store(xb, xbr, row0, dstT, xt_p)

    def transpose_store(xb, xbr, row0, dstT, xt_p):
        for fc in range(8):
            T.op('pe', lambda e, fc=fc: e.transpose(psT[:, fc * 128:(fc + 1) * 128], xb[:, fc * 128:(fc + 1) * 128], ident[:]),
                 reads=[xbr, ("ident",)], writes=[("psT", fc)])
        xt, xtr = xt_p.next()
        T.op('act', lambda e: e.activation(out=xt[:], in_=psT[:], func=AF.Copy),
             reads=[("psT", fc) for fc in range(8)], writes=[xtr])
        T.dma('sp', dstT[:, row0:row0 + 128].rearrange("(fc p) t -> p fc t", p=128),
              xt[:].rearrange("p (fc t) -> p fc t", t=128), reads=[xtr], key="st_xT")

    with ExitStack() as st:
        xl = [sb(st, "p_xl%d" % i, [128, D], F32) for i in range(2)]
        xbs = [sb(st, "p_xb%d" % i, [128, D], BF16) for i in range(2)]
        xts = [sb(st, "p_xt%d" % i, [128, D], BF16) for i in range(2)]
        xl_p, xb_p, xt_p = Rot("p_xl", xl), Rot("p_xb", xbs), Rot("p_xt", xts)
        zt = sb(st, "p_zero", [128, 4096], BF16)
        T.op('pool', lambda e: e.memset(zt[:], 0.0), writes=[("zt",)])
        XSv = XS_d.rearrange("(p a) d -> p (a d)", p=128)
        for c in range(R_XS * D // 128 // 4096):
            T.dma('sp', XSv[:, c * 4096:(c + 1) * 4096], zt[:], reads=[("zt",)], key="zero")
        for tt in range(32):
            xt_, xr = xl_p.next()
            T.dma('sp', xt_[:], x_in[tt * 128:(tt + 1) * 128, :], writes=[xr], key=("ldx", xr))
            xb, xbr = xb_p.next()
            T.op('act', lambda e: e.activation(out=xb[:], in_=xt_[:], func=AF.Copy), reads=[xr], writes=[xbr])
            transpose_store(xb, xbr, tt * 128, xT_d, xt_p)
        T.barrier()

    x_cur = x_in
    for l in range(depth):
        lam_init = 0.8 - 0.6 * math.exp(-0.3 * l)
        last = (l == depth - 1)
        with ExitStack() as st:
            wq = sb(st, "a_wq", [128, 8, 3072], BF16)
            wrot = sb(st, "a_wrot", [128, 8, 1536], BF16)
            for kc in range(8):
                T.dma('pool', wq[:, kc, :], w_in[l * D + kc * 128:l * D + (kc + 1) * 128, 0:3072],
                      writes=[("wq", kc)], key=("wq", kc % 2))
            for kc in range(8):
                for (sbase, dbase, nu, w) in ((768, 0, 12, 64), (1920, 768, 16, 48)):
                    h = w // 2
                    src = wq[:, kc, sbase:sbase + nu * w].rearrange("p (u j) -> p u j", j=w)
                    dst = wrot[:, kc, dbase:dbase + nu * w].rearrange("p (u j) -> p u j", j=w)
                    T.op('pool', lambda e, src=src, dst=dst, h=h, w=w: e.tensor_scalar(
                        dst[:, :, 0:h], src[:, :, h:w], -1.0, None, ALU.mult),
                        reads=[("wq", kc)], writes=[("wrot", kc, dbase, 0)])
                    T.op('pool', lambda e, src=src, dst=dst, h=h, w=w: e.tensor_copy(dst[:, :, h:w], src[:, :, 0:h]),
                         reads=[("wq", kc)], writes=[("wrot", kc, dbase, 1)])
            wq_res = [("wq", kc) for kc in range(8)]
            wrot_res = [("wrot", kc, db, hh) for kc in range(8) for db in (0, 768) for hh in (0, 1)]
            xts = [sb(st, "a_xt%d" % i, [128, 8, 512], BF16) for i in range(2)]
            tabs = [sb(st, "a_tab%d" % i, [128, 4, 512], F32) for i in range(2)]
            t1s = [sb(st, "a_t1%d" % i, [128, 512], F32) for i in range(2)]
            t2s = [sb(st, "a_t2%d" % i, [128, 512], F32) for i in range(2)]
            obs = [sb(st, "a_ob%d" % i, [128, 512], BF16) for i in range(3)]
            vts = [sb(st, "a_vt%d" % i, [128, 1024], BF16) for i in range(2)]
            xt_p, tab_p, t1_p, t2_p, ob_p, vt_p = (Rot("a_xt", xts), Rot("a_tab", tabs), Rot("a_t1", t1s),
                                                   Rot("a_t2", t2s), Rot("a_ob", obs), Rot("a_vt", vts))
            chunks = []
            for i in range(2):
                chunks.append((i * 128, 128, QT_d, i * 128, None, 0, 0.125))
                chunks.append((256 + i * 128, 128, KT_d, i * 128, None, 0, 1.0))
            for i in range(3):
                chunks.append((768 + i * 128, 128, QT_d, 256 + i * 128, i * 128, 0, 1.0))
                chunks.append((1152 + i * 128, 128, KT_d, 256 + i * 128, 384 + i * 128, 0, 1.0))
            for i in range(4):
                chunks.append((1920 + i * 96, 96, QT_d, 640 + i * 96, 768 + i * 96, 1, 1.0))
                chunks.append((2304 + i * 96, 96, KT_d, 640 + i * 96, 1152 + i * 96, 1, 1.0))
            for tb in range(NTB):
                c0 = tb * 512
                xt, xtr = xt_p.next()
                T.dma('sp', xt[:], xT_d[:, c0:c0 + 512].rearrange("(kc p) t -> p kc t", p=128), writes=[xtr], key=("a_xt", xtr))
                tab, tabr = tab_p.next()
                T.dma('sp', tab[:, 0, :], c_cosB[:, c0:c0 + 512], writes=[tabr + (0,)], key=("a_tab", tabr))
                T.dma('sp', tab[:, 1, :], c_sinB[:, c0:c0 + 512], writes=[tabr + (1,)], key=("a_tab", tabr))
                T.dma('sp', tab[0:96, 2, :], c_cosC[:, c0:c0 + 512], writes=[tabr + (2,)], key=("a_tab", tabr))
                T.dma('sp', tab[0:96, 3, :], c_sinC[:, c0:c0 + 512], writes=[tabr + (3,)], key=("a_tab", tabr))
                for (col0, M, dst, drow, rc0, ti, scale) in chunks:
                    p1, p1r = Spool.next()
                    for kc in range(8):
                        T.op('pe', lambda e, kc=kc, p1=p1: e.matmul(p1[0:M, :], wq[:, kc, col0:col0 + M], xt[:, kc, :],
                                                                 start=(kc == 0), stop=(kc == 7)),
                             reads=[("wq", kc), xtr], writes=[p1r])
                    ob, obr = ob_p.next()
                    if rc0 is None:
                        T.op('act', lambda e, p1=p1, ob=ob: e.activation(out=ob[0:M, :], in_=p1[0:M, :], func=AF.Copy, scale=scale),
                             reads=[p1r], writes=[obr])
                    else:
                        p2, p2r = Spool.next()
                        for kc in range(8):
                            T.op('pe', lambda e, kc=kc, p2=p2: e.matmul(p2[0:M, :], wrot[:, kc, rc0:rc0 + M], xt[:, kc, :],
                                                                     start=(kc == 0), stop=(kc == 7)),
                                 reads=wrot_res[kc * 4:(kc + 1) * 4] + [xtr], writes=[p2r])
                        t1, t1r = t1_p.next()
                        t2, t2r = t2_p.next()
                        T.op('dve', lambda e, p1=p1, t1=t1: e.tensor_tensor(t1[0:M, :], p1[0:M, :], tab[0:M, 2 * ti, :], ALU.mult),
                             reads=[p1r, tabr + (2 * ti,)], writes=[t1r])
                        T.op('dve', lambda e, p2=p2, t2=t2: e.tensor_tensor(t2[0:M, :], p2[0:M, :], tab[0:M, 2 * ti + 1, :], ALU.mult),
                             reads=[p2r, tabr + (2 * ti + 1,)], writes=[t2r])
                        T.op('pool', lambda e, t1=t1, t2=t2, ob=ob: e.tensor_tensor(ob[0:M, :], t1[0:M, :], t2[0:M, :], ALU.add),
                             reads=[t1r, t2r], writes=[obr])
                    T.dma('sp', dst[drow:drow + M, c0:c0 + 512], ob[0:M, :], reads=[obr], key="st_a")
                for tt in range(4):
                    vt, vtr = vt_p.next()
                    for (vc0, n, dcol) in ((512, 256, 0), (1536, 384, 256), (2688, 384, 640)):
                        pv, pvr = Spool.next()
                        for kc in range(8):
                            T.op('pe', lambda e, kc=kc, pv=pv: e.matmul(pv[:, 0:n], xt[:, kc, tt * 128:(tt + 1) * 128],
                                                                     wq[:, kc, vc0:vc0 + n], start=(kc == 0), stop=(kc == 7)),
                                 reads=[("wq", kc), xtr], writes=[pvr])
                        T.op('act', lambda e, pv=pv, vt=vt: e.activation(out=vt[:, dcol:dcol + n], in_=pv[:, 0:n], func=AF.Copy),
                             reads=[pvr], writes=[vtr + (dcol,)])
                    T.dma('sp', V_d[c0 + tt * 128:c0 + (tt + 1) * 128, :], vt[:],
                          reads=[vtr + (0,), vtr + (256,), vtr + (640,)], key="st_v")
            T.barrier()

        with ExitStack() as st:
            kts = [sb(st, "b_kt%d" % i, [64, S], BF16) for i in range(6)]
            vhs = [sb(st, "b_vh%d" % i, [128, 32, 96], BF16) for i in range(6)]
            maskt = sb(st, "b_mask", [128, 34, 512], BF16)
            biast = sb(st, "b_bias", [128, 8, 512], BF16)
            qts = [sb(st, "b_q%d" % i, [64, 512], BF16) for i in range(4)]
            ets = [sb(st, "b_e%d" % i, [128, 512], BF16) for i in range(4)]
            rts = [sb(st, "b_r%d" % i, [96, 512], F32) for i in range(2)]
            fts = [sb(st, "b_f%d" % i, [96, 512], F32) for i in range(4)]
            sqs = [sb(st, "b_sq%d" % i, [96, 512], BF16) for i in range(2)]
            yos = [sb(st, "b_yo%d" % i, [96, 512], BF16) for i in range(2)]
            lamt = sb(st, "b_lam", [1, 200], F32)
            nlam = sb(st, "b_nlam", [96, 1], F32)
            gpr = sb(st, "b_gpr", [96, 1], F32)
            q_p, e_p, r_p, f_p, sq_p, yo_p = (Rot("b_q", qts), Rot("b_e", ets), Rot("b_r", rts), Rot("b_f", fts),
                                              Rot("b_sq", sqs), Rot("b_yo", yos))

            def load_k(slot, row0, d):
                T.dma('sp', kts[slot][0:d, :], KT_d[row0:row0 + d, :], writes=[("kt", slot)], key=("kt", slot))

            def load_v(slot, col0, e_):
                T.dma('sp', vhs[slot][:, :, 0:e_], V_d[:, col0:col0 + e_].rearrange("(kb p) e -> p kb e", p=128),
                      writes=[("vh", slot)], key=("vh", slot))

            def load_q(row0, d, qb):
                q, qr = q_p.next()
                T.dma('sp', q[0:d, :], QT_d[row0:row0 + d, qb * 512:(qb + 1) * 512], writes=[qr], key=("q", qr))
                return q, qr

            def attn_tiles(items, e_, accs, scale):
                num, numr, den, denr = accs
                n = len(items)
                for i, (ks, d, q, qr, kb, biases, vs) in enumerate(items):
                    sp_, spr = Spool.next()
                    T.op('pe', lambda e, sp_=sp_, ks=ks, d=d, q=q, kb=kb: e.matmul(
                        sp_[:, :], kts[ks][0:d, kb * 128:(kb + 1) * 128], q[0:d, :], start=True, stop=(len(biases) == 0)),
                        reads=[("kt", ks), qr], writes=[spr])
                    for bi, (bap, bres) in enumerate(biases):
                        T.op('pe', lambda e, sp_=sp_, bap=bap, bi=bi: e.matmul(sp_[:, :], ident[:], bap, start=False,
                                                                             stop=(bi == len(biases) - 1)),
                             reads=[("ident",), bres], writes=[spr])
                    et, etr = e_p.next()
                    T.op('act', lambda e, sp_=sp_, et=et: e.activation(out=et[:], in_=sp_[:, :], func=AF.Exp, scale=scale),
                         reads=[spr], writes=[etr])
                    T.op('pe', lambda e, et=et, vs=vs, kb=kb, i=i: e.matmul(num[0:e_, :], vhs[vs][:, kb, 0:e_], et[:],
                                                                          start=(i == 0), stop=(i == n - 1)),
                         reads=[("vh", vs), etr], writes=[numr])
                    T.op('pe', lambda e, et=et, i=i: e.matmul(den[0:e_, :], onesb[:, 0:e_], et[:],
                                                            start=(i == 0), stop=(i == n - 1)),
                         reads=[("onesb",), etr], writes=[denr])

            def finalize_simple(accs, e_, yrow, qb):
                num, numr, den, denr = accs
                r, rr = r_p.next()
                T.op('dve', lambda e: e.reciprocal(r[0:e_, :], den[0:e_, :]), reads=[denr], writes=[rr])
                yo, yor = yo_p.next()
                T.op('dve', lambda e: e.tensor_tensor(yo[0:e_, :], num[0:e_, :], r[0:e_, :], ALU.mult),
                     reads=[numr, rr], writes=[yor])
                T.dma('sp', YT_d[yrow:yrow + e_, qb * 512:(qb + 1) * 512], yo[0:e_, :], reads=[yor], key="st_y")

            T.dma('sp', maskt[:, 0:24, :], c_maskA[:, :].rearrange("p (j q) -> p j q", q=512), writes=[("mask",)], key="mask")
            for h in range(4):
                load_k(0, h * 64, 64)
                load_v(0, h * 64, 64)
                r0 = (l * 4 + h) * 128
                T.dma('pool', biast[:], biasA[r0:r0 + 128, :].rearrange("p (j q) -> p j q", q=512), writes=[("bias",)], key="bias")
                for qb in range(8):
                    q, qr = load_q(h * 64, 64, qb)
                    var = 0 if qb == 0 else (2 if qb == 7 else 1)
                    items = []
                    for j in range(8):
                        kb = 4 * qb - 2 + j
                        if 0 <= kb < 32:
                            items.append((0, 64, q, qr, kb, [(biast[:, j, :], ("bias",)), (maskt[:, var * 8 + j, :], ("mask",))], 0))
                    num, numr = Apool.next()
                    den, denr = Apool.next()
                    attn_tiles(items, 64, (num, numr, den, denr), 1.0)
                    finalize_simple((num, numr, den, denr), 64, h * 64, qb)
            T.dma('sp', maskt[:, :, :], c_maskB[:, :].rearrange("p (j q) -> p j q", q=512), writes=[("mask",)], key="mask")
            for u in range(6):
                load_k(u, 256 + u * 64, 64)
                load_v(u, 256 + u * 64, 64)
            for jo in range(2):
                for qb in range(8):
                    items = []
                    for g in range(3):
                        u = 2 * g + jo
                        q, qr = load_q(256 + u * 64, 64, qb)
                        for jb in range(B_JB[g][0], B_JB[g][1] + 1):
                            kb = 4 * qb + jb
                            if 0 <= kb < 32:
                                mi = B_OFF[g] + jb - B_JB[g][0]
                                items.append((u, 64, q, qr, kb, [(maskt[:, mi, :], ("mask",))], u))
                    num, numr = Apool.next()
                    den, denr = Apool.next()
                    attn_tiles(items, 64, (num, numr, den, denr), 0.125)
                    finalize_simple((num, numr, den, denr), 64, 256 + jo * 64, qb)
            T.dma('sp', lamt[:, 0:192], dlam[l:l + 1, :], writes=[("lamt",)], key="lam")
            T.op('dve', lambda e: e.tensor_tensor(lamt[:, 0:48], lamt[:, 0:48], lamt[:, 48:96], ALU.mult),
                 reads=[("lamt",)], writes=[("lamt", 0)])
            T.op('dve', lambda e: e.tensor_tensor(lamt[:, 48:96], lamt[:, 96:144], lamt[:, 144:192], ALU.mult),
                 reads=[("lamt",)], writes=[("lamt", 1)])
            T.op('dve', lambda e: e.reduce_sum(lamt[:, 192:194], lamt[:, 0:96].rearrange("p (a b) -> p a b", b=48), AX.X),
                 reads=[("lamt", 0), ("lamt", 1)], writes=[("lamt", 2)])
            T.op('act', lambda e: e.activation(out=lamt[:, 194:196], in_=lamt[:, 192:194], func=AF.Exp),
                 reads=[("lamt", 2)], writes=[("lamt", 3)])
            T.op('dve', lambda e: e.tensor_tensor(lamt[:, 196:197], lamt[:, 194:195], lamt[:, 195:196], ALU.subtract),
                 reads=[("lamt", 3)], writes=[("lamt", 4)])
            T.op('dve', lambda e: e.tensor_scalar(lamt[:, 197:198], lamt[:, 196:197], -1.0, -lam_init, ALU.mult, ALU.add),
                 reads=[("lamt", 4)], writes=[("lamt", 5)])
            T.dma('sp', lam_d[0:1, 0:1], lamt[:, 197:198], reads=[("lamt", 5)], writes=[("lam_d",)], key="lam2")
            T.dma('sp', nlam[:], lam_d[0, 0:1].partition_broadcast(96), reads=[("lam_d",)], writes=[("nlam",)], key="lam3")
            T.dma('sp', gpr[:], dng[l * 96:(l + 1) * 96, :], writes=[("gpr",)], key="lam4")
            T.op('dve', lambda e: e.tensor_scalar(gpr[:], gpr[:], 1.0 - lam_init, None, ALU.mult),
                 reads=[("gpr",)], writes=[("gpr",)])
            for h in range(4):
                for c in range(2):
                    load_k(c, 640 + h * 96 + c * 48, 48)
                load_v(0, 640 + h * 96, 96)
                for qb in range(8):
                    accs = []
                    for c in range(2):
                        q, qr = load_q(640 + h * 96 + c * 48, 48, qb)
                        num, numr = Apool.next()
                        den, denr = Apool.next()
                        items = [(c, 48, q, qr, kb, [], 0) for kb in range(32)]
                        attn_tiles(items, 96, (num, numr, den, denr), 48 ** -0.5)
                        accs.append((num, numr, den, denr))
                    fs = []
                    for c in range(2):
                        num, numr, den, denr = accs[c]
                        r, rr = r_p.next()
                        T.op('dve', lambda e, r=r, den=den: e.reciprocal(r[:, :], den[0:96, :]), reads=[denr], writes=[rr])
                        f, fr = f_p.next()
                        T.op('dve', lambda e, f=f, num=num, r=r: e.tensor_tensor(f[:, :], num[0:96, :], r[:, :], ALU.mult),
                             reads=[numr, rr], writes=[fr])
                        fs.append((f, fr))
                    o, orr = f_p.next()
                    T.op('dve', lambda e: e.scalar_tensor_tensor(o[:, :], fs[1][0][:, :], nlam[:, 0:1], fs[0][0][:, :], ALU.mult, ALU.add),
                         reads=[fs[0][1], fs[1][1], ("nlam",)], writes=[orr])
                    sq, sqr = sq_p.next()
                    T.op('pool', lambda e: e.tensor_tensor(sq[:, :], o[:, :], o[:, :], ALU.mult), reads=[orr], writes=[sqr])
                    ms, msr = Spool.next()
                    T.op('pe', lambda e: e.matmul(ms[0:96, :], onesb[0:96, 0:96], sq[:, :], start=True, stop=True),
                         reads=[("onesb",), sqr], writes=[msr])
                    lnv, lnr = f_p.next()
                    T.op('act', lambda e: e.activation(out=lnv[:, :], in_=ms[0:96, :], func=AF.Ln, scale=1.0 / 96.0, bias=LN_EPS),
                         reads=[msr], writes=[lnr])
                    T.op('act', lambda e: e.activation(out=lnv[:, :], in_=lnv[:, :], func=AF.Exp, scale=-0.5),
                         reads=[lnr], writes=[lnr])
                    yo, yor = yo_p.next()
                    T.op('dve', lambda e: e.scalar_tensor_tensor(yo[:, :], o[:, :], gpr[:, 0:1], lnv[:, :], ALU.mult, ALU.mult),
                         reads=[orr, lnr, ("gpr",)], writes=[yor])
                    T.dma('sp', YT_d[384 + h * 96:384 + (h + 1) * 96, qb * 512:(qb + 1) * 512], yo[:, :], reads=[yor], key="st_y")
            T.barrier()

        with ExitStack() as st:
            wg = sb(st, "c_wg", [128, 8, 3072], BF16)
            wp = sb(st, "c_wp", [128, 6, D], BF16)
            wo = sb(st, "c_wo", [128, 8, D], BF16)
            bgt = sb(st, "c_bg", [128, 24], F32)
            g_t = sb(st, "c_lng", [128, D], F32)
            b_t = sb(st, "c_lnb", [128, D], F32)
            for kc in range(8):
                T.dma('pool', wg[:, kc, :], w_in[l * D + kc * 128:l * D + (kc + 1) * 128, 3072:6144],
                      writes=[("wg", kc)], key=("wg", kc % 2))
                T.dma('pool', wo[:, kc, :], w_out[l * D + kc * 128:l * D + (kc + 1) * 128, :], writes=[("wo", kc)], key=("wo", kc % 2))
            for c in range(2):
                T.dma('pool', wp[:, c, :], w_pa[l * 256 + c * 128:l * 256 + (c + 1) * 128, :], writes=[("wp", c)], key="wp")
            T.dma('pool', wp[:, 2, :], w_pb[l * 128:(l + 1) * 128, :], writes=[("wp", 2)], key="wp")
            for c in range(3):
                T.dma('pool', wp[:, 3 + c, :], w_pc[l * 384 + c * 128:l * 384 + (c + 1) * 128, :], writes=[("wp", 3 + c)], key="wp")
            T.dma('sp', bgt[:], bgT[l * 128:(l + 1) * 128, :], writes=[("bgt",)], key="cmisc")
            T.dma('sp', g_t[:], ln1g[l, :].partition_broadcast(128), writes=[("lng",)], key="cmisc")
            T.dma('sp', b_t[:], ln1b[l, :].partition_broadcast(128), writes=[("lnb",)], key="cmisc")
            xts = [sb(st, "c_xt%d" % i, [128, 8, 512], BF16) for i in range(2)]
            yts = [sb(st, "c_yt%d" % i, [128, 6, 512], BF16) for i in range(2)]
            gts = [sb(st, "c_g%d" % i, [128, 512], F32) for i in range(3)]
            mts = [sb(st, "c_m%d" % i, [128, 512], F32) for i in range(4)]
            mgs = [sb(st, "c_mg%d" % i, [128, 8, 512], BF16) for i in range(2)]
            xrs = [sb(st, "c_xr%d" % i, [128, D], F32) for i in range(2)]
            zs = [sb(st, "c_z%d" % i, [128, D], F32) for i in range(2)]
            lnpools = (Rot("c_st", [sb(st, "c_st%d" % i, [128, 12], F32) for i in range(2)]),
                       Rot("c_mv", [sb(st, "c_mv%d" % i, [128, 4], F32) for i in range(2)]),
                       Rot("c_xo", [sb(st, "c_xo%d" % i, [128, D], F32) for i in range(2)]),
                       Rot("c_xb", [sb(st, "c_xb%d" % i, [128, D], BF16) for i in range(2)]),
                       Rot("c_xT", [sb(st, "c_xT%d" % i, [128, D], BF16) for i in range(2)]))
            xt_p, yt_p, g_p, m_p, mg_p, xr_p, z_p = (Rot("c_xt", xts), Rot("c_yt", yts), Rot("c_g", gts), Rot("c_m", mts),
                                                     Rot("c_mg", mgs), Rot("c_xr", xrs), Rot("c_z", zs))
            br_chunks = [(0, 2), (2, 3), (3, 6)]
            alpha = (2 * depth) ** 0.25
            for tb in range(NTB):
                c0 = tb * 512
                xt, xtr = xt_p.next()
                T.dma('sp', xt[:], xT_d[:, c0:c0 + 512].rearrange("(kc p) t -> p kc t", p=128), writes=[xtr], key=("c_xt", xtr))
                yt, ytr = yt_p.next()
                T.dma('sp', yt[:], YT_d[:, c0:c0 + 512].rearrange("(c p) t -> p c t", p=128), writes=[ytr], key=("c_yt", ytr))
                mg, mgr = mg_p.next()
                for fc in range(8):
                    ms_ = []
                    for i in range(3):
                        pg, pgr = Spool.next()
                        for kc in range(8):
                            T.op('pe', lambda e, kc=kc, pg=pg, i=i: e.matmul(
                                pg[:, :], wg[:, kc, i * D + fc * 128:i * D + (fc + 1) * 128], xt[:, kc, :],
                                start=(kc == 0), stop=(kc == 7)), reads=[("wg", kc), xtr], writes=[pgr])
                        g, gr = g_p.next()
                        T.op('act', lambda e, pg=pg, g=g, i=i: e.activation(out=g[:], in_=pg[:, :], func=AF.Sigmoid,
                                                                         bias=bgt[:, i * 8 + fc:i * 8 + fc + 1]),
                             reads=[pgr, ("bgt",)], writes=[gr])
                        pp, ppr = Spool.next()
                        cs = list(range(*br_chunks[i]))
                        for ci, c in enumerate(cs):
                            T.op('pe', lambda e, c=c, ci=ci, pp=pp, cs=cs: e.matmul(
                                pp[:, :], wp[:, c, fc * 128:(fc + 1) * 128], yt[:, c, :],
                                start=(ci == 0), stop=(ci == len(cs) - 1)), reads=[("wp", c), ytr], writes=[ppr])
                        m, mr = m_p.next()
                        T.op('dve', lambda e, m=m, g=g, pp=pp: e.tensor_tensor(m[:], pp[:, :], g[:], ALU.mult),
                             reads=[ppr, gr], writes=[mr])
                        ms_.append((m, mr))
                    T.op('pool', lambda e, ms_=ms_: e.tensor_tensor(ms_[0][0][:], ms_[0][0][:], ms_[1][0][:], ALU.add),
                         reads=[ms_[0][1], ms_[1][1]], writes=[ms_[0][1]])
                    T.op('pool', lambda e, ms_=ms_, fc=fc: e.tensor_tensor(mg[:, fc, :], ms_[0][0][:], ms_[2][0][:], ALU.add),
                         reads=[ms_[0][1], ms_[2][1]], writes=[mgr + (fc,)])
                for tt in range(4):
                    row0 = c0 + tt * 128
                    xr, xrr = xr_p.next()
                    T.dma('sp', xr[:], x_cur[row0:row0 + 128, :], writes=[xrr], key=("c_xr", xrr))
                    z, zr = z_p.next()
                    for half in range(2):
                        py, pyr = Apool.next()
                        for fc in range(8):
                            T.op('pe', lambda e, fc=fc, py=py, half=half: e.matmul(
                                py[:, :], mg[:, fc, tt * 128:(tt + 1) * 128], wo[:, fc, half * 512:(half + 1) * 512],
                                start=(fc == 0), stop=(fc == 7)), reads=[mgr + (fc,), ("wo", fc)], writes=[pyr])
                        T.op('dve', lambda e, py=py, half=half: e.scalar_tensor_tensor(
                            z[:, half * 512:(half + 1) * 512], xr[:, half * 512:(half + 1) * 512], alpha, py[:, :], ALU.mult, ALU.add),
                            reads=[pyr, xrr], writes=[zr + (half,)])
                    ln_epilogue(st, z, [zr + (0,), zr + (1,)], g_t, b_t, row0, x1_d, x1T_d, lnpools, dst_b16=x1b_d)
            T.barrier()

        alpha = (2 * depth) ** 0.25
        dst_f32 = y_out if last else x2_d
        dstT = None if last else xT_d
        IND = bass.IndirectOffsetOnAxis
        with ExitStack() as stD:
            gT = sb(stD, "d_gT", [E, S], BF16)
            idxk = sb(stD, "d_idxk", [128, 4, 32], I32)
            wk = sb(stD, "d_wk", [128, 4, 32], F32)
            idxw = sb(stD, "d_idxw", [128, 8, 64], I32)
            idxb = sb(stD, "d_idxb", [128, 64], I32)
            with ExitStack() as st:
                wr = sb(st, "e_wr", [128, 8, E], BF16)
                brt = sb(st, "e_br", [128, E], F32)
                tri = sb(st, "e_tri", [128, 128], BF16)
                iop = sb(st, "e_iop", [128, 1], F32)
                T.dma('pool', wr[:], w_r[l * D:(l + 1) * D, :].rearrange("(kc p) e -> p kc e", p=128), writes=[("wr",)], key="emisc_p")
                T.dma('sp', brt[:], b_r[l, :].partition_broadcast(128), writes=[("brt",)], key="emisc")
                T.dma('sp', tri[:], c_tri[:, :], writes=[("tri",)], key="emisc")
                T.dma('sp', iop[:], c_iop[:, :], writes=[("iop",)], key="emisc")
                xq = sb(st, "e_xq", [128, 8, 1024], BF16)
                lgr = sb(st, "e_lgr", [128, 32, E], F32)
                lge = sb(st, "e_lge", [128, 32, E], F32)
                mk = sb(st, "e_mk", [128, 32, E], F32)
                mkb = sb(st, "e_mkb", [128, 32, E], BF16)
                gates = sb(st, "e_gates", [128, 32, E], F32)
                gbf = sb(st, "e_gbf", [128, 8, E], BF16)
                mx8 = sb(st, "e_mx8", [128, 32, 8], F32)
                sm = sb(st, "e_sm", [128, 32, 4], F32)
                pos = sb(st, "e_pos", [128, 32, E], F32)
                posf = sb(st, "e_posf", [128, 32, E], F32)
                oh = sb(st, "e_oh", [128, 32, E], F32)
                tmp = sb(st, "e_tmp", [128, 32, E], F32)
                tmp2 = sb(st, "e_tmp2", [128, 32, E], F32)
                cnt = sb(st, "e_cnt", [128, E], F32)
                scA = sb(st, "e_scA", [128, E], F32)
                scB = sb(st, "e_scB", [128, E], F32)
                ntl = sb(st, "e_ntl", [128, E], F32)
                off = sb(st, "e_off", [128, E], F32)
                cmp = sb(st, "e_cmp", [128, 64, E], F32)
                eidf = sb(st, "e_eidf", [128, 64], F32)
                ef2 = sb(st, "e_ef2", [128, 64], F32)
                pkf = sb(st, "e_pkf", [128, 4, 32], F32)
                xbs = [sb(st, "e_xb%d" % i, [128, D], BF16) for i in range(2)]
                xb_p = Rot("e_xb", xbs)
                for qi in range(4):
                    t0 = qi * 1024
                    T.dma('sp', xq[:], x1T_d[:, t0:t0 + 1024].rearrange("(kc p) t -> p kc t", p=128), writes=[("xq",)], key="xq")
                    for tt in range(8):
                        gtt = qi * 8 + tt
                        pl, plr = Spool.next()
                        for kc in range(8):
                            T.op('pe', lambda e, kc=kc, pl=pl: e.matmul(pl[:, 0:E], xq[:, kc, tt * 128:(tt + 1) * 128], wr[:, kc, :],
                                                                     start=(kc == 0), stop=(kc == 7)),
                                 reads=[("xq",), ("wr",)], writes=[plr])
                        R = lambda n: ("rt", n, gtt)
                        T.op('dve', lambda e, pl=pl: e.tensor_tensor(lgr[:, gtt, :], pl[:, 0:E], brt[:], ALU.add),
                             reads=[plr, ("brt",)], writes=[R("lg")])
                        T.op('dve', lambda e: e.max(mx8[:, gtt, :], lgr[:, gtt, :]), reads=[R("lg")], writes=[R("mx")])
                        T.op('dve', lambda e: e.tensor_scalar(mk[:, gtt, :], lgr[:, gtt, :], mx8[:, gtt, 3:4], None, ALU.is_ge),
                             reads=[R("lg"), R("mx")], writes=[R("mk")])
                        T.op('dve', lambda e: e.tensor_scalar(sm[:, gtt, 0:1], mx8[:, gtt, 0:1], -1.0, None, ALU.mult),
                             reads=[R("mx")], writes=[R("nm")])
                        T.op('act', lambda e: e.activation(out=lge[:, gtt, :], in_=lgr[:, gtt, :], func=AF.Exp, bias=sm[:, gtt, 0:1]),
                             reads=[R("lg"), R("nm")], writes=[R("le")])
                        T.op('act', lambda e: e.activation(out=mkb[:, gtt, :], in_=mk[:, gtt, :], func=AF.Copy),
                             reads=[R("mk")], writes=[("mkb", gtt)])
                        T.op('dve', lambda e: e.tensor_tensor(lge[:, gtt, :], mk[:, gtt, :], lge[:, gtt, :], ALU.mult),
                             reads=[R("le"), R("mk")], writes=[R("le")])
                        T.op('dve', lambda e: e.reduce_sum(sm[:, gtt, 1:2], lge[:, gtt, :], AX.X), reads=[R("le")], writes=[R("ss")])
                        T.op('dve', lambda e: e.reciprocal(sm[:, gtt, 2:3], sm[:, gtt, 1:2]), reads=[R("ss")], writes=[R("rs")])
                        T.op('dve', lambda e: e.tensor_scalar(gates[:, gtt, :], lge[:, gtt, :], sm[:, gtt, 2:3], None, ALU.mult),
                             reads=[R("le"), R("rs")], writes=[("gates", gtt)])
                        T.op('act', lambda e: e.activation(out=gbf[:, tt, :], in_=gates[:, gtt, :], func=AF.Copy),
                             reads=[("gates", gtt)], writes=[("gbf", tt)])
                        T.op('pe', lambda e: e.transpose(psT[0:E, tt * 128:(tt + 1) * 128], gbf[:, tt, :], ident[:]),
                             reads=[("gbf", tt), ("ident",)], writes=[("psT", tt)])
                    T.op('act', lambda e: e.activation(out=gT[:, t0:t0 + 1024], in_=psT[0:E, :], func=AF.Copy),
                         reads=[("psT", tt) for tt in range(8)], writes=[("gT", qi)])
                for i in range(32):
                    bank = psS[i // 16]
                    c = (i % 16) * E
                    for j in range(i + 1):
                        lhs, lres = (onesb, ("onesb",)) if j < i else (tri, ("tri",))
                        T.op('pe', lambda e, bank=bank, c=c, lhs=lhs, j=j, i=i: e.matmul(
                            bank[:, c:c + E], lhs[:, :], mkb[:, j, :], start=(j == 0), stop=(j == i)),
                            reads=[("mkb", j), lres], writes=[("psS", i // 16)])
                for j in range(32):
                    T.op('pe', lambda e, j=j: e.matmul(psS[2][:, 0:E], onesb[:, :], mkb[:, j, :], start=(j == 0), stop=(j == 31)),
                         reads=[("mkb", j), ("onesb",)], writes=[("psS", 2)])
                pos2 = pos[:].rearrange("p a e -> p (a e)")
                T.op('act', lambda e: e.activation(out=pos2[:, 0:16 * E], in_=psS[0][:, 0:16 * E], func=AF.Copy),
                     reads=[("psS", 0)], writes=[("pos", 0)])
                T.op('dve', lambda e: e.tensor_copy(pos2[:, 16 * E:32 * E], psS[1][:, 0:16 * E]), reads=[("psS", 1)], writes=[("pos", 1)])
                T.op('dve', lambda e: e.tensor_copy(cnt[:], psS[2][:, 0:E]), reads=[("psS", 2)], writes=[("cnt",)])
                RS = [("rsx",)]
                T.op('dve', lambda e: e.tensor_scalar(ntl[:], cnt[:], 0.0, None, ALU.is_gt), reads=[("cnt",)], writes=RS)
                for m_ in range(1, 8):
                    T.op('dve', lambda e, m_=m_: e.scalar_tensor_tensor(ntl[:], cnt[:], float(TS * m_), ntl[:], ALU.is_gt, ALU.add),
                         reads=RS, writes=RS)
                T.op('dve', lambda e: e.tensor_copy(scA[:], ntl[:]), reads=RS, writes=RS)
                bufs = [scA, scB]
                for s_, d_ in enumerate((1, 2, 4, 8, 16)):
                    src_, dst_ = bufs[s_ % 2], bufs[(s_ + 1) % 2]
                    T.op('dve', lambda e, src_=src_, dst_=dst_, d_=d_: e.tensor_copy(dst_[:, 0:d_], src_[:, 0:d_]), reads=RS, writes=RS)
                    T.op('dve', lambda e, src_=src_, dst_=dst_, d_=d_: e.tensor_tensor(dst_[:, d_:E], src_[:, d_:E], src_[:, 0:E - d_], ALU.add),
                         reads=RS, writes=RS)
                cend = scB
                T.op('dve', lambda e: e.tensor_tensor(off[:], cend[:], ntl[:], ALU.subtract), reads=RS, writes=RS)
                T.op('dve', lambda e: e.tensor_scalar(off[:], off[:], float(TS), None, ALU.mult), reads=RS, writes=RS)
                for j in range(NT):
                    T.op('dve', lambda e, j=j: e.tensor_scalar(cmp[:, j, :], cend[:], float(j), None, ALU.is_le), reads=RS, writes=RS)
                T.op('dve', lambda e: e.reduce_sum(eidf[:, 0:NT], cmp[:, 0:NT, :], AX.X), reads=RS, writes=RS)
                T.op('dve', lambda e: e.tensor_scalar(eidf[:, 0:NT], eidf[:, 0:NT], float(E - 1), None, ALU.min), reads=RS, writes=RS)
                T.op('dve', lambda e: e.tensor_scalar(ef2[:, 0:NT], eidf[:, 0:NT], float(D), None, ALU.mult), reads=RS, writes=RS)
                T.op('dve', lambda e: e.tensor_scalar(ef2[:, 0:NT], ef2[:, 0:NT], iop[:, 0:1], None, ALU.add), reads=RS + [("iop",)], writes=RS)
                for kc in range(8):
                    T.op('dve', lambda e, kc=kc: e.tensor_scalar(idxw[:, kc, 0:NT], ef2[:, 0:NT], float(l * E * D + kc * 128), None, ALU.add),
                         reads=RS, writes=[("idxw",)])
                T.op('dve', lambda e: e.tensor_scalar(ef2[:, 0:NT], eidf[:, 0:NT], 128.0, None, ALU.mult), reads=RS + [("idxw",)], writes=RS)
                T.op('dve', lambda e: e.tensor_scalar(ef2[:, 0:NT], ef2[:, 0:NT], iop[:, 0:1], None, ALU.add), reads=RS, writes=RS)
                T.op('dve', lambda e: e.tensor_scalar(idxb[:, 0:NT], ef2[:, 0:NT], float(l * E * 128), None, ALU.add), reads=RS, writes=[("idxb",)])
                T.op('dve', lambda e: e.tensor_tensor(posf[:], pos[:], off[:].unsqueeze(1).to_broadcast([128, 32, E]), ALU.add),
                     reads=RS + [("pos", 0), ("pos", 1)], writes=[("posf",)])
                all_lg = [("rt", "lg", g_) for g_ in range(32)] + [("rt", "mx", g_) for g_ in range(32)]
                all_gates = [("gates", g_) for g_ in range(32)]
                for k in range(4):
                    T.op('dve', lambda e, k=k: e.tensor_tensor(oh[:], lgr[:], mx8[:, :, k:k + 1].to_broadcast([128, 32, E]), ALU.is_equal),
                         reads=all_lg, writes=[("oh",)])
                    T.op('dve', lambda e: e.tensor_tensor(tmp[:], oh[:], posf[:], ALU.mult), reads=[("oh",), ("posf",)], writes=[("tmp",)])
                    T.op('dve', lambda e, k=k: e.reduce_sum(pkf[:, k, :], tmp[:], AX.X), reads=[("tmp",)], writes=[("pkf", k)])
                    T.op('pool', lambda e: e.tensor_tensor(tmp2[:], oh[:], gates[:], ALU.mult), reads=[("oh",)] + all_gates, writes=[("tmp2",)])
                    T.op('dve', lambda e, k=k: e.reduce_sum(wk[:, k, :], tmp2[:], AX.X), reads=[("tmp2",)], writes=[("wk", k)])
                T.op('dve', lambda e: e.tensor_copy(idxk[:], pkf[:]), reads=[("pkf", k) for k in range(4)], writes=[("idxk",)])
                for gtt in range(32):
                    xb, xbr = xb_p.next()
                    T.dma('sp', xb[:], x1b_d[gtt * 128:(gtt + 1) * 128, :], writes=[xbr], key=("e_xb", xbr))
                    for k in range(4):
                        T.idma(out=XS_d[:, :], out_off=IND(ap=idxk[:, k, gtt:gtt + 1], axis=0), in_=xb[:, :], bound=R_XS - 1,
                               reads=[xbr, ("idxk",)], key="scat")
                T.barrier()

            with ExitStack() as st:
                wgs = [sb(st, "x_wg%d" % i, [128, 8, D], BF16) for i in range(2)]
                wus = [sb(st, "x_wu%d" % i, [128, 8, D], BF16) for i in range(2)]
                wds = [sb(st, "x_wd%d" % i, [128, 8, D], BF16) for i in range(2)]
                bgs = [sb(st, "x_bg%d" % i, [128, 8], F32) for i in range(2)]
                bus = [sb(st, "x_bu%d" % i, [128, 8], F32) for i in range(2)]
                xsrs = [sb(st, "x_xsr%d" % i, [128, 4, D], BF16) for i in range(2)]
                xsTs = [sb(st, "x_xsT%d" % i, [128, 8, TS], BF16) for i in range(2)]
                hhs = [sb(st, "x_hh%d" % i, [128, 8, TS], BF16) for i in range(2)]
                hgs = [sb(st, "x_hg%d" % i, [128, 512], F32) for i in range(2)]
                sgs = [sb(st, "x_sg%d" % i, [128, 512], F32) for i in range(2)]
                hus = [sb(st, "x_hu%d" % i, [128, 512], F32) for i in range(2)]
                yrs = [sb(st, "x_yr%d" % i, [128, D], BF16) for i in range(3)]
                wg_p, wu_p, wd_p, bg_p, bu_p = Rot("x_wg", wgs), Rot("x_wu", wus), Rot("x_wd", wds), Rot("x_bg", bgs), Rot("x_bu", bus)
                xsr_p, xsT_p, hh_p, hg_p, sg_p, hu_p, yr_p = (Rot("x_xsr", xsrs), Rot("x_xsT", xsTs), Rot("x_hh", hhs), Rot("x_hg", hgs),
                                                              Rot("x_sg", sgs), Rot("x_hu", hus), Rot("x_yr", yrs))

                def prep(j):
                    P = {}
                    for nm, pool_, src_ in (("wg", wg_p, w_eg), ("wu", wu_p, w_eu), ("wd", wd_p, w_ed)):
                        wt, wres = pool_.next()
                        for kc in range(8):
                            T.idma(out=wt[:, kc, :], in_=src_[:, :], in_off=IND(ap=idxw[:, kc, j:j + 1], axis=0), bound=depth * E * D - 1,
                                   reads=[("idxw",)], writes=[wres + (kc // 4,)], key=wres)
                        P[nm] = (wt, wres)
                    for nm, pool_, src_ in (("bg", bg_p, begP), ("bu", bu_p, beuP)):
                        bt, bres = pool_.next()
                        T.idma(out=bt[:, :], in_=src_[:, :], in_off=IND(ap=idxb[:, j:j + 1], axis=0), bound=depth * E * 128 - 1,
                               reads=[("idxb",)], writes=[bres], key="ebias")
                        P[nm] = (bt, bres)
                    xsr, xsrr = xsr_p.next()
                    T.dma('sp', xsr[:], XS_d[j * TS:(j + 1) * TS, :].rearrange("(a p) d -> p a d", p=128), writes=[xsrr], key=xsrr)
                    P["xsr"] = (xsr, xsrr)
                    return P

                nxt = prep(0)
                for j in range(NT):
                    P = nxt
                    xsr, xsrr = P["xsr"]
                    wgt, wut, wdt = P["wg"], P["wu"], P["wd"]
                    bg, bgr = P["bg"]
                    bu, bur = P["bu"]
                    xsT, xsTr = xsT_p.next()
                    for a in range(4):
                        for fc in range(8):
                            T.op('pe', lambda e, a=a, fc=fc: e.transpose(psT[:, fc * 128:(fc + 1) * 128], xsr[:, a, fc * 128:(fc + 1) * 128], ident[:]),
                                 reads=[xsrr, ("ident",)], writes=[("psT", fc)])
                        T.op('act', lambda e, a=a: e.activation(out=xsT[:, :, a * 128:(a + 1) * 128],
                                                                in_=psT[:].rearrange("p (fc t) -> p fc t", t=128), func=AF.Copy),
                             reads=[("psT", fc) for fc in range(8)], writes=[xsTr + (a,)])
                    if j + 1 < NT:
                        nxt = prep(j + 1)
                    xsT_res = [xsTr + (a,) for a in range(4)]
                    hh, hhr = hh_p.next()
                    for fc in range(8):
                        pg, pgr = Spool.next()
                        pu, pur = Spool.next()
                        for (pt, ptr, wt) in ((pg, pgr, wgt), (pu, pur, wut)):
                            for kc in range(8):
                                T.op('pe', lambda e, kc=kc, pt=pt, wt=wt, fc=fc: e.matmul(
                                    pt[:, :], wt[0][:, kc, fc * 128:(fc + 1) * 128], xsT[:, kc, :],
                                    start=(kc == 0), stop=(kc == 7)), reads=[wt[1] + (kc // 4,)] + xsT_res, writes=[ptr])
                        hg, hgr = hg_p.next()
                        sg, sgr = sg_p.next()
                        hu, hur = hu_p.next()
                        T.op('dve', lambda e, pg=pg, hg=hg, fc=fc: e.tensor_scalar(hg[:], pg[:, :], bg[:, fc:fc + 1], SW_LIM, ALU.add, ALU.min),
                             reads=[pgr, bgr], writes=[hgr])
                        T.op('act', lambda e, hg=hg, sg=sg: e.activation(out=sg[:], in_=hg[:], func=AF.Sigmoid, scale=SW_ALPHA),
                             reads=[hgr], writes=[sgr])
                        T.op('dve', lambda e, pu=pu, hu=hu, fc=fc: e.tensor_scalar(hu[:], pu[:, :], bu[:, fc:fc + 1], SW_LIM, ALU.add, ALU.min),
                             reads=[pur, bur], writes=[hur])
                        T.op('pool', lambda e, hu=hu: e.tensor_scalar(hu[:], hu[:], -SW_LIM, 1.0, ALU.max, ALU.add),
                             reads=[hur], writes=[hur])
                        T.op('pool', lambda e, hg=hg, sg=sg: e.tensor_tensor(hg[:], hg[:], sg[:], ALU.mult),
                             reads=[hgr, sgr], writes=[hgr])
                        T.op('pool', lambda e, hg=hg, hu=hu, fc=fc: e.tensor_tensor(hh[:, fc, :], hg[:], hu[:], ALU.mult),
                             reads=[hgr, hur], writes=[hhr + (fc,)])
                    for a in range(4):
                        yr, yrr = yr_p.next()
                        for half in range(2):
                            py, pyr = Apool.next()
                            for fc in range(8):
                                T.op('pe', lambda e, fc=fc, py=py, half=half, a=a: e.matmul(
                                    py[:, :], hh[:, fc, a * 128:(a + 1) * 128], wdt[0][:, fc, half * 512:(half + 1) * 512],
                                    start=(fc == 0), stop=(fc == 7)),
                                    reads=[hhr + (fc,), wdt[1] + (fc // 4,)], writes=[pyr])
                            if half == 0:
                                T.op('act', lambda e, py=py, yr=yr: e.activation(out=yr[:, 0:512], in_=py[:, :], func=AF.Copy),
                                     reads=[pyr], writes=[yrr + (0,)])
                            else:
                                T.op('dve', lambda e, py=py, yr=yr: e.tensor_copy(yr[:, 512:1024], py[:, :]), reads=[pyr], writes=[yrr + (1,)])
                        T.dma('sp', YS_d[j * TS + a * 128:j * TS + (a + 1) * 128, :], yr[:], reads=[yrr + (0,), yrr + (1,)], key="st_ys")
                T.barrier()

            with ExitStack() as st:
                bdn = sb(st, "f_bdn", [E, D], BF16)
                g_t = sb(st, "f_lng", [128, D], F32)
                b_t = sb(st, "f_lnb", [128, D], F32)
                T.dma('pool', bdn[:], b_ed[l * E:(l + 1) * E, :], writes=[("bdn",)], key="emisc_p")
                T.dma('sp', g_t[:], ln2g[l, :].partition_broadcast(128), writes=[("lng",)], key="emisc")
                T.dma('sp', b_t[:], ln2b[l, :].partition_broadcast(128), writes=[("lnb",)], key="emisc")
                ygs = [sb(st, "f_yg%d" % i, [128, 4, D], BF16) for i in range(2)]
                xrs = [sb(st, "f_xr%d" % i, [128, D], F32) for i in range(2)]
                zs = [sb(st, "f_z%d" % i, [128, D], F32) for i in range(2)]
                lnpools = (Rot("f_st", [sb(st, "f_st%d" % i, [128, 12], F32) for i in range(2)]),
                           Rot("f_mv", [sb(st, "f_mv%d" % i, [128, 4], F32) for i in range(2)]),
                           Rot("f_xo", [sb(st, "f_xo%d" % i, [128, D], F32) for i in range(2)]),
                           Rot("f_xb", [sb(st, "f_xb%d" % i, [128, D], BF16) for i in range(2)]),
                           Rot("f_xT", [sb(st, "f_xT%d" % i, [128, D], BF16) for i in range(2)]))
                yg_p, xr_p, z_p = Rot("f_yg", ygs), Rot("f_xr", xrs), Rot("f_z", zs)
                for gtt in range(32):
                    row0 = gtt * 128
                    yg, ygr = yg_p.next()
                    for k in range(4):
                        T.idma(out=yg[:, k, :], in_=YS_d[:, :], in_off=IND(ap=idxk[:, k, gtt:gtt + 1], axis=0), bound=R_XS - 1,
                               reads=[("idxk",)], writes=[ygr + (k,)], key=ygr)
                    xr, xrr = xr_p.next()
                    T.dma('sp', xr[:], x1_d[row0:row0 + 128, :], writes=[xrr], key=xrr)
                    z, zr = z_p.next()
                    for half in range(2):
                        py, pyr = Apool.next()
                        T.op('pe', lambda e, py=py, half=half: e.matmul(py[:, :], gT[:, row0:row0 + 128],
                                                                      bdn[:, half * 512:(half + 1) * 512], start=True, stop=True),
                             reads=[("gT", gtt // 8), ("bdn",)], writes=[pyr])
                        T.op('dve', lambda e, py=py, half=half: e.scalar_tensor_tensor(
                            z[:, half * 512:(half + 1) * 512], xr[:, half * 512:(half + 1) * 512], alpha, py[:, :], ALU.mult, ALU.add),
                            reads=[pyr, xrr], writes=[zr + (half,)])
                    zres = [zr + (0,), zr + (1,)]
                    for k in range(4):
                        T.op('dve', lambda e, k=k: e.scalar_tensor_tensor(z[:], yg[:, k, :], wk[:, k, gtt:gtt + 1], z[:], ALU.mult, ALU.add),
                             reads=[ygr + (k,), ("wk", k)] + zres, writes=zres)
                    ln_epilogue(st, z, zres, g_t, b_t, row0, dst_f32, dstT, lnpools)
                T.barrier()
        x_cur = x2_d

    T.barrier()
    es.close()
    return nc


def _consts():
    bf = ml_dtypes.bfloat16
    c = {}
    c["c_ident"] = np.eye(128, dtype=np.float32).astype(bf)
    c["c_tri"] = np.triu(np.ones((128, 128), np.float32), k=1).astype(bf)
    c["c_iop"] = np.arange(128, dtype=np.float32).reshape(128, 1)
    t = np.arange(S, dtype=np.float32)

    def tab(dim, rows):
        inv = (1.0 / (np.float32(10000.0) ** (np.arange(0, dim, 2, dtype=np.float32) / np.float32(dim)))).astype(np.float32)
        ang = (t[:, None] * inv[None, :]).astype(np.float32)
        cs, sn = np.cos(ang).astype(np.float32), np.sin(ang).astype(np.float32)
        idx = (np.arange(rows) % dim) % (dim // 2)
        return np.ascontiguousarray(cs[:, idx].T), np.ascontiguousarray(sn[:, idx].T)

    c["c_cosB"], c["c_sinB"] = tab(64, 128)
    c["c_cosC"], c["c_sinC"] = tab(48, 96)
    k = np.arange(128)
    q = np.arange(512)
    mA = np.zeros((3, 8, 128, 512), np.float32)
    for var, qb in ((0, 0), (1, 3), (2, 7)):
        r0 = 8 * qb
        r = r0 + q // 64
        cc = q % 64
        rs = np.clip(r - 4, 0, 56)
        cs_ = np.clip(cc - 8, 0, 48)
        for j in range(8):
            kr = r0 - 4 + 2 * j + k // 64
            kc = k % 64
            ok = ((kr[:, None] >= rs[None, :]) & (kr[:, None] < rs[None, :] + 8) &
                  (kc[:, None] >= cs_[None, :]) & (kc[:, None] < cs_[None, :] + 16))
            mA[var, j] = np.where(ok, 0.0, NEG)
    c["c_maskA"] = np.ascontiguousarray(mA.reshape(24, 128, 512).transpose(1, 0, 2)).reshape(128, 24 * 512).astype(bf)
    mB = np.zeros((34, 128, 512), np.float32)
    for g, d in enumerate((1, 4, 16)):
        for jb in range(B_JB[g][0], B_JB[g][1] + 1):
            kk = jb * 128 + k
            diff = kk[:, None] - q[None, :]
            ok = (np.abs(diff) <= 64 * d) & (diff % d == 0)
            mB[B_OFF[g] + jb - B_JB[g][0]] = np.where(ok, 0.0, NEG)
    c["c_maskB"] = np.ascontiguousarray(mB.transpose(1, 0, 2)).reshape(128, 34 * 512).astype(bf)
    return c


def _bias_gather(na_rpb):
    L, H = na_rpb.shape[0], na_rpb.shape[1]
    k = np.arange(128)
    q = np.arange(512)
    j = np.arange(8)
    dr = (-4 + 2 * j[None, :, None] + k[:, None, None] // 64) - (q[None, None, :] // 64)
    dc = (k[:, None, None] % 64) - (q[None, None, :] % 64) + 0 * j[None, :, None]
    ri = np.clip(dr + 7, 0, 14)
    ci = np.clip(dc + 15, 0, 30)
    out = na_rpb[:, :, ri, ci]
    return np.ascontiguousarray(out).reshape(L * H * 128, 8 * 512)


def prepare_inputs(inp, depth, n_exp):
    f = lambda a: np.ascontiguousarray(np.asarray(a, dtype=np.float32))
    E = n_exp
    m = {}
    m["w_in"] = f(inp["w_in"]).reshape(depth * D, 6144)
    m["bgT"] = np.ascontiguousarray(f(inp["b_gates"]).reshape(depth, 24, 128).transpose(0, 2, 1)).reshape(depth * 128, 24)
    m["w_proj_a"] = f(inp["w_proj_a"]).reshape(depth * 256, D)
    m["w_proj_b"] = f(inp["w_proj_b"]).reshape(depth * 128, D)
    m["w_proj_c"] = f(inp["w_proj_c"]).reshape(depth * 384, D)
    m["w_out"] = f(inp["w_out"]).reshape(depth * D, D)
    m["biasA"] = _bias_gather(f(inp["na_rpb"]))
    m["diff_lambda"] = f(inp["diff_lambda"]).reshape(depth, 192)
    m["diff_norm_g"] = f(inp["diff_norm_g"]).reshape(depth * 96, 1)
    for k_ in ("ln1_g", "ln1_b", "ln2_g", "ln2_b"):
        m[k_] = f(inp[k_]).reshape(depth, D)
    m["w_router"] = f(inp["w_router"]).reshape(depth * D, E)
    m["b_router"] = f(inp["b_router"]).reshape(depth, E)
    m["w_exp_gate"] = f(inp["w_exp_gate"]).reshape(depth * E * D, D)
    m["w_exp_up"] = f(inp["w_exp_up"]).reshape(depth * E * D, D)
    m["w_exp_down"] = f(inp["w_exp_down"]).reshape(depth * E * D, D)
    m["begP"] = np.ascontiguousarray(f(inp["b_exp_gate"]).reshape(depth, E, 8, 128).transpose(0, 1, 3, 2)).reshape(depth * E * 128, 8)
    m["beuP"] = np.ascontiguousarray(f(inp["b_exp_up"]).reshape(depth, E, 8, 128).transpose(0, 1, 3, 2)).reshape(depth * E * 128, 8)
    m["b_exp_down"] = f(inp["b_exp_down"]).reshape(depth * E, D)
    m.update(_consts())
    return m


def kernel(**inputs):
    depth, n_exp, n_cores = 4, 32, 8
    x = np.asarray(inputs["x"], dtype=np.float32)
    shared = prepare_inputs(inputs, depth, n_exp)
    nc = build_program(depth, n_exp)
    in_maps = []
    for c in range(n_cores):
        m = dict(shared)
        m["x"] = np.ascontiguousarray(x[c])
        in_maps.append(m)
    res = run_bass_kernel_spmd(nc, in_maps, core_ids=list(range(n_cores)))
    return np.stack([np.asarray(res.results[c]["y"], dtype=np.float32) for c in range(n_cores)], axis=0)
```

```python
import math
from contextlib import ExitStack
import numpy as np
import ml_dtypes
import concourse.bass as bass
import concourse.mybir as mybir
from concourse.bass_utils import run_bass_kernel_spmd

F32 = mybir.dt.float32
BF16 = mybir.dt.bfloat16
I32 = mybir.dt.int32
AF = mybir.ActivationFunctionType
ALU = mybir.AluOpType
AX = mybir.AxisListType

S = 4096
D = 1024
NTB = 8
LN_EPS = 1e-5
NEG = -30000.0
SW_ALPHA = 1.702
SW_LIM = 7.0
B_JB = [(-1, 4), (-2, 5), (-8, 11)]
B_OFF = [0, 6, 14]
TS = 512
NT = 63
R_XS = NT * TS


class Trk:
    def __init__(s, nc, es):
        s.nc = nc
        s.es = es
        s.eng = {'pe': nc.tensor, 'act': nc.scalar, 'dve': nc.vector, 'pool': nc.gpsimd, 'sp': nc.sync}
        s.esem = {k: es.enter_context(nc.semaphore('e_' + k)) for k in ('pe', 'act', 'dve', 'pool')}
        s.ecnt = {k: 0 for k in s.esem}
        s.dsem = {}
        s.dcnt = {}
        s.waited = {k: {} for k in s.eng}
        s.lastw = {}
        s.rd = {}

    def _wait(s, e, tok):
        kind, key, val = tok
        if kind == 'e':
            if key == 'pe' and e == 'pe':
                return
            if val <= 0 or s.waited[e].get(('e', key), 0) >= val:
                return
            s.eng[e].wait_ge(s.esem[key], val)
            s.waited[e][('e', key)] = val
        else:
            tgt = s.dcnt[key]
            if tgt <= 0 or s.waited[e].get(('d', key), 0) >= tgt:
                return
            s.eng[e].wait_ge(s.dsem[key], tgt)
            s.waited[e][('d', key)] = tgt

    def _deps(s, e, reads, writes):
        for r in reads:
            t = s.lastw.get(r)
            if t:
                s._wait(e, t)
        for w in writes:
            t = s.lastw.get(w)
            if t:
                s._wait(e, t)
            for t in s.rd.get(w, {}).values():
                s._wait(e, t)

    def _record(s, tok, reads, writes):
        for r in reads:
            s.rd.setdefault(r, {})[(tok[0], tok[1])] = tok
        for w in writes:
            s.lastw[w] = tok
            s.rd[w] = {}

    def op(s, e, fn, reads=(), writes=()):
        s._deps(e, reads, writes)
        ins = fn(s.eng[e])
        s.ecnt[e] += 1
        ins.then_inc(s.esem[e], 1)
        s._record(('e', e, s.ecnt[e]), reads, writes)

    def dma(s, q, out, in_, reads=(), writes=(), key=None):
        s._deps(q, reads, writes)
        if key not in s.dsem:
            s.dsem[key] = s.es.enter_context(s.nc.semaphore('d_%d' % len(s.dsem)))
            s.dcnt[key] = 0
        s.eng[q].dma_start(out=out, in_=in_).then_inc(s.dsem[key], 16)
        s.dcnt[key] += 16
        s._record(('d', key, s.dcnt[key]), reads, writes)

    def idma(s, out, in_, out_off=None, in_off=None, bound=None, reads=(), writes=(), key=None):
        s._deps('pool', reads, writes)
        if key not in s.dsem:
            s.dsem[key] = s.es.enter_context(s.nc.semaphore('d_%d' % len(s.dsem)))
            s.dcnt[key] = 0
        s.nc.gpsimd.indirect_dma_start(out=out, out_offset=out_off, in_=in_, in_offset=in_off).then_inc(s.dsem[key], 16)
        s.dcnt[key] += 16
        s._record(('d', key, s.dcnt[key]), reads, writes)

    def barrier(s):
        for e in s.eng:
            for k in s.esem:
                s._wait(e, ('e', k, s.ecnt[k]))
            for k in s.dsem:
                s._wait(e, ('d', k, 0))
        s.lastw = {}
        s.rd = {}


class Rot:
    def __init__(s, name, views):
        s.name = name
        s.views = views
        s.i = -1

    def next(s):
        s.i = (s.i + 1) % len(s.views)
        return s.views[s.i], (s.name, s.i)


def build_program(depth, n_exp, debug=False):
    nc = bass.Bass("TRN2", target_bir_lowering=False)
    es = ExitStack()
    T = Trk(nc, es)
    E = n_exp

    def din(name, shape, dt=F32):
        return nc.dram_tensor(name, list(shape), dt, kind="ExternalInput").ap()

    def dscr(name, shape, dt):
        kind = "ExternalOutput" if debug else "Internal"
        return nc.dram_tensor(name, list(shape), dt, kind=kind).ap()

    x_in = din("x", [S, D])
    w_in = din("w_in", [depth * D, 6144])
    bgT = din("bgT", [depth * 128, 24])
    w_pa = din("w_proj_a", [depth * 256, D])
    w_pb = din("w_proj_b", [depth * 128, D])
    w_pc = din("w_proj_c", [depth * 384, D])
    w_out = din("w_out", [depth * D, D])
    biasA = din("biasA", [depth * 4 * 128, 8 * 512])
    dlam = din("diff_lambda", [depth, 192])
    dng = din("diff_norm_g", [depth * 96, 1])
    ln1g = din("ln1_g", [depth, D])
    ln1b = din("ln1_b", [depth, D])
    ln2g = din("ln2_g", [depth, D])
    ln2b = din("ln2_b", [depth, D])
    w_r = din("w_router", [depth * D, E])
    b_r = din("b_router", [depth, E])
    w_eg = din("w_exp_gate", [depth * E * D, D])
    w_eu = din("w_exp_up", [depth * E * D, D])
    w_ed = din("w_exp_down", [depth * E * D, D])
    begP = din("begP", [depth * E * 128, 8])
    beuP = din("beuP", [depth * E * 128, 8])
    c_tri = din("c_tri", [128, 128], BF16)
    c_iop = din("c_iop", [128, 1])
    b_ed = din("b_exp_down", [depth * E, D])
    c_ident = din("c_ident", [128, 128], BF16)
    c_cosB = din("c_cosB", [128, S])
    c_sinB = din("c_sinB", [128, S])
    c_cosC = din("c_cosC", [96, S])
    c_sinC = din("c_sinC", [96, S])
    c_maskA = din("c_maskA", [128, 24 * 512], BF16)
    c_maskB = din("c_maskB", [128, 34 * 512], BF16)
    y_out = nc.dram_tensor("y", [S, D], F32, kind="ExternalOutput").ap()

    xT_d = dscr("xT_d", [D, S], BF16)
    x1T_d = dscr("x1T_d", [D, S], BF16)
    QT_d = dscr("QT_d", [D, S], BF16)
    KT_d = dscr("KT_d", [D, S], BF16)
    V_d = dscr("V_d", [S, D], BF16)
    YT_d = dscr("YT_d", [768, S], BF16)
    x1_d = dscr("x1_d", [S, D], F32)
    x2_d = dscr("x2_d", [S, D], F32)
    lam_d = dscr("lam_d", [1, 8], F32)
    x1b_d = dscr("x1b_d", [S, D], BF16)
    XS_d = dscr("XS_d", [R_XS, D], BF16)
    YS_d = dscr("YS_d", [R_XS, D], BF16)

    sb_n = [0]

    def sb(stack, name, shape, dt):
        sb_n[0] += 1
        return stack.enter_context(nc.sbuf_tensor("%s_%d" % (name, sb_n[0]), list(shape), dt))

    ident = sb(es, "ident", [128, 128], BF16)
    onesb = sb(es, "onesb", [128, 128], BF16)
    psS = [es.enter_context(nc.psum_tensor("psS%d" % i, [128, 512], F32)) for i in range(3)]
    psA = [es.enter_context(nc.psum_tensor("psA%d" % i, [128, 512], F32)) for i in range(4)]
    psT = es.enter_context(nc.psum_tensor("psT", [128, 1024], BF16))
    Spool = Rot("psS", psS)
    Apool = Rot("psA", psA)

    T.dma('sp', ident[:], c_ident[:, :], writes=[("ident",)], key="const")
    T.op('pool', lambda e: e.memset(onesb[:], 1.0), writes=[("onesb",)])

    def ln_epilogue(st, z, zres, g_t, b_t, row0, dst_f32, dstT, pools, dst_b16=None):
        stt, mv, xo_p, xb_p, xt_p = pools
        st6, st6r = stt.next()
        T.op('dve', lambda e: e.bn_stats(st6[:, 0:6], z[:, 0:512]), reads=zres, writes=[st6r + ("a",)])
        T.op('dve', lambda e: e.bn_stats(st6[:, 6:12], z[:, 512:1024]), reads=zres, writes=[st6r + ("b",)])
        m, mr = mv.next()
        T.op('dve', lambda e: e.bn_aggr(m[:, 0:2], st6[:, 0:12]), reads=[st6r + ("a",), st6r + ("b",)], writes=[mr])
        T.op('act', lambda e: e.activation(out=m[:, 3:4], in_=m[:, 1:2], func=AF.Ln, bias=LN_EPS),
             reads=[mr], writes=[mr + ("l",)])
        T.op('act', lambda e: e.activation(out=m[:, 2:3], in_=m[:, 3:4], func=AF.Exp, scale=-0.5),
             reads=[mr + ("l",)], writes=[mr + ("r",)])
        xo, xor_ = xo_p.next()
        T.op('dve', lambda e: e.tensor_scalar(xo[:], z[:], m[:, 0:1], m[:, 2:3], ALU.subtract, ALU.mult),
             reads=zres + [mr, mr + ("r",)], writes=[xor_])
        T.op('dve', lambda e: e.tensor_tensor(xo[:], xo[:], g_t[:], ALU.mult), reads=[xor_, ("lng",)], writes=[xor_])
        T.op('dve', lambda e: e.tensor_tensor(xo[:], xo[:], b_t[:], ALU.add), reads=[xor_, ("lnb",)], writes=[xor_])
        T.dma('sp', dst_f32[row0:row0 + 128, :], xo[:], reads=[xor_], key="st_x")
        if dstT is not None:
            xb, xbr = xb_p.next()
            T.op('act', lambda e: e.activation(out=xb[:], in_=xo[:], func=AF.Copy), reads=[xor_], writes=[xbr])
            if dst_b16 is not None:
                T.dma('sp', dst_b16[row0:row0 + 128, :], xb[:], reads=[xbr], key="st_xb")
            transpose_store(xb, xbr, row0, dstT, xt_p)

    def transpose_store(xb, xbr, row0, dstT, xt_p):
        for fc in range(8):
            T.op('pe', lambda e, fc=fc: e.transpose(psT[:, fc * 128:(fc + 1) * 128], xb[:, fc * 128:(fc + 1) * 128], ident[:]),
                 reads=[xbr, ("ident",)], writes=[("psT", fc)])
        xt, xtr = xt_p.next()
        T.op('act', lambda e: e.activation(out=xt[:], in_=psT[:], func=AF.Copy),
             reads=[("psT", fc) for fc in range(8)], writes=[xtr])
        T.dma('sp', dstT[:, row0:row0 + 128].rearrange("(fc p) t -> p fc t", p=128),
              xt[:].rearrange("p (fc t) -> p fc t", t=128), reads=[xtr], key="st_xT")

    with ExitStack() as st:
        xl = [sb(st, "p_xl%d" % i, [128, D], F32) for i in range(2)]
        xbs = [sb(st, "p_xb%d" % i, [128, D], BF16) for i in range(2)]
        xts = [sb(st, "p_xt%d" % i, [128, D], BF16) for i in range(2)]
        xl_p, xb_p, xt_p = Rot("p_xl", xl), Rot("p_xb", xbs), Rot("p_xt", xts)
        zt = sb(st, "p_zero", [128, 4096], BF16)
        T.op('pool', lambda e: e.memset(zt[:], 0.0), writes=[("zt",)])
        XSv = XS_d.rearrange("(p a) d -> p (a d)", p=128)
        for c in range(R_XS * D // 128 // 4096):
            T.dma('sp', XSv[:, c * 4096:(c + 1) * 4096], zt[:], reads=[("zt",)], key="zero")
        for tt in range(32):
            xt_, xr = xl_p.next()
            T.dma('sp', xt_[:], x_in[tt * 128:(tt + 1) * 128, :], writes=[xr], key=("ldx", xr))
            xb, xbr = xb_p.next()
            T.op('act', lambda e: e.activation(out=xb[:], in_=xt_[:], func=AF.Copy), reads=[xr], writes=[xbr])
            transpose_store(xb, xbr, tt * 128, xT_d, xt_p)
        T.barrier()

    x_cur = x_in
    for l in range(depth):
        lam_init = 0.8 - 0.6 * math.exp(-0.3 * l)
        last = (l == depth - 1)
        with ExitStack() as st:
            wq = sb(st, "a_wq", [128, 8, 3072], BF16)
            wrot = sb(st, "a_wrot", [128, 8, 1536], BF16)
            for kc in range(8):
                T.dma('pool', wq[:, kc, :], w_in[l * D + kc * 128:l * D + (kc + 1) * 128, 0:3072],
                      writes=[("wq", kc)], key=("wq", kc % 2))
            for kc in range(8):
                for (sbase, dbase, nu, w) in ((768, 0, 12, 64), (1920, 768, 16, 48)):
                    h = w // 2
                    src = wq[:, kc, sbase:sbase + nu * w].rearrange("p (u j) -> p u j", j=w)
                    dst = wrot[:, kc, dbase:dbase + nu * w].rearrange("p (u j) -> p u j", j=w)
                    T.op('pool', lambda e, src=src, dst=dst, h=h, w=w: e.tensor_scalar(
                        dst[:, :, 0:h], src[:, :, h:w], -1.0, None, ALU.mult),
                        reads=[("wq", kc)], writes=[("wrot", kc, dbase, 0)])
                    T.op('pool', lambda e, src=src, dst=dst, h=h, w=w: e.tensor_copy(dst[:, :, h:w], src[:, :, 0:h]),
                         reads=[("wq", kc)], writes=[("wrot", kc, dbase, 1)])
            wq_res = [("wq", kc) for kc in range(8)]
            wrot_res = [("wrot", kc, db, hh) for kc in range(8) for db in (0, 768) for hh in (0, 1)]
            xts = [sb(st, "a_xt%d" % i, [128, 8, 512], BF16) for i in range(2)]
            tabs = [sb(st, "a_tab%d" % i, [128, 4, 512], F32) for i in range(2)]
            t1s = [sb(st, "a_t1%d" % i, [128, 512], F32) for i in range(2)]
            t2s = [sb(st, "a_t2%d" % i, [128, 512], F32) for i in range(2)]
            obs = [sb(st, "a_ob%d" % i, [128, 512], BF16) for i in range(3)]
            vts = [sb(st, "a_vt%d" % i, [128, 1024], BF16) for i in range(2)]
            xt_p, tab_p, t1_p, t2_p, ob_p, vt_p = (Rot("a_xt", xts), Rot("a_tab", tabs), Rot("a_t1", t1s),
                                                   Rot("a_t2", t2s), Rot("a_ob", obs), Rot("a_vt", vts))
            chunks = []
            for i in range(2):
                chunks.append((i * 128, 128, QT_d, i * 128, None, 0, 0.125))
                chunks.append((256 + i * 128, 128, KT_d, i * 128, None, 0, 1.0))
            for i in range(3):
                chunks.append((768 + i * 128, 128, QT_d, 256 + i * 128, i * 128, 0, 1.0))
                chunks.append((1152 + i * 128, 128, KT_d, 256 + i * 128, 384 + i * 128, 0, 1.0))
            for i in range(4):
                chunks.append((1920 + i * 96, 96, QT_d, 640 + i * 96, 768 + i * 96, 1, 1.0))
                chunks.append((2304 + i * 96, 96, KT_d, 640 + i * 96, 1152 + i * 96, 1, 1.0))
            for tb in range(NTB):
                c0 = tb * 512
                xt, xtr = xt_p.next()
                T.dma('sp', xt[:], xT_d[:, c0:c0 + 512].rearrange("(kc p) t -> p kc t", p=128), writes=[xtr], key=("a_xt", xtr))
                tab, tabr = tab_p.next()
                T.dma('sp', tab[:, 0, :], c_cosB[:, c0:c0 + 512], writes=[tabr + (0,)], key=("a_tab", tabr))
                T.dma('sp', tab[:, 1, :], c_sinB[:, c0:c0 + 512], writes=[tabr + (1,)], key=("a_tab", tabr))
                T.dma('sp', tab[0:96, 2, :], c_cosC[:, c0:c0 + 512], writes=[tabr + (2,)], key=("a_tab", tabr))
                T.dma('sp', tab[0:96, 3, :], c_sinC[:, c0:c0 + 512], writes=[tabr + (3,)], key=("a_tab", tabr))
                for (col0, M, dst, drow, rc0, ti, scale) in chunks:
                    p1, p1r = Spool.next()
                    for kc in range(8):
                        T.op('pe', lambda e, kc=kc, p1=p1: e.matmul(p1[0:M, :], wq[:, kc, col0:col0 + M], xt[:, kc, :],
                                                                 start=(kc == 0), stop=(kc == 7)),
                             reads=[("wq", kc), xtr], writes=[p1r])
                    ob, obr = ob_p.next()
                    if rc0 is None:
                        T.op('act', lambda e, p1=p1, ob=ob: e.activation(out=ob[0:M, :], in_=p1[0:M, :], func=AF.Copy, scale=scale),
                             reads=[p1r], writes=[obr])
                    else:
                        p2, p2r = Spool.next()
                        for kc in range(8):
                            T.op('pe', lambda e, kc=kc, p2=p2: e.matmul(p2[0:M, :], wrot[:, kc, rc0:rc0 + M], xt[:, kc, :],
                                                                     start=(kc == 0), stop=(kc == 7)),
                                 reads=wrot_res[kc * 4:(kc + 1) * 4] + [xtr], writes=[p2r])
                        t1, t1r = t1_p.next()
                        t2, t2r = t2_p.next()
                        T.op('dve', lambda e, p1=p1, t1=t1: e.tensor_tensor(t1[0:M, :], p1[0:M, :], tab[0:M, 2 * ti, :], ALU.mult),
                             reads=[p1r, tabr + (2 * ti,)], writes=[t1r])
                        T.op('dve', lambda e, p2=p2, t2=t2: e.tensor_tensor(t2[0:M, :], p2[0:M, :], tab[0:M, 2 * ti + 1, :], ALU.mult),
                             reads=[p2r, tabr + (2 * ti + 1,)], writes=[t2r])
                        T.op('dve', lambda e, t1=t1, t2=t2, ob=ob: e.tensor_tensor(ob[0:M, :], t1[0:M, :], t2[0:M, :], ALU.add),
                             reads=[t1r, t2r], writes=[obr])
                    T.dma('sp', dst[drow:drow + M, c0:c0 + 512], ob[0:M, :], reads=[obr], key="st_a")
                for tt in range(4):
                    vt, vtr = vt_p.next()
                    for (vc0, n, dcol) in ((512, 256, 0), (1536, 384, 256), (2688, 384, 640)):
                        pv, pvr = Spool.next()
                        for kc in range(8):
                            T.op('pe', lambda e, kc=kc, pv=pv: e.matmul(pv[:, 0:n], xt[:, kc, tt * 128:(tt + 1) * 128],
                                                                     wq[:, kc, vc0:vc0 + n], start=(kc == 0), stop=(kc == 7)),
                                 reads=[("wq", kc), xtr], writes=[pvr])
                        T.op('act', lambda e, pv=pv, vt=vt: e.activation(out=vt[:, dcol:dcol + n], in_=pv[:, 0:n], func=AF.Copy),
                             reads=[pvr], writes=[vtr + (dcol,)])
                    T.dma('sp', V_d[c0 + tt * 128:c0 + (tt + 1) * 128, :], vt[:],
                          reads=[vtr + (0,), vtr + (256,), vtr + (640,)], key="st_v")
            T.barrier()

        with ExitStack() as st:
            kts = [sb(st, "b_kt%d" % i, [64, S], BF16) for i in range(6)]
            vhs = [sb(st, "b_vh%d" % i, [128, 32, 96], BF16) for i in range(6)]
            maskt = sb(st, "b_mask", [128, 34, 512], BF16)
            biast = sb(st, "b_bias", [128, 8, 512], BF16)
            qts = [sb(st, "b_q%d" % i, [64, 512], BF16) for i in range(4)]
            ets = [sb(st, "b_e%d" % i, [128, 512], BF16) for i in range(4)]
            rts = [sb(st, "b_r%d" % i, [96, 512], F32) for i in range(2)]
            fts = [sb(st, "b_f%d" % i, [96, 512], F32) for i in range(4)]
            sqs = [sb(st, "b_sq%d" % i, [96, 512], BF16) for i in range(2)]
            yos = [sb(st, "b_yo%d" % i, [96, 512], BF16) for i in range(2)]
            lamt = sb(st, "b_lam", [1, 200], F32)
            nlam = sb(st, "b_nlam", [96, 1], F32)
            gpr = sb(st, "b_gpr", [96, 1], F32)
            q_p, e_p, r_p, f_p, sq_p, yo_p = (Rot("b_q", qts), Rot("b_e", ets), Rot("b_r", rts), Rot("b_f", fts),
                                              Rot("b_sq", sqs), Rot("b_yo", yos))

            def load_k(slot, row0, d):
                T.dma('sp', kts[slot][0:d, :], KT_d[row0:row0 + d, :], writes=[("kt", slot)], key=("kt", slot))

            def load_v(slot, col0, e_):
                T.dma('sp', vhs[slot][:, :, 0:e_], V_d[:, col0:col0 + e_].rearrange("(kb p) e -> p kb e", p=128),
                      writes=[("vh", slot)], key=("vh", slot))

            def load_q(row0, d, qb):
                q, qr = q_p.next()
                T.dma('sp', q[0:d, :], QT_d[row0:row0 + d, qb * 512:(qb + 1) * 512], writes=[qr], key=("q", qr))
                return q, qr

            def attn_tiles(items, e_, accs, scale):
                num, numr, den, denr = accs
                n = len(items)
                for i, (ks, d, q, qr, kb, biases, vs) in enumerate(items):
                    sp_, spr = Spool.next()
                    T.op('pe', lambda e, sp_=sp_, ks=ks, d=d, q=q, kb=kb: e.matmul(
                        sp_[:, :], kts[ks][0:d, kb * 128:(kb + 1) * 128], q[0:d, :], start=True, stop=(len(biases) == 0)),
                        reads=[("kt", ks), qr], writes=[spr])
                    for bi, (bap, bres) in enumerate(biases):
                        T.op('pe', lambda e, sp_=sp_, bap=bap, bi=bi: e.matmul(sp_[:, :], ident[:], bap, start=False,
                                                                             stop=(bi == len(biases) - 1)),
                             reads=[("ident",), bres], writes=[spr])
                    et, etr = e_p.next()
                    T.op('act', lambda e, sp_=sp_, et=et: e.activation(out=et[:], in_=sp_[:, :], func=AF.Exp, scale=scale),
                         reads=[spr], writes=[etr])
                    T.op('pe', lambda e, et=et, vs=vs, kb=kb, i=i: e.matmul(num[0:e_, :], vhs[vs][:, kb, 0:e_], et[:],
                                                                          start=(i == 0), stop=(i == n - 1)),
                         reads=[("vh", vs), etr], writes=[numr])
                    T.op('pe', lambda e, et=et, i=i: e.matmul(den[0:e_, :], onesb[:, 0:e_], et[:],
                                                            start=(i == 0), stop=(i == n - 1)),
                         reads=[("onesb",), etr], writes=[denr])

            def finalize_simple(accs, e_, yrow, qb):
                num, numr, den, denr = accs
                r, rr = r_p.next()
                T.op('dve', lambda e: e.reciprocal(r[0:e_, :], den[0:e_, :]), reads=[denr], writes=[rr])
                yo, yor = yo_p.next()
                T.op('dve', lambda e: e.tensor_tensor(yo[0:e_, :], num[0:e_, :], r[0:e_, :], ALU.mult),
                     reads=[numr, rr], writes=[yor])
                T.dma('sp', YT_d[yrow:yrow + e_, qb * 512:(qb + 1) * 512], yo[0:e_, :], reads=[yor], key="st_y")

            T.dma('sp', maskt[:, 0:24, :], c_maskA[:, :].rearrange("p (j q) -> p j q", q=512), writes=[("mask",)], key="mask")
            for h in range(4):
                load_k(0, h * 64, 64)
                load_v(0, h * 64, 64)
                r0 = (l * 4 + h) * 128
                T.dma('pool', biast[:], biasA[r0:r0 + 128, :].rearrange("p (j q) -> p j q", q=512), writes=[("bias",)], key="bias")
                for qb in range(8):
                    q, qr = load_q(h * 64, 64, qb)
                    var = 0 if qb == 0 else (2 if qb == 7 else 1)
                    items = []
                    for j in range(8):
                        kb = 4 * qb - 2 + j
                        if 0 <= kb < 32:
                            items.append((0, 64, q, qr, kb, [(biast[:, j, :], ("bias",)), (maskt[:, var * 8 + j, :], ("mask",))], 0))
                    num, numr = Apool.next()
                    den, denr = Apool.next()
                    attn_tiles(items, 64, (num, numr, den, denr), 1.0)
                    finalize_simple((num, numr, den, denr), 64, h * 64, qb)
            T.dma('sp', maskt[:, :, :], c_maskB[:, :].rearrange("p (j q) -> p j q", q=512), writes=[("mask",)], key="mask")
            for u in range(6):
                load_k(u, 256 + u * 64, 64)
                load_v(u, 256 + u * 64, 64)
            for jo in range(2):
                for qb in range(8):
                    items = []
                    for g in range(3):
                        u = 2 * g + jo
                        q, qr = load_q(256 + u * 64, 64, qb)
                        for jb in range(B_JB[g][0], B_JB[g][1] + 1):
                            kb = 4 * qb + jb
                            if 0 <= kb < 32:
                                mi = B_OFF[g] + jb - B_JB[g][0]
                                items.append((u, 64, q, qr, kb, [(maskt[:, mi, :], ("mask",))], u))
                    num, numr = Apool.next()
                    den, denr = Apool.next()
                    attn_tiles(items, 64, (num, numr, den, denr), 0.125)
                    finalize_simple((num, numr, den, denr), 64, 256 + jo * 64, qb)
            T.dma('sp', lamt[:, 0:192], dlam[l:l + 1, :], writes=[("lamt",)], key="lam")
            T.op('dve', lambda e: e.tensor_tensor(lamt[:, 0:48], lamt[:, 0:48], lamt[:, 48:96], ALU.mult),
                 reads=[("lamt",)], writes=[("lamt", 0)])
            T.op('dve', lambda e: e.tensor_tensor(lamt[:, 48:96], lamt[:, 96:144], lamt[:, 144:192], ALU.mult),
                 reads=[("lamt",)], writes=[("lamt", 1)])
            T.op('dve', lambda e: e.reduce_sum(lamt[:, 192:194], lamt[:, 0:96].rearrange("p (a b) -> p a b", b=48), AX.X),
                 reads=[("lamt", 0), ("lamt", 1)], writes=[("lamt", 2)])
            T.op('act', lambda e: e.activation(out=lamt[:, 194:196], in_=lamt[:, 192:194], func=AF.Exp),
                 reads=[("lamt", 2)], writes=[("lamt", 3)])
            T.op('dve', lambda e: e.tensor_tensor(lamt[:, 196:197], lamt[:, 194:195], lamt[:, 195:196], ALU.subtract),
                 reads=[("lamt", 3)], writes=[("lamt", 4)])
            T.op('dve', lambda e: e.tensor_scalar(lamt[:, 197:198], lamt[:, 196:197], -1.0, -lam_init, ALU.mult, ALU.add),
                 reads=[("lamt", 4)], writes=[("lamt", 5)])
            T.dma('sp', lam_d[0:1, 0:1], lamt[:, 197:198], reads=[("lamt", 5)], writes=[("lam_d",)], key="lam2")
            T.dma('sp', nlam[:], lam_d[0, 0:1].partition_broadcast(96), reads=[("lam_d",)], writes=[("nlam",)], key="lam3")
            T.dma('sp', gpr[:], dng[l * 96:(l + 1) * 96, :], writes=[("gpr",)], key="lam4")
            T.op('dve', lambda e: e.tensor_scalar(gpr[:], gpr[:], 1.0 - lam_init, None, ALU.mult),
                 reads=[("gpr",)], writes=[("gpr",)])
            for h in range(4):
                for c in range(2):
                    load_k(c, 640 + h * 96 + c * 48, 48)
                load_v(0, 640 + h * 96, 96)
                for qb in range(8):
                    accs = []
                    for c in range(2):
                        q, qr = load_q(640 + h * 96 + c * 48, 48, qb)
                        num, numr = Apool.next()
                        den, denr = Apool.next()
                        items = [(c, 48, q, qr, kb, [], 0) for kb in range(32)]
                        attn_tiles(items, 96, (num, numr, den, denr), 48 ** -0.5)
                        accs.append((num, numr, den, denr))
                    fs = []
                    for c in range(2):
                        num, numr, den, denr = accs[c]
                        r, rr = r_p.next()
                        T.op('dve', lambda e, r=r, den=den: e.reciprocal(r[:, :], den[0:96, :]), reads=[denr], writes=[rr])
                        f, fr = f_p.next()
                        T.op('dve', lambda e, f=f, num=num, r=r: e.tensor_tensor(f[:, :], num[0:96, :], r[:, :], ALU.mult),
                             reads=[numr, rr], writes=[fr])
                        fs.append((f, fr))
                    o, orr = f_p.next()
                    T.op('dve', lambda e: e.scalar_tensor_tensor(o[:, :], fs[1][0][:, :], nlam[:, 0:1], fs[0][0][:, :], ALU.mult, ALU.add),
                         reads=[fs[0][1], fs[1][1], ("nlam",)], writes=[orr])
                    sq, sqr = sq_p.next()
                    T.op('pool', lambda e: e.tensor_tensor(sq[:, :], o[:, :], o[:, :], ALU.mult), reads=[orr], writes=[sqr])
                    ms, msr = Spool.next()
                    T.op('pe', lambda e: e.matmul(ms[0:96, :], onesb[0:96, 0:96], sq[:, :], start=True, stop=True),
                         reads=[("onesb",), sqr], writes=[msr])
                    lnv, lnr = f_p.next()
                    T.op('act', lambda e: e.activation(out=lnv[:, :], in_=ms[0:96, :], func=AF.Ln, scale=1.0 / 96.0, bias=LN_EPS),
                         reads=[msr], writes=[lnr])
                    T.op('act', lambda e: e.activation(out=lnv[:, :], in_=lnv[:, :], func=AF.Exp, scale=-0.5),
                         reads=[lnr], writes=[lnr])
                    yo, yor = yo_p.next()
                    T.op('dve', lambda e: e.scalar_tensor_tensor(yo[:, :], o[:, :], gpr[:, 0:1], lnv[:, :], ALU.mult, ALU.mult),
                         reads=[orr, lnr, ("gpr",)], writes=[yor])
                    T.dma('sp', YT_d[384 + h * 96:384 + (h + 1) * 96, qb * 512:(qb + 1) * 512], yo[:, :], reads=[yor], key="st_y")
            T.barrier()

        with ExitStack() as st:
            wg = sb(st, "c_wg", [128, 8, 3072], BF16)
            wp = sb(st, "c_wp", [128, 6, D], BF16)
            wo = sb(st, "c_wo", [128, 8, D], BF16)
            bgt = sb(st, "c_bg", [128, 24], F32)
            g_t = sb(st, "c_lng", [128, D], F32)
            b_t = sb(st, "c_lnb", [128, D], F32)
            for kc in range(8):
                T.dma('pool', wg[:, kc, :], w_in[l * D + kc * 128:l * D + (kc + 1) * 128, 3072:6144],
                      writes=[("wg", kc)], key=("wg", kc % 2))
                T.dma('pool', wo[:, kc, :], w_out[l * D + kc * 128:l * D + (kc + 1) * 128, :], writes=[("wo", kc)], key=("wo", kc % 2))
            for c in range(2):
                T.dma('pool', wp[:, c, :], w_pa[l * 256 + c * 128:l * 256 + (c + 1) * 128, :], writes=[("wp", c)], key="wp")
            T.dma('pool', wp[:, 2, :], w_pb[l * 128:(l + 1) * 128, :], writes=[("wp", 2)], key="wp")
            for c in range(3):
                T.dma('pool', wp[:, 3 + c, :], w_pc[l * 384 + c * 128:l * 384 + (c + 1) * 128, :], writes=[("wp", 3 + c)], key="wp")
            T.dma('sp', bgt[:], bgT[l * 128:(l + 1) * 128, :], writes=[("bgt",)], key="cmisc")
            T.dma('sp', g_t[:], ln1g[l, :].partition_broadcast(128), writes=[("lng",)], key="cmisc")
            T.dma('sp', b_t[:], ln1b[l, :].partition_broadcast(128), writes=[("lnb",)], key="cmisc")
            xts = [sb(st, "c_xt%d" % i, [128, 8, 512], BF16) for i in range(2)]
            yts = [sb(st, "c_yt%d" % i, [128, 6, 512], BF16) for i in range(2)]
            gts = [sb(st, "c_g%d" % i, [128, 512], F32) for i in range(3)]
            mts = [sb(st, "c_m%d" % i, [128, 512], F32) for i in range(4)]
            mgs = [sb(st, "c_mg%d" % i, [128, 8, 512], BF16) for i in range(2)]
            xrs = [sb(st, "c_xr%d" % i, [128, D], F32) for i in range(2)]
            zs = [sb(st, "c_z%d" % i, [128, D], F32) for i in range(2)]
            lnpools = (Rot("c_st", [sb(st, "c_st%d" % i, [128, 12], F32) for i in range(2)]),
                       Rot("c_mv", [sb(st, "c_mv%d" % i, [128, 4], F32) for i in range(2)]),
                       Rot("c_xo", [sb(st, "c_xo%d" % i, [128, D], F32) for i in range(2)]),
                       Rot("c_xb", [sb(st, "c_xb%d" % i, [128, D], BF16) for i in range(2)]),
                       Rot("c_xT", [sb(st, "c_xT%d" % i, [128, D], BF16) for i in range(2)]))
            xt_p, yt_p, g_p, m_p, mg_p, xr_p, z_p = (Rot("c_xt", xts), Rot("c_yt", yts), Rot("c_g", gts), Rot("c_m", mts),
                                                     Rot("c_mg", mgs), Rot("c_xr", xrs), Rot("c_z", zs))
            br_chunks = [(0, 2), (2, 3), (3, 6)]
            alpha = (2 * depth) ** 0.25
            for tb in range(NTB):
                c0 = tb * 512
                xt, xtr = xt_p.next()
                T.dma('sp', xt[:], xT_d[:, c0:c0 + 512].rearrange("(kc p) t -> p kc t", p=128), writes=[xtr], key=("c_xt", xtr))
                yt, ytr = yt_p.next()
                T.dma('sp', yt[:], YT_d[:, c0:c0 + 512].rearrange("(c p) t -> p c t", p=128), writes=[ytr], key=("c_yt", ytr))
                mg, mgr = mg_p.next()
                for fc in range(8):
                    ms_ = []
                    for i in range(3):
                        pg, pgr = Spool.next()
                        for kc in range(8):
                            T.op('pe', lambda e, kc=kc, pg=pg, i=i: e.matmul(
                                pg[:, :], wg[:, kc, i * D + fc * 128:i * D + (fc + 1) * 128], xt[:, kc, :],
                                start=(kc == 0), stop=(kc == 7)), reads=[("wg", kc), xtr], writes=[pgr])
                        g, gr = g_p.next()
                        T.op('act', lambda e, pg=pg, g=g, i=i: e.activation(out=g[:], in_=pg[:, :], func=AF.Sigmoid,
                                                                         bias=bgt[:, i * 8 + fc:i * 8 + fc + 1]),
                             reads=[pgr, ("bgt",)], writes=[gr])
                        pp, ppr = Spool.next()
                        cs = list(range(*br_chunks[i]))
                        for ci, c in enumerate(cs):
                            T.op('pe', lambda e, c=c, ci=ci, pp=pp, cs=cs: e.matmul(
                                pp[:, :], wp[:, c, fc * 128:(fc + 1) * 128], yt[:, c, :],
                                start=(ci == 0), stop=(ci == len(cs) - 1)), reads=[("wp", c), ytr], writes=[ppr])
                        m, mr = m_p.next()
                        T.op('dve', lambda e, m=m, g=g, pp=pp: e.tensor_tensor(m[:], pp[:, :], g[:], ALU.mult),
                             reads=[ppr, gr], writes=[mr])
                        ms_.append((m, mr))
                    T.op('dve', lambda e, ms_=ms_: e.tensor_tensor(ms_[0][0][:], ms_[0][0][:], ms_[1][0][:], ALU.add),
                         reads=[ms_[0][1], ms_[1][1]], writes=[ms_[0][1]])
                    T.op('dve', lambda e, ms_=ms_, fc=fc: e.tensor_tensor(mg[:, fc, :], ms_[0][0][:], ms_[2][0][:], ALU.add),
                         reads=[ms_[0][1], ms_[2][1]], writes=[mgr + (fc,)])
                for tt in range(4):
                    row0 = c0 + tt * 128
                    xr, xrr = xr_p.next()
                    T.dma('sp', xr[:], x_cur[row0:row0 + 128, :], writes=[xrr], key=("c_xr", xrr))
                    z, zr = z_p.next()
                    for half in range(2):
                        py, pyr = Apool.next()
                        for fc in range(8):
                            T.op('pe', lambda e, fc=fc, py=py, half=half: e.matmul(
                                py[:, :], mg[:, fc, tt * 128:(tt + 1) * 128], wo[:, fc, half * 512:(half + 1) * 512],
                                start=(fc == 0), stop=(fc == 7)), reads=[mgr + (fc,), ("wo", fc)], writes=[pyr])
                        T.op('dve', lambda e, py=py, half=half: e.scalar_tensor_tensor(
                            z[:, half * 512:(half + 1) * 512], xr[:, half * 512:(half + 1) * 512], alpha, py[:, :], ALU.mult, ALU.add),
                            reads=[pyr, xrr], writes=[zr + (half,)])
                    ln_epilogue(st, z, [zr + (0,), zr + (1,)], g_t, b_t, row0, x1_d, x1T_d, lnpools, dst_b16=x1b_d)
            T.barrier()

        alpha = (2 * depth) ** 0.25
        dst_f32 = y_out if last else x2_d
        dstT = None if last else xT_d
        IND = bass.IndirectOffsetOnAxis
        with ExitStack() as stD:
            gT = sb(stD, "d_gT", [E, S], BF16)
            idxk = sb(stD, "d_idxk", [128, 4, 32], I32)
            wk = sb(stD, "d_wk", [128, 4, 32], F32)
            idxw = sb(stD, "d_idxw", [128, 8, 64], I32)
            idxb = sb(stD, "d_idxb", [128, 64], I32)
            with ExitStack() as st:
                wr = sb(st, "e_wr", [128, 8, E], BF16)
                brt = sb(st, "e_br", [128, E], F32)
                tri = sb(st, "e_tri", [128, 128], BF16)
                iop = sb(st, "e_iop", [128, 1], F32)
                T.dma('pool', wr[:], w_r[l * D:(l + 1) * D, :].rearrange("(kc p) e -> p kc e", p=128), writes=[("wr",)], key="emisc_p")
                T.dma('sp', brt[:], b_r[l, :].partition_broadcast(128), writes=[("brt",)], key="emisc")
                T.dma('sp', tri[:], c_tri[:, :], writes=[("tri",)], key="emisc")
                T.dma('sp', iop[:], c_iop[:, :], writes=[("iop",)], key="emisc")
                xq = sb(st, "e_xq", [128, 8, 1024], BF16)
                lgr = sb(st, "e_lgr", [128, 32, E], F32)
                lge = sb(st, "e_lge", [128, 32, E], F32)
                mk = sb(st, "e_mk", [128, 32, E], F32)
                mkb = sb(st, "e_mkb", [128, 32, E], BF16)
                gates = sb(st, "e_gates", [128, 32, E], F32)
                gbf = sb(st, "e_gbf", [128, 8, E], BF16)
                mx8 = sb(st, "e_mx8", [128, 32, 8], F32)
                sm = sb(st, "e_sm", [128, 32, 4], F32)
                pos = sb(st, "e_pos", [128, 32, E], F32)
                posf = sb(st, "e_posf", [128, 32, E], F32)
                oh = sb(st, "e_oh", [128, 32, E], F32)
                tmp = sb(st, "e_tmp", [128, 32, E], F32)
                tmp2 = sb(st, "e_tmp2", [128, 32, E], F32)
                cnt = sb(st, "e_cnt", [128, E], F32)
                scA = sb(st, "e_scA", [128, E], F32)
                scB = sb(st, "e_scB", [128, E], F32)
                ntl = sb(st, "e_ntl", [128, E], F32)
                off = sb(st, "e_off", [128, E], F32)
                cmp = sb(st, "e_cmp", [128, 64, E], F32)
                eidf = sb(st, "e_eidf", [128, 64], F32)
                ef2 = sb(st, "e_ef2", [128, 64], F32)
                pkf = sb(st, "e_pkf", [128, 4, 32], F32)
                xbs = [sb(st, "e_xb%d" % i, [128, D], BF16) for i in range(2)]
                xb_p = Rot("e_xb", xbs)
                for qi in range(4):
                    t0 = qi * 1024
                    T.dma('sp', xq[:], x1T_d[:, t0:t0 + 1024].rearrange("(kc p) t -> p kc t", p=128), writes=[("xq",)], key="xq")
                    for tt in range(8):
                        gtt = qi * 8 + tt
                        pl, plr = Spool.next()
                        for kc in range(8):
                            T.op('pe', lambda e, kc=kc, pl=pl: e.matmul(pl[:, 0:E], xq[:, kc, tt * 128:(tt + 1) * 128], wr[:, kc, :],
                                                                     start=(kc == 0), stop=(kc == 7)),
                                 reads=[("xq",), ("wr",)], writes=[plr])
                        R = lambda n: ("rt", n, gtt)
                        T.op('dve', lambda e, pl=pl: e.tensor_tensor(lgr[:, gtt, :], pl[:, 0:E], brt[:], ALU.add),
                             reads=[plr, ("brt",)], writes=[R("lg")])
                        T.op('dve', lambda e: e.max(mx8[:, gtt, :], lgr[:, gtt, :]), reads=[R("lg")], writes=[R("mx")])
                        T.op('dve', lambda e: e.tensor_scalar(mk[:, gtt, :], lgr[:, gtt, :], mx8[:, gtt, 3:4], None, ALU.is_ge),
                             reads=[R("lg"), R("mx")], writes=[R("mk")])
                        T.op('dve', lambda e: e.tensor_scalar(sm[:, gtt, 0:1], mx8[:, gtt, 0:1], -1.0, None, ALU.mult),
                             reads=[R("mx")], writes=[R("nm")])
                        T.op('act', lambda e: e.activation(out=lge[:, gtt, :], in_=lgr[:, gtt, :], func=AF.Exp, bias=sm[:, gtt, 0:1]),
                             reads=[R("lg"), R("nm")], writes=[R("le")])
                        T.op('act', lambda e: e.activation(out=mkb[:, gtt, :], in_=mk[:, gtt, :], func=AF.Copy),
                             reads=[R("mk")], writes=[("mkb", gtt)])
                        T.op('dve', lambda e: e.tensor_tensor(lge[:, gtt, :], mk[:, gtt, :], lge[:, gtt, :], ALU.mult),
                             reads=[R("le"), R("mk")], writes=[R("le")])
                        T.op('dve', lambda e: e.reduce_sum(sm[:, gtt, 1:2], lge[:, gtt, :], AX.X), reads=[R("le")], writes=[R("ss")])
                        T.op('dve', lambda e: e.reciprocal(sm[:, gtt, 2:3], sm[:, gtt, 1:2]), reads=[R("ss")], writes=[R("rs")])
                        T.op('dve', lambda e: e.tensor_scalar(gates[:, gtt, :], lge[:, gtt, :], sm[:, gtt, 2:3], None, ALU.mult),
                             reads=[R("le"), R("rs")], writes=[("gates", gtt)])
                        T.op('act', lambda e: e.activation(out=gbf[:, tt, :], in_=gates[:, gtt, :], func=AF.Copy),
                             reads=[("gates", gtt)], writes=[("gbf", tt)])
                        T.op('pe', lambda e: e.transpose(psT[0:E, tt * 128:(tt + 1) * 128], gbf[:, tt, :], ident[:]),
                             reads=[("gbf", tt), ("ident",)], writes=[("psT", tt)])
                    T.op('act', lambda e: e.activation(out=gT[:, t0:t0 + 1024], in_=psT[0:E, :], func=AF.Copy),
                         reads=[("psT", tt) for tt in range(8)], writes=[("gT", qi)])
                for i in range(32):
                    bank = psS[i // 16]
                    c = (i % 16) * E
                    for j in range(i + 1):
                        lhs, lres = (onesb, ("onesb",)) if j < i else (tri, ("tri",))
                        T.op('pe', lambda e, bank=bank, c=c, lhs=lhs, j=j, i=i: e.matmul(
                            bank[:, c:c + E], lhs[:, :], mkb[:, j, :], start=(j == 0), stop=(j == i)),
                            reads=[("mkb", j), lres], writes=[("psS", i // 16)])
                for j in range(32):
                    T.op('pe', lambda e, j=j: e.matmul(psS[2][:, 0:E], onesb[:, :], mkb[:, j, :], start=(j == 0), stop=(j == 31)),
                         reads=[("mkb", j), ("onesb",)], writes=[("psS", 2)])
                pos2 = pos[:].rearrange("p a e -> p (a e)")
                T.op('act', lambda e: e.activation(out=pos2[:, 0:16 * E], in_=psS[0][:, 0:16 * E], func=AF.Copy),
                     reads=[("psS", 0)], writes=[("pos", 0)])
                T.op('dve', lambda e: e.tensor_copy(pos2[:, 16 * E:32 * E], psS[1][:, 0:16 * E]), reads=[("psS", 1)], writes=[("pos", 1)])
                T.op('dve', lambda e: e.tensor_copy(cnt[:], psS[2][:, 0:E]), reads=[("psS", 2)], writes=[("cnt",)])
                RS = [("rsx",)]
                T.op('dve', lambda e: e.tensor_scalar(ntl[:], cnt[:], 0.0, None, ALU.is_gt), reads=[("cnt",)], writes=RS)
                for m_ in range(1, 8):
                    T.op('dve', lambda e, m_=m_: e.scalar_tensor_tensor(ntl[:], cnt[:], float(TS * m_), ntl[:], ALU.is_gt, ALU.add),
                         reads=RS, writes=RS)
                T.op('dve', lambda e: e.tensor_copy(scA[:], ntl[:]), reads=RS, writes=RS)
                bufs = [scA, scB]
                for s_, d_ in enumerate((1, 2, 4, 8, 16)):
                    src_, dst_ = bufs[s_ % 2], bufs[(s_ + 1) % 2]
                    T.op('dve', lambda e, src_=src_, dst_=dst_, d_=d_: e.tensor_copy(dst_[:, 0:d_], src_[:, 0:d_]), reads=RS, writes=RS)
                    T.op('dve', lambda e, src_=src_, dst_=dst_, d_=d_: e.tensor_tensor(dst_[:, d_:E], src_[:, d_:E], src_[:, 0:E - d_], ALU.add),
                         reads=RS, writes=RS)
                cend = scB
                T.op('dve', lambda e: e.tensor_tensor(off[:], cend[:], ntl[:], ALU.subtract), reads=RS, writes=RS)
                T.op('dve', lambda e: e.tensor_scalar(off[:], off[:], float(TS), None, ALU.mult), reads=RS, writes=RS)
                for j in range(NT):
                    T.op('dve', lambda e, j=j: e.tensor_scalar(cmp[:, j, :], cend[:], float(j), None, ALU.is_le), reads=RS, writes=RS)
                T.op('dve', lambda e: e.reduce_sum(eidf[:, 0:NT], cmp[:, 0:NT, :], AX.X), reads=RS, writes=RS)
                T.op('dve', lambda e: e.tensor_scalar(eidf[:, 0:NT], eidf[:, 0:NT], float(E - 1), None, ALU.min), reads=RS, writes=RS)
                T.op('dve', lambda e: e.tensor_scalar(ef2[:, 0:NT], eidf[:, 0:NT], float(D), None, ALU.mult), reads=RS, writes=RS)
                T.op('dve', lambda e: e.tensor_scalar(ef2[:, 0:NT], ef2[:, 0:NT], iop[:, 0:1], None, ALU.add), reads=RS + [("iop",)], writes=RS)
                for kc in range(8):
                    T.op('dve', lambda e, kc=kc: e.tensor_scalar(idxw[:, kc, 0:NT], ef2[:, 0:NT], float(l * E * D + kc * 128), None, ALU.add),
                         reads=RS, writes=[("idxw",)])
                T.op('dve', lambda e: e.tensor_scalar(ef2[:, 0:NT], eidf[:, 0:NT], 128.0, None, ALU.mult), reads=RS + [("idxw",)], writes=RS)
                T.op('dve', lambda e: e.tensor_scalar(ef2[:, 0:NT], ef2[:, 0:NT], iop[:, 0:1], None, ALU.add), reads=RS, writes=RS)
                T.op('dve', lambda e: e.tensor_scalar(idxb[:, 0:NT], ef2[:, 0:NT], float(l * E * 128), None, ALU.add), reads=RS, writes=[("idxb",)])
                T.op('dve', lambda e: e.tensor_tensor(posf[:], pos[:], off[:].unsqueeze(1).to_broadcast([128, 32, E]), ALU.add),
                     reads=RS + [("pos", 0), ("pos", 1)], writes=[("posf",)])
                all_lg = [("rt", "lg", g_) for g_ in range(32)] + [("rt", "mx", g_) for g_ in range(32)]
                all_gates = [("gates", g_) for g_ in range(32)]
                for k in range(4):
                    T.op('dve', lambda e, k=k: e.tensor_tensor(oh[:], lgr[:], mx8[:, :, k:k + 1].to_broadcast([128, 32, E]), ALU.is_equal),
                         reads=all_lg, writes=[("oh",)])
                    T.op('dve', lambda e: e.tensor_tensor(tmp[:], oh[:], posf[:], ALU.mult), reads=[("oh",), ("posf",)], writes=[("tmp",)])
                    T.op('dve', lambda e, k=k: e.reduce_sum(pkf[:, k, :], tmp[:], AX.X), reads=[("tmp",)], writes=[("pkf", k)])
                    T.op('dve', lambda e: e.tensor_tensor(tmp2[:], oh[:], gates[:], ALU.mult), reads=[("oh",)] + all_gates, writes=[("tmp2",)])
                    T.op('dve', lambda e, k=k: e.reduce_sum(wk[:, k, :], tmp2[:], AX.X), reads=[("tmp2",)], writes=[("wk", k)])
                T.op('dve', lambda e: e.tensor_copy(idxk[:], pkf[:]), reads=[("pkf", k) for k in range(4)], writes=[("idxk",)])
                for gtt in range(32):
                    xb, xbr = xb_p.next()
                    T.dma('sp', xb[:], x1b_d[gtt * 128:(gtt + 1) * 128, :], writes=[xbr], key=("e_xb", xbr))
                    for k in range(4):
                        T.idma(out=XS_d[:, :], out_off=IND(ap=idxk[:, k, gtt:gtt + 1], axis=0), in_=xb[:, :], bound=R_XS - 1,
                               reads=[xbr, ("idxk",)], key="scat")
                T.barrier()

            with ExitStack() as st:
                wgs = [sb(st, "x_wg%d" % i, [128, 8, D], BF16) for i in range(2)]
                wus = [sb(st, "x_wu%d" % i, [128, 8, D], BF16) for i in range(2)]
                wds = [sb(st, "x_wd%d" % i, [128, 8, D], BF16) for i in range(2)]
                bgs = [sb(st, "x_bg%d" % i, [128, 8], F32) for i in range(2)]
                bus = [sb(st, "x_bu%d" % i, [128, 8], F32) for i in range(2)]
                xsrs = [sb(st, "x_xsr%d" % i, [128, 4, D], BF16) for i in range(2)]
                xsTs = [sb(st, "x_xsT%d" % i, [128, 8, TS], BF16) for i in range(2)]
                hhs = [sb(st, "x_hh%d" % i, [128, 8, TS], BF16) for i in range(2)]
                hgs = [sb(st, "x_hg%d" % i, [128, 512], F32) for i in range(2)]
                sgs = [sb(st, "x_sg%d" % i, [128, 512], F32) for i in range(2)]
                hus = [sb(st, "x_hu%d" % i, [128, 512], F32) for i in range(2)]
                yrs = [sb(st, "x_yr%d" % i, [128, D], BF16) for i in range(3)]
                wg_p, wu_p, wd_p, bg_p, bu_p = Rot("x_wg", wgs), Rot("x_wu", wus), Rot("x_wd", wds), Rot("x_bg", bgs), Rot("x_bu", bus)
                xsr_p, xsT_p, hh_p, hg_p, sg_p, hu_p, yr_p = (Rot("x_xsr", xsrs), Rot("x_xsT", xsTs), Rot("x_hh", hhs), Rot("x_hg", hgs),
                                                              Rot("x_sg", sgs), Rot("x_hu", hus), Rot("x_yr", yrs))

                def prep(j):
                    P = {}
                    for nm, pool_, src_ in (("wg", wg_p, w_eg), ("wu", wu_p, w_eu), ("wd", wd_p, w_ed)):
                        wt, wres = pool_.next()
                        for kc in range(8):
                            T.idma(out=wt[:, kc, :], in_=src_[:, :], in_off=IND(ap=idxw[:, kc, j:j + 1], axis=0), bound=depth * E * D - 1,
                                   reads=[("idxw",)], writes=[wres + (kc // 4,)], key=wres)
                        P[nm] = (wt, wres)
                    for nm, pool_, src_ in (("bg", bg_p, begP), ("bu", bu_p, beuP)):
                        bt, bres = pool_.next()
                        T.idma(out=bt[:, :], in_=src_[:, :], in_off=IND(ap=idxb[:, j:j + 1], axis=0), bound=depth * E * 128 - 1,
                               reads=[("idxb",)], writes=[bres], key="ebias")
                        P[nm] = (bt, bres)
                    xsr, xsrr = xsr_p.next()
                    T.dma('sp', xsr[:], XS_d[j * TS:(j + 1) * TS, :].rearrange("(a p) d -> p a d", p=128), writes=[xsrr], key=xsrr)
                    P["xsr"] = (xsr, xsrr)
                    return P

                nxt = prep(0)
                for j in range(NT):
                    P = nxt
                    xsr, xsrr = P["xsr"]
                    wgt, wut, wdt = P["wg"], P["wu"], P["wd"]
                    bg, bgr = P["bg"]
                    bu, bur = P["bu"]
                    xsT, xsTr = xsT_p.next()
                    for a in range(4):
                        for fc in range(8):
                            T.op('pe', lambda e, a=a, fc=fc: e.transpose(psT[:, fc * 128:(fc + 1) * 128], xsr[:, a, fc * 128:(fc + 1) * 128], ident[:]),
                                 reads=[xsrr, ("ident",)], writes=[("psT", fc)])
                        T.op('act', lambda e, a=a: e.activation(out=xsT[:, :, a * 128:(a + 1) * 128],
                                                                in_=psT[:].rearrange("p (fc t) -> p fc t", t=128), func=AF.Copy),
                             reads=[("psT", fc) for fc in range(8)], writes=[xsTr + (a,)])
                    if j + 1 < NT:
                        nxt = prep(j + 1)
                    xsT_res = [xsTr + (a,) for a in range(4)]
                    hh, hhr = hh_p.next()
                    for fc in range(8):
                        pg, pgr = Spool.next()
                        pu, pur = Spool.next()
                        for (pt, ptr, wt) in ((pg, pgr, wgt), (pu, pur, wut)):
                            for kc in range(8):
                                T.op('pe', lambda e, kc=kc, pt=pt, wt=wt, fc=fc: e.matmul(
                                    pt[:, :], wt[0][:, kc, fc * 128:(fc + 1) * 128], xsT[:, kc, :],
                                    start=(kc == 0), stop=(kc == 7)), reads=[wt[1] + (kc // 4,)] + xsT_res, writes=[ptr])
                        hg, hgr = hg_p.next()
                        sg, sgr = sg_p.next()
                        hu, hur = hu_p.next()
                        T.op('dve', lambda e, pg=pg, hg=hg, fc=fc: e.tensor_scalar(hg[:], pg[:, :], bg[:, fc:fc + 1], SW_LIM, ALU.add, ALU.min),
                             reads=[pgr, bgr], writes=[hgr])
                        T.op('act', lambda e, hg=hg, sg=sg: e.activation(out=sg[:], in_=hg[:], func=AF.Sigmoid, scale=SW_ALPHA),
                             reads=[hgr], writes=[sgr])
                        T.op('dve', lambda e, pu=pu, hu=hu, fc=fc: e.tensor_scalar(hu[:], pu[:, :], bu[:, fc:fc + 1], SW_LIM, ALU.add, ALU.min),
                             reads=[pur, bur], writes=[hur])
                        T.op('dve', lambda e, hu=hu: e.tensor_scalar(hu[:], hu[:], -SW_LIM, 1.0, ALU.max, ALU.add),
                             reads=[hur], writes=[hur])
                        T.op('dve', lambda e, hg=hg, sg=sg: e.tensor_tensor(hg[:], hg[:], sg[:], ALU.mult),
                             reads=[hgr, sgr], writes=[hgr])
                        T.op('dve', lambda e, hg=hg, hu=hu, fc=fc: e.tensor_tensor(hh[:, fc, :], hg[:], hu[:], ALU.mult),
                             reads=[hgr, hur], writes=[hhr + (fc,)])
                    for a in range(4):
                        yr, yrr = yr_p.next()
                        for half in range(2):
                            py, pyr = Apool.next()
                            for fc in range(8):
                                T.op('pe', lambda e, fc=fc, py=py, half=half, a=a: e.matmul(
                                    py[:, :], hh[:, fc, a * 128:(a + 1) * 128], wdt[0][:, fc, half * 512:(half + 1) * 512],
                                    start=(fc == 0), stop=(fc == 7)),
                                    reads=[hhr + (fc,), wdt[1] + (fc // 4,)], writes=[pyr])
                            if half == 0:
                                T.op('act', lambda e, py=py, yr=yr: e.activation(out=yr[:, 0:512], in_=py[:, :], func=AF.Copy),
                                     reads=[pyr], writes=[yrr + (0,)])
                            else:
                                T.op('dve', lambda e, py=py, yr=yr: e.tensor_copy(yr[:, 512:1024], py[:, :]), reads=[pyr], writes=[yrr + (1,)])
                        T.dma('sp', YS_d[j * TS + a * 128:j * TS + (a + 1) * 128, :], yr[:], reads=[yrr + (0,), yrr + (1,)], key="st_ys")
                T.barrier()

            with ExitStack() as st:
                bdn = sb(st, "f_bdn", [E, D], BF16)
                g_t = sb(st, "f_lng", [128, D], F32)
                b_t = sb(st, "f_lnb", [128, D], F32)
                T.dma('pool', bdn[:], b_ed[l * E:(l + 1) * E, :], writes=[("bdn",)], key="emisc_p")
                T.dma('sp', g_t[:], ln2g[l, :].partition_broadcast(128), writes=[("lng",)], key="emisc")
                T.dma('sp', b_t[:], ln2b[l, :].partition_broadcast(128), writes=[("lnb",)], key="emisc")
                ygs = [sb(st, "f_yg%d" % i, [128, 4, D], BF16) for i in range(2)]
                xrs = [sb(st, "f_xr%d" % i, [128, D], F32) for i in range(2)]
                zs = [sb(st, "f_z%d" % i, [128, D], F32) for i in range(2)]
                lnpools = (Rot("f_st", [sb(st, "f_st%d" % i, [128, 12], F32) for i in range(2)]),
                           Rot("f_mv", [sb(st, "f_mv%d" % i, [128, 4], F32) for i in range(2)]),
                           Rot("f_xo", [sb(st, "f_xo%d" % i, [128, D], F32) for i in range(2)]),
                           Rot("f_xb", [sb(st, "f_xb%d" % i, [128, D], BF16) for i in range(2)]),
                           Rot("f_xT", [sb(st, "f_xT%d" % i, [128, D], BF16) for i in range(2)]))
                yg_p, xr_p, z_p = Rot("f_yg", ygs), Rot("f_xr", xrs), Rot("f_z", zs)
                for gtt in range(32):
                    row0 = gtt * 128
                    yg, ygr = yg_p.next()
                    for k in range(4):
                        T.idma(out=yg[:, k, :], in_=YS_d[:, :], in_off=IND(ap=idxk[:, k, gtt:gtt + 1], axis=0), bound=R_XS - 1,
                               reads=[("idxk",)], writes=[ygr + (k,)], key=ygr)
                    xr, xrr = xr_p.next()
                    T.dma('sp', xr[:], x1_d[row0:row0 + 128, :], writes=[xrr], key=xrr)
                    z, zr = z_p.next()
                    for half in range(2):
                        py, pyr = Apool.next()
                        T.op('pe', lambda e, py=py, half=half: e.matmul(py[:, :], gT[:, row0:row0 + 128],
                                                                      bdn[:, half * 512:(half + 1) * 512], start=True, stop=True),
                             reads=[("gT", gtt // 8), ("bdn",)], writes=[pyr])
                        T.op('dve', lambda e, py=py, half=half: e.scalar_tensor_tensor(
                            z[:, half * 512:(half + 1) * 512], xr[:, half * 512:(half + 1) * 512], alpha, py[:, :], ALU.mult, ALU.add),
                            reads=[pyr, xrr], writes=[zr + (half,)])
                    zres = [zr + (0,), zr + (1,)]
                    for k in range(4):
                        T.op('dve', lambda e, k=k: e.scalar_tensor_tensor(z[:], yg[:, k, :], wk[:, k, gtt:gtt + 1], z[:], ALU.mult, ALU.add),
                             reads=[ygr + (k,), ("wk", k)] + zres, writes=zres)
                    ln_epilogue(st, z, zres, g_t, b_t, row0, dst_f32, dstT, lnpools)
                T.barrier()
        x_cur = x2_d

    T.barrier()
    es.close()
    return nc


def _consts():
    bf = ml_dtypes.bfloat16
    c = {}
    c["c_ident"] = np.eye(128, dtype=np.float32).astype(bf)
    c["c_tri"] = np.triu(np.ones((128, 128), np.float32), k=1).astype(bf)
    c["c_iop"] = np.arange(128, dtype=np.float32).reshape(128, 1)
    t = np.arange(S, dtype=np.float32)

    def tab(dim, rows):
        inv = (1.0 / (np.float32(10000.0) ** (np.arange(0, dim, 2, dtype=np.float32) / np.float32(dim)))).astype(np.float32)
        ang = (t[:, None] * inv[None, :]).astype(np.float32)
        cs, sn = np.cos(ang).astype(np.float32), np.sin(ang).astype(np.float32)
        idx = (np.arange(rows) % dim) % (dim // 2)
        return np.ascontiguousarray(cs[:, idx].T), np.ascontiguousarray(sn[:, idx].T)

    c["c_cosB"], c["c_sinB"] = tab(64, 128)
    c["c_cosC"], c["c_sinC"] = tab(48, 96)
    k = np.arange(128)
    q = np.arange(512)
    mA = np.zeros((3, 8, 128, 512), np.float32)
    for var, qb in ((0, 0), (1, 3), (2, 7)):
        r0 = 8 * qb
        r = r0 + q // 64
        cc = q % 64
        rs = np.clip(r - 4, 0, 56)
        cs_ = np.clip(cc - 8, 0, 48)
        for j in range(8):
            kr = r0 - 4 + 2 * j + k // 64
            kc = k % 64
            ok = ((kr[:, None] >= rs[None, :]) & (kr[:, None] < rs[None, :] + 8) &
                  (kc[:, None] >= cs_[None, :]) & (kc[:, None] < cs_[None, :] + 16))
            mA[var, j] = np.where(ok, 0.0, NEG)
    c["c_maskA"] = np.ascontiguousarray(mA.reshape(24, 128, 512).transpose(1, 0, 2)).reshape(128, 24 * 512).astype(bf)
    mB = np.zeros((34, 128, 512), np.float32)
    for g, d in enumerate((1, 4, 16)):
        for jb in range(B_JB[g][0], B_JB[g][1] + 1):
            kk = jb * 128 + k
            diff = kk[:, None] - q[None, :]
            ok = (np.abs(diff) <= 64 * d) & (diff % d == 0)
            mB[B_OFF[g] + jb - B_JB[g][0]] = np.where(ok, 0.0, NEG)
    c["c_maskB"] = np.ascontiguousarray(mB.transpose(1, 0, 2)).reshape(128, 34 * 512).astype(bf)
    return c


def _bias_gather(na_rpb):
    L, H = na_rpb.shape[0], na_rpb.shape[1]
    k = np.arange(128)
    q = np.arange(512)
    j = np.arange(8)
    dr = (-4 + 2 * j[None, :, None] + k[:, None, None] // 64) - (q[None, None, :] // 64)
    dc = (k[:, None, None] % 64) - (q[None, None, :] % 64) + 0 * j[None, :, None]
    ri = np.clip(dr + 7, 0, 14)
    ci = np.clip(dc + 15, 0, 30)
    out = na_rpb[:, :, ri, ci]
    return np.ascontiguousarray(out).reshape(L * H * 128, 8 * 512)


def prepare_inputs(inp, depth, n_exp):
    f = lambda a: np.ascontiguousarray(np.asarray(a, dtype=np.float32))
    E = n_exp
    m = {}
    m["w_in"] = f(inp["w_in"]).reshape(depth * D, 6144)
    m["bgT"] = np.ascontiguousarray(f(inp["b_gates"]).reshape(depth, 24, 128).transpose(0, 2, 1)).reshape(depth * 128, 24)
    m["w_proj_a"] = f(inp["w_proj_a"]).reshape(depth * 256, D)
    m["w_proj_b"] = f(inp["w_proj_b"]).reshape(depth * 128, D)
    m["w_proj_c"] = f(inp["w_proj_c"]).reshape(depth * 384, D)
    m["w_out"] = f(inp["w_out"]).reshape(depth * D, D)
    m["biasA"] = _bias_gather(f(inp["na_rpb"]))
    m["diff_lambda"] = f(inp["diff_lambda"]).reshape(depth, 192)
    m["diff_norm_g"] = f(inp["diff_norm_g"]).reshape(depth * 96, 1)
    for k_ in ("ln1_g", "ln1_b", "ln2_g", "ln2_b"):
        m[k_] = f(inp[k_]).reshape(depth, D)
    m["w_router"] = f(inp["w_router"]).reshape(depth * D, E)
    m["b_router"] = f(inp["b_router"]).reshape(depth, E)
    m["w_exp_gate"] = f(inp["w_exp_gate"]).reshape(depth * E * D, D)
    m["w_exp_up"] = f(inp["w_exp_up"]).reshape(depth * E * D, D)
    m["w_exp_down"] = f(inp["w_exp_down"]).reshape(depth * E * D, D)
    m["begP"] = np.ascontiguousarray(f(inp["b_exp_gate"]).reshape(depth, E, 8, 128).transpose(0, 1, 3, 2)).reshape(depth * E * 128, 8)
    m["beuP"] = np.ascontiguousarray(f(inp["b_exp_up"]).reshape(depth, E, 8, 128).transpose(0, 1, 3, 2)).reshape(depth * E * 128, 8)
    m["b_exp_down"] = f(inp["b_exp_down"]).reshape(depth * E, D)
    m.update(_consts())
    return m


def kernel(**inputs):
    depth, n_exp, n_cores = 4, 32, 8
    x = np.asarray(inputs["x"], dtype=np.float32)
    shared = prepare_inputs(inputs, depth, n_exp)
    nc = build_program(depth, n_exp)
    in_maps = []
    for c in range(n_cores):
        m = dict(shared)
        m["x"] = np.ascontiguousarray(x[c])
        in_maps.append(m)
    res = run_bass_kernel_spmd(nc, in_maps, core_ids=list(range(n_cores)))
    return np.stack([np.asarray(res.results[c]["y"], dtype=np.float32) for c in range(n_cores)], axis=0)
```

```python
import math
from contextlib import ExitStack
import numpy as np
import ml_dtypes
import concourse.bass as bass
import concourse.mybir as mybir
from concourse.bass_utils import run_bass_kernel_spmd

F32 = mybir.dt.float32
BF16 = mybir.dt.bfloat16
I32 = mybir.dt.int32
AF = mybir.ActivationFunctionType
ALU = mybir.AluOpType
AX = mybir.AxisListType

S = 4096
D = 1024
NTB = 8
LN_EPS = 1e-5
NEG = -30000.0
SW_ALPHA = 1.702
SW_LIM = 7.0
B_JB = [(-1, 4), (-2, 5), (-8, 11)]
B_OFF = [0, 6, 14]
TS = 512
NT = 63
R_XS = NT * TS


class Trk:
    def __init__(s, nc, es):
        s.nc = nc
        s.es = es
        s.eng = {'pe': nc.tensor, 'act': nc.scalar, 'dve': nc.vector, 'pool': nc.gpsimd, 'sp': nc.sync}
        s.esem = {k: es.enter_context(nc.semaphore('e_' + k)) for k in ('pe', 'act', 'dve', 'pool')}
        s.ecnt = {k: 0 for k in s.esem}
        s.dsem = {}
        s.dcnt = {}
        s.waited = {k: {} for k in s.eng}
        s.lastw = {}
        s.rd = {}

    def _wait(s, e, tok):
        kind, key, val = tok
        if kind == 'e':
            if key == 'pe' and e == 'pe':
                return
            if val <= 0 or s.waited[e].get(('e', key), 0) >= val:
                return
            s.eng[e].wait_ge(s.esem[key], val)
            s.waited[e][('e', key)] = val
        else:
            tgt = s.dcnt[key]
            if tgt <= 0 or s.waited[e].get(('d', key), 0) >= tgt:
                return
            s.eng[e].wait_ge(s.dsem[key], tgt)
            s.waited[e][('d', key)] = tgt

    def _deps(s, e, reads, writes):
        for r in reads:
            t = s.lastw.get(r)
            if t:
                s._wait(e, t)
        for w in writes:
            t = s.lastw.get(w)
            if t:
                s._wait(e, t)
            for t in s.rd.get(w, {}).values():
                s._wait(e, t)

    def _record(s, tok, reads, writes):
        for r in reads:
            s.rd.setdefault(r, {})[(tok[0], tok[1])] = tok
        for w in writes:
            s.lastw[w] = tok
            s.rd[w] = {}

    def op(s, e, fn, reads=(), writes=()):
        s._deps(e, reads, writes)
        ins = fn(s.eng[e])
        s.ecnt[e] += 1
        ins.then_inc(s.esem[e], 1)
        s._record(('e', e, s.ecnt[e]), reads, writes)

    def dma(s, q, out, in_, reads=(), writes=(), key=None):
        s._deps(q, reads, writes)
        if key not in s.dsem:
            s.dsem[key] = s.es.enter_context(s.nc.semaphore('d_%d' % len(s.dsem)))
            s.dcnt[key] = 0
        s.eng[q].dma_start(out=out, in_=in_).then_inc(s.dsem[key], 16)
        s.dcnt[key] += 16
        s._record(('d', key, s.dcnt[key]), reads, writes)

    def idma(s, out, in_, out_off=None, in_off=None, bound=None, reads=(), writes=(), key=None):
        s._deps('pool', reads, writes)
        if key not in s.dsem:
            s.dsem[key] = s.es.enter_context(s.nc.semaphore('d_%d' % len(s.dsem)))
            s.dcnt[key] = 0
        s.nc.gpsimd.indirect_dma_start(out=out, out_offset=out_off, in_=in_, in_offset=in_off).then_inc(s.dsem[key], 16)
        s.dcnt[key] += 16
        s._record(('d', key, s.dcnt[key]), reads, writes)

    def barrier(s):
        for e in s.eng:
            for k in s.esem:
                s._wait(e, ('e', k, s.ecnt[k]))
            for k in s.dsem:
                s._wait(e, ('d', k, 0))
        s.lastw = {}
        s.rd = {}


class Rot:
    def __init__(s, name, views):
        s.name = name
        s.views = views
        s.i = -1

    def next(s):
        s.i = (s.i + 1) % len(s.views)
        return s.views[s.i], (s.name, s.i)


def build_program(depth, n_exp, debug=False):
    nc = bass.Bass("TRN2", target_bir_lowering=False)
    es = ExitStack()
    T = Trk(nc, es)
    E = n_exp

    def din(name, shape, dt=F32):
        return nc.dram_tensor(name, list(shape), dt, kind="ExternalInput").ap()

    def dscr(name, shape, dt):
        kind = "ExternalOutput" if debug else "Internal"
        return nc.dram_tensor(name, list(shape), dt, kind=kind).ap()

    x_in = din("x", [S, D])
    w_in = din("w_in", [depth * D, 6144])
    bgT = din("bgT", [depth * 128, 24])
    w_pa = din("w_proj_a", [depth * 256, D])
    w_pb = din("w_proj_b", [depth * 128, D])
    w_pc = din("w_proj_c", [depth * 384, D])
    w_out = din("w_out", [depth * D, D])
    biasA = din("biasA", [depth * 4 * 128, 8 * 512])
    dlam = din("diff_lambda", [depth, 192])
    dng = din("diff_norm_g", [depth * 96, 1])
    ln1g = din("ln1_g", [depth, D])
    ln1b = din("ln1_b", [depth, D])
    ln2g = din("ln2_g", [depth, D])
    ln2b = din("ln2_b", [depth, D])
    w_r = din("w_router", [depth * D, E])
    b_r = din("b_router", [depth, E])
    w_eg = din("w_exp_gate", [depth * E * D, D])
    w_eu = din("w_exp_up", [depth * E * D, D])
    w_ed = din("w_exp_down", [depth * E * D, D])
    begP = din("begP", [depth * E * 128, 8])
    beuP = din("beuP", [depth * E * 128, 8])
    c_tri = din("c_tri", [128, 128], BF16)
    c_iop = din("c_iop", [128, 1])
    b_ed = din("b_exp_down", [depth * E, D])
    c_ident = din("c_ident", [128, 128], BF16)
    c_cosB = din("c_cosB", [128, S])
    c_sinB = din("c_sinB", [128, S])
    c_cosC = din("c_cosC", [96, S])
    c_sinC = din("c_sinC", [96, S])
    c_maskA = din("c_maskA", [128, 24 * 512], BF16)
    c_maskB = din("c_maskB", [128, 34 * 512], BF16)
    y_out = nc.dram_tensor("y", [S, D], F32, kind="ExternalOutput").ap()

    xT_d = dscr("xT_d", [D, S], BF16)
    x1T_d = dscr("x1T_d", [D, S], BF16)
    QT_d = dscr("QT_d", [D, S], BF16)
    KT_d = dscr("KT_d", [D, S], BF16)
    V_d = dscr("V_d", [S, D], BF16)
    YT_d = dscr("YT_d", [768, S], BF16)
    x1_d = dscr("x1_d", [S, D], F32)
    x2_d = dscr("x2_d", [S, D], F32)
    lam_d = dscr("lam_d", [1, 8], F32)
    x1b_d = dscr("x1b_d", [S, D], BF16)
    XS_d = dscr("XS_d", [R_XS, D], BF16)
    YS_d = dscr("YS_d", [R_XS, D], BF16)

    sb_n = [0]

    def sb(stack, name, shape, dt):
        sb_n[0] += 1
        return stack.enter_context(nc.sbuf_tensor("%s_%d" % (name, sb_n[0]), list(shape), dt))

    ident = sb(es, "ident", [128, 128], BF16)
    onesb = sb(es, "onesb", [128, 128], BF16)
    psS = [es.enter_context(nc.psum_tensor("psS%d" % i, [128, 512], F32)) for i in range(3)]
    psA = [es.enter_context(nc.psum_tensor("psA%d" % i, [128, 512], F32)) for i in range(4)]
    psT = es.enter_context(nc.psum_tensor("psT", [128, 1024], BF16))
    Spool = Rot("psS", psS)
    Apool = Rot("psA", psA)

    T.dma('sp', ident[:], c_ident[:, :], writes=[("ident",)], key="const")
    T.op('pool', lambda e: e.memset(onesb[:], 1.0), writes=[("onesb",)])

    def ln_epilogue(st, z, zres, g_t, b_t, row0, dst_f32, dstT, pools, dst_b16=None):
        stt, mv, xo_p, xb_p, xt_p = pools
        st6, st6r = stt.next()
        T.op('dve', lambda e: e.bn_stats(st6[:, 0:6], z[:, 0:512]), reads=zres, writes=[st6r + ("a",)])
        T.op('dve', lambda e: e.bn_stats(st6[:, 6:12], z[:, 512:1024]), reads=zres, writes=[st6r + ("b",)])
        m, mr = mv.next()
        T.op('dve', lambda e: e.bn_aggr(m[:, 0:2], st6[:, 0:12]), reads=[st6r + ("a",), st6r + ("b",)], writes=[mr])
        T.op('act', lambda e: e.activation(out=m[:, 3:4], in_=m[:, 1:2], func=AF.Ln, bias=LN_EPS),
             reads=[mr], writes=[mr + ("l",)])
        T.op('act', lambda e: e.activation(out=m[:, 2:3], in_=m[:, 3:4], func=AF.Exp, scale=-0.5),
             reads=[mr + ("l",)], writes=[mr + ("r",)])
        xo, xor_ = xo_p.next()
        T.op('dve', lambda e: e.tensor_scalar(xo[:], z[:], m[:, 0:1], m[:, 2:3], ALU.subtract, ALU.mult),
             reads=zres + [mr, mr + ("r",)], writes=[xor_])
        T.op('dve', lambda e: e.tensor_tensor(xo[:], xo[:], g_t[:], ALU.mult), reads=[xor_, ("lng",)], writes=[xor_])
        T.op('dve', lambda e: e.tensor_tensor(xo[:], xo[:], b_t[:], ALU.add), reads=[xor_, ("lnb",)], writes=[xor_])
        T.dma('sp', dst_f32[row0:row0 + 128, :], xo[:], reads=[xor_], key=("st_x",) + xor_)
        if dstT is not None:
            xb, xbr = xb_p.next()
            T.op('act', lambda e: e.activation(out=xb[:], in_=xo[:], func=AF.Copy), reads=[xor_], writes=[xbr])
            if dst_b16 is not None:
                T.dma('sp', dst_b16[row0:row0 + 128, :], xb[:], reads=[xbr], key=("st_xb",) + xbr)
            transpose_store(xb, xbr, row0, dstT, xt_p)

    def transpose_store(xb, xbr, row0, dstT, xt_p):
        for fc in range(8):
            T.op('pe', lambda e, fc=fc: e.transpose(psT[:, fc * 128:(fc + 1) * 128], xb[:, fc * 128:(fc + 1) * 128], ident[:]),
                 reads=[xbr, ("ident",)], writes=[("psT", fc)])
        xt, xtr = xt_p.next()
        T.op('act', lambda e: e.activation(out=xt[:], in_=psT[:], func=AF.Copy),
             reads=[("psT", fc) for fc in range(8)], writes=[xtr])
        T.dma('sp', dstT[:, row0:row0 + 128].rearrange("(fc p) t -> p fc t", p=128),
              xt[:].rearrange("p (fc t) -> p fc t", t=128), reads=[xtr], key=("st_xT",) + xtr)

    with ExitStack() as st:
        xl = [sb(st, "p_xl%d" % i, [128, D], F32) for i in range(2)]
        xbs = [sb(st, "p_xb%d" % i, [128, D], BF16) for i in range(2)]
        xts = [sb(st, "p_xt%d" % i, [128, D], BF16) for i in range(2)]
        xl_p, xb_p, xt_p = Rot("p_xl", xl), Rot("p_xb", xbs), Rot("p_xt", xts)
        zt = sb(st, "p_zero", [128, 4096], BF16)
        T.op('pool', lambda e: e.memset(zt[:], 0.0), writes=[("zt",)])
        XSv = XS_d.rearrange("(p a) d -> p (a d)", p=128)
        for c in range(R_XS * D // 128 // 4096):
            T.dma('sp', XSv[:, c * 4096:(c + 1) * 4096], zt[:], reads=[("zt",)], key="zero")
        for tt in range(32):
            xt_, xr = xl_p.next()
            T.dma('sp', xt_[:], x_in[tt * 128:(tt + 1) * 128, :], writes=[xr], key=("ldx", xr))
            xb, xbr = xb_p.next()
            T.op('act', lambda e: e.activation(out=xb[:], in_=xt_[:], func=AF.Copy), reads=[xr], writes=[xbr])
            transpose_store(xb, xbr, tt * 128, xT_d, xt_p)
        T.barrier()

    x_cur = x_in
    for l in range(depth):
        lam_init = 0.8 - 0.6 * math.exp(-0.3 * l)
        last = (l == depth - 1)
        with ExitStack() as st:
            wq = sb(st, "a_wq", [128, 8, 3072], BF16)
            wrot = sb(st, "a_wrot", [128, 8, 1536], BF16)
            for kc in range(8):
                T.dma('pool', wq[:, kc, :], w_in[l * D + kc * 128:l * D + (kc + 1) * 128, 0:3072],
                      writes=[("wq", kc)], key=("wq", kc % 2))
            for kc in range(8):
                for (sbase, dbase, nu, w) in ((768, 0, 12, 64), (1920, 768, 16, 48)):
                    h = w // 2
                    src = wq[:, kc, sbase:sbase + nu * w].rearrange("p (u j) -> p u j", j=w)
                    dst = wrot[:, kc, dbase:dbase + nu * w].rearrange("p (u j) -> p u j", j=w)
                    T.op('pool', lambda e, src=src, dst=dst, h=h, w=w: e.tensor_scalar(
                        dst[:, :, 0:h], src[:, :, h:w], -1.0, None, ALU.mult),
                        reads=[("wq", kc)], writes=[("wrot", kc, dbase, 0)])
                    T.op('pool', lambda e, src=src, dst=dst, h=h, w=w: e.tensor_copy(dst[:, :, h:w], src[:, :, 0:h]),
                         reads=[("wq", kc)], writes=[("wrot", kc, dbase, 1)])
            wq_res = [("wq", kc) for kc in range(8)]
            wrot_res = [("wrot", kc, db, hh) for kc in range(8) for db in (0, 768) for hh in (0, 1)]
            xts = [sb(st, "a_xt%d" % i, [128, 8, 512], BF16) for i in range(2)]
            tabs = [sb(st, "a_tab%d" % i, [128, 4, 512], F32) for i in range(2)]
            t1s = [sb(st, "a_t1%d" % i, [128, 512], F32) for i in range(2)]
            t2s = [sb(st, "a_t2%d" % i, [128, 512], F32) for i in range(2)]
            obs = [sb(st, "a_ob%d" % i, [128, 512], BF16) for i in range(3)]
            vts = [sb(st, "a_vt%d" % i, [128, 1024], BF16) for i in range(2)]
            xt_p, tab_p, t1_p, t2_p, ob_p, vt_p = (Rot("a_xt", xts), Rot("a_tab", tabs), Rot("a_t1", t1s),
                                                   Rot("a_t2", t2s), Rot("a_ob", obs), Rot("a_vt", vts))
            chunks = []
            for i in range(2):
                chunks.append((i * 128, 128, QT_d, i * 128, None, 0, 0.125))
                chunks.append((256 + i * 128, 128, KT_d, i * 128, None, 0, 1.0))
            for i in range(3):
                chunks.append((768 + i * 128, 128, QT_d, 256 + i * 128, i * 128, 0, 1.0))
                chunks.append((1152 + i * 128, 128, KT_d, 256 + i * 128, 384 + i * 128, 0, 1.0))
            for i in range(4):
                chunks.append((1920 + i * 96, 96, QT_d, 640 + i * 96, 768 + i * 96, 1, 1.0))
                chunks.append((2304 + i * 96, 96, KT_d, 640 + i * 96, 1152 + i * 96, 1, 1.0))
            for tb in range(NTB):
                c0 = tb * 512
                xt, xtr = xt_p.next()
                T.dma('sp', xt[:], xT_d[:, c0:c0 + 512].rearrange("(kc p) t -> p kc t", p=128), writes=[xtr], key=("a_xt", xtr))
                tab, tabr = tab_p.next()
                T.dma('sp', tab[:, 0, :], c_cosB[:, c0:c0 + 512], writes=[tabr + (0,)], key=("a_tab", tabr))
                T.dma('sp', tab[:, 1, :], c_sinB[:, c0:c0 + 512], writes=[tabr + (1,)], key=("a_tab", tabr))
                T.dma('sp', tab[0:96, 2, :], c_cosC[:, c0:c0 + 512], writes=[tabr + (2,)], key=("a_tab", tabr))
                T.dma('sp', tab[0:96, 3, :], c_sinC[:, c0:c0 + 512], writes=[tabr + (3,)], key=("a_tab", tabr))
                for (col0, M, dst, drow, rc0, ti, scale) in chunks:
                    p1, p1r = Spool.next()
                    for kc in range(8):
                        T.op('pe', lambda e, kc=kc, p1=p1: e.matmul(p1[0:M, :], wq[:, kc, col0:col0 + M], xt[:, kc, :],
                                                                 start=(kc == 0), stop=(kc == 7)),
                             reads=[("wq", kc), xtr], writes=[p1r])
                    ob, obr = ob_p.next()
                    if rc0 is None:
                        T.op('act', lambda e, p1=p1, ob=ob: e.activation(out=ob[0:M, :], in_=p1[0:M, :], func=AF.Copy, scale=scale),
                             reads=[p1r], writes=[obr])
                    else:
                        p2, p2r = Spool.next()
                        for kc in range(8):
                            T.op('pe', lambda e, kc=kc, p2=p2: e.matmul(p2[0:M, :], wrot[:, kc, rc0:rc0 + M], xt[:, kc, :],
                                                                     start=(kc == 0), stop=(kc == 7)),
                                 reads=wrot_res[kc * 4:(kc + 1) * 4] + [xtr], writes=[p2r])
                        t1, t1r = t1_p.next()
                        t2, t2r = t2_p.next()
                        T.op('dve', lambda e, p1=p1, t1=t1: e.tensor_tensor(t1[0:M, :], p1[0:M, :], tab[0:M, 2 * ti, :], ALU.mult),
                             reads=[p1r, tabr + (2 * ti,)], writes=[t1r])
                        T.op('dve', lambda e, p2=p2, t2=t2: e.tensor_tensor(t2[0:M, :], p2[0:M, :], tab[0:M, 2 * ti + 1, :], ALU.mult),
                             reads=[p2r, tabr + (2 * ti + 1,)], writes=[t2r])
                        T.op('dve', lambda e, t1=t1, t2=t2, ob=ob: e.tensor_tensor(ob[0:M, :], t1[0:M, :], t2[0:M, :], ALU.add),
                             reads=[t1r, t2r], writes=[obr])
                    T.dma('sp', dst[drow:drow + M, c0:c0 + 512], ob[0:M, :], reads=[obr], key=("st_a",) + obr)
                for tt in range(4):
                    vt, vtr = vt_p.next()
                    for (vc0, n, dcol) in ((512, 256, 0), (1536, 384, 256), (2688, 384, 640)):
                        pv, pvr = Spool.next()
                        for kc in range(8):
                            T.op('pe', lambda e, kc=kc, pv=pv: e.matmul(pv[:, 0:n], xt[:, kc, tt * 128:(tt + 1) * 128],
                                                                     wq[:, kc, vc0:vc0 + n], start=(kc == 0), stop=(kc == 7)),
                                 reads=[("wq", kc), xtr], writes=[pvr])
                        T.op('act', lambda e, pv=pv, vt=vt: e.activation(out=vt[:, dcol:dcol + n], in_=pv[:, 0:n], func=AF.Copy),
                             reads=[pvr], writes=[vtr + (dcol,)])
                    T.dma('sp', V_d[c0 + tt * 128:c0 + (tt + 1) * 128, :], vt[:],
                          reads=[vtr + (0,), vtr + (256,), vtr + (640,)], key=("st_v",) + vtr)
            T.barrier()

        with ExitStack() as st:
            kts = [sb(st, "b_kt%d" % i, [64, S], BF16) for i in range(6)]
            vhs = [sb(st, "b_vh%d" % i, [128, 32, 96], BF16) for i in range(6)]
            maskt = sb(st, "b_mask", [128, 34, 512], BF16)
            biast = sb(st, "b_bias", [128, 8, 512], BF16)
            qts = [sb(st, "b_q%d" % i, [64, 512], BF16) for i in range(4)]
            ets = [sb(st, "b_e%d" % i, [128, 512], BF16) for i in range(4)]
            rts = [sb(st, "b_r%d" % i, [96, 512], F32) for i in range(2)]
            fts = [sb(st, "b_f%d" % i, [96, 512], F32) for i in range(4)]
            sqs = [sb(st, "b_sq%d" % i, [96, 512], BF16) for i in range(2)]
            yos = [sb(st, "b_yo%d" % i, [96, 512], BF16) for i in range(2)]
            lamt = sb(st, "b_lam", [1, 200], F32)
            nlam = sb(st, "b_nlam", [96, 1], F32)
            gpr = sb(st, "b_gpr", [96, 1], F32)
            q_p, e_p, r_p, f_p, sq_p, yo_p = (Rot("b_q", qts), Rot("b_e", ets), Rot("b_r", rts), Rot("b_f", fts),
                                              Rot("b_sq", sqs), Rot("b_yo", yos))

            def load_k(slot, row0, d):
                T.dma('sp', kts[slot][0:d, :], KT_d[row0:row0 + d, :], writes=[("kt", slot)], key=("kt", slot))

            def load_v(slot, col0, e_):
                T.dma('sp', vhs[slot][:, :, 0:e_], V_d[:, col0:col0 + e_].rearrange("(kb p) e -> p kb e", p=128),
                      writes=[("vh", slot)], key=("vh", slot))

            def load_q(row0, d, qb):
                q, qr = q_p.next()
                T.dma('sp', q[0:d, :], QT_d[row0:row0 + d, qb * 512:(qb + 1) * 512], writes=[qr], key=("q", qr))
                return q, qr

            def attn_tiles(items, e_, accs, scale):
                num, numr, den, denr = accs
                n = len(items)
                for i, (ks, d, q, qr, kb, biases, vs) in enumerate(items):
                    sp_, spr = Spool.next()
                    T.op('pe', lambda e, sp_=sp_, ks=ks, d=d, q=q, kb=kb: e.matmul(
                        sp_[:, :], kts[ks][0:d, kb * 128:(kb + 1) * 128], q[0:d, :], start=True, stop=(len(biases) == 0)),
                        reads=[("kt", ks), qr], writes=[spr])
                    for bi, (bap, bres) in enumerate(biases):
                        T.op('pe', lambda e, sp_=sp_, bap=bap, bi=bi: e.matmul(sp_[:, :], ident[:], bap, start=False,
                                                                             stop=(bi == len(biases) - 1)),
                             reads=[("ident",), bres], writes=[spr])
                    et, etr = e_p.next()
                    T.op('act', lambda e, sp_=sp_, et=et: e.activation(out=et[:], in_=sp_[:, :], func=AF.Exp, scale=scale),
                         reads=[spr], writes=[etr])
                    T.op('pe', lambda e, et=et, vs=vs, kb=kb, i=i: e.matmul(num[0:e_, :], vhs[vs][:, kb, 0:e_], et[:],
                                                                          start=(i == 0), stop=(i == n - 1)),
                         reads=[("vh", vs), etr], writes=[numr])
                    T.op('pe', lambda e, et=et, i=i: e.matmul(den[0:e_, :], onesb[:, 0:e_], et[:],
                                                            start=(i == 0), stop=(i == n - 1)),
                         reads=[("onesb",), etr], writes=[denr])

            def finalize_simple(accs, e_, yrow, qb):
                num, numr, den, denr = accs
                r, rr = r_p.next()
                T.op('dve', lambda e: e.reciprocal(r[0:e_, :], den[0:e_, :]), reads=[denr], writes=[rr])
                yo, yor = yo_p.next()
                T.op('dve', lambda e: e.tensor_tensor(yo[0:e_, :], num[0:e_, :], r[0:e_, :], ALU.mult),
                     reads=[numr, rr], writes=[yor])
                T.dma('sp', YT_d[yrow:yrow + e_, qb * 512:(qb + 1) * 512], yo[0:e_, :], reads=[yor], key=("st_y",) + yor)

            T.dma('sp', maskt[:, 0:24, :], c_maskA[:, :].rearrange("p (j q) -> p j q", q=512), writes=[("mask",)], key="mask")
            for h in range(4):
                load_k(0, h * 64, 64)
                load_v(0, h * 64, 64)
                r0 = (l * 4 + h) * 128
                T.dma('pool', biast[:], biasA[r0:r0 + 128, :].rearrange("p (j q) -> p j q", q=512), writes=[("bias",)], key="bias")
                for qb in range(8):
                    q, qr = load_q(h * 64, 64, qb)
                    var = 0 if qb == 0 else (2 if qb == 7 else 1)
                    items = []
                    for j in range(8):
                        kb = 4 * qb - 2 + j
                        if 0 <= kb < 32:
                            items.append((0, 64, q, qr, kb, [(biast[:, j, :], ("bias",)), (maskt[:, var * 8 + j, :], ("mask",))], 0))
                    num, numr = Apool.next()
                    den, denr = Apool.next()
                    attn_tiles(items, 64, (num, numr, den, denr), 1.0)
                    finalize_simple((num, numr, den, denr), 64, h * 64, qb)
            T.dma('sp', maskt[:, :, :], c_maskB[:, :].rearrange("p (j q) -> p j q", q=512), writes=[("mask",)], key="mask")
            for u in range(6):
                load_k(u, 256 + u * 64, 64)
                load_v(u, 256 + u * 64, 64)
            for jo in range(2):
                for qb in range(8):
                    items = []
                    for g in range(3):
                        u = 2 * g + jo
                        q, qr = load_q(256 + u * 64, 64, qb)
                        for jb in range(B_JB[g][0], B_JB[g][1] + 1):
                            kb = 4 * qb + jb
                            if 0 <= kb < 32:
                                mi = B_OFF[g] + jb - B_JB[g][0]
                                items.append((u, 64, q, qr, kb, [(maskt[:, mi, :], ("mask",))], u))
                    num, numr = Apool.next()
                    den, denr = Apool.next()
                    attn_tiles(items, 64, (num, numr, den, denr), 0.125)
                    finalize_simple((num, numr, den, denr), 64, 256 + jo * 64, qb)
            T.dma('sp', lamt[:, 0:192], dlam[l:l + 1, :], writes=[("lamt",)], key="lam")
            T.op('dve', lambda e: e.tensor_tensor(lamt[:, 0:48], lamt[:, 0:48], lamt[:, 48:96], ALU.mult),
                 reads=[("lamt",)], writes=[("lamt", 0)])
            T.op('dve', lambda e: e.tensor_tensor(lamt[:, 48:96], lamt[:, 96:144], lamt[:, 144:192], ALU.mult),
                 reads=[("lamt",)], writes=[("lamt", 1)])
            T.op('dve', lambda e: e.reduce_sum(lamt[:, 192:194], lamt[:, 0:96].rearrange("p (a b) -> p a b", b=48), AX.X),
                 reads=[("lamt", 0), ("lamt", 1)], writes=[("lamt", 2)])
            T.op('act', lambda e: e.activation(out=lamt[:, 194:196], in_=lamt[:, 192:194], func=AF.Exp),
                 reads=[("lamt", 2)], writes=[("lamt", 3)])
            T.op('dve', lambda e: e.tensor_tensor(lamt[:, 196:197], lamt[:, 194:195], lamt[:, 195:196], ALU.subtract),
                 reads=[("lamt", 3)], writes=[("lamt", 4)])
            T.op('dve', lambda e: e.tensor_scalar(lamt[:, 197:198], lamt[:, 196:197], -1.0, -lam_init, ALU.mult, ALU.add),
                 reads=[("lamt", 4)], writes=[("lamt", 5)])
            T.dma('sp', lam_d[0:1, 0:1], lamt[:, 197:198], reads=[("lamt", 5)], writes=[("lam_d",)], key="lam2")
            T.dma('sp', nlam[:], lam_d[0, 0:1].partition_broadcast(96), reads=[("lam_d",)], writes=[("nlam",)], key="lam3")
            T.dma('sp', gpr[:], dng[l * 96:(l + 1) * 96, :], writes=[("gpr",)], key="lam4")
            T.op('dve', lambda e: e.tensor_scalar(gpr[:], gpr[:], 1.0 - lam_init, None, ALU.mult),
                 reads=[("gpr",)], writes=[("gpr",)])
            for h in range(4):
                for c in range(2):
                    load_k(c, 640 + h * 96 + c * 48, 48)
                load_v(0, 640 + h * 96, 96)
                for qb in range(8):
                    accs = []
                    for c in range(2):
                        q, qr = load_q(640 + h * 96 + c * 48, 48, qb)
                        num, numr = Apool.next()
                        den, denr = Apool.next()
                        items = [(c, 48, q, qr, kb, [], 0) for kb in range(32)]
                        attn_tiles(items, 96, (num, numr, den, denr), 48 ** -0.5)
                        accs.append((num, numr, den, denr))
                    fs = []
                    for c in range(2):
                        num, numr, den, denr = accs[c]
                        r, rr = r_p.next()
                        T.op('dve', lambda e, r=r, den=den: e.reciprocal(r[:, :], den[0:96, :]), reads=[denr], writes=[rr])
                        f, fr = f_p.next()
                        T.op('dve', lambda e, f=f, num=num, r=r: e.tensor_tensor(f[:, :], num[0:96, :], r[:, :], ALU.mult),
                             reads=[numr, rr], writes=[fr])
                        fs.append((f, fr))
                    o, orr = f_p.next()
                    T.op('dve', lambda e: e.scalar_tensor_tensor(o[:, :], fs[1][0][:, :], nlam[:, 0:1], fs[0][0][:, :], ALU.mult, ALU.add),
                         reads=[fs[0][1], fs[1][1], ("nlam",)], writes=[orr])
                    sq, sqr = sq_p.next()
                    T.op('pool', lambda e: e.tensor_tensor(sq[:, :], o[:, :], o[:, :], ALU.mult), reads=[orr], writes=[sqr])
                    ms, msr = Spool.next()
                    T.op('pe', lambda e: e.matmul(ms[0:96, :], onesb[0:96, 0:96], sq[:, :], start=True, stop=True),
                         reads=[("onesb",), sqr], writes=[msr])
                    lnv, lnr = f_p.next()
                    T.op('act', lambda e: e.activation(out=lnv[:, :], in_=ms[0:96, :], func=AF.Ln, scale=1.0 / 96.0, bias=LN_EPS),
                         reads=[msr], writes=[lnr])
                    T.op('act', lambda e: e.activation(out=lnv[:, :], in_=lnv[:, :], func=AF.Exp, scale=-0.5),
                         reads=[lnr], writes=[lnr])
                    yo, yor = yo_p.next()
                    T.op('dve', lambda e: e.scalar_tensor_tensor(yo[:, :], o[:, :], gpr[:, 0:1], lnv[:, :], ALU.mult, ALU.mult),
                         reads=[orr, lnr, ("gpr",)], writes=[yor])
                    T.dma('sp', YT_d[384 + h * 96:384 + (h + 1) * 96, qb * 512:(qb + 1) * 512], yo[:, :], reads=[yor], key=("st_y",) + yor)
            T.barrier()

        with ExitStack() as st:
            wg = sb(st, "c_wg", [128, 8, 3072], BF16)
            wp = sb(st, "c_wp", [128, 6, D], BF16)
            wo = sb(st, "c_wo", [128, 8, D], BF16)
            bgt = sb(st, "c_bg", [128, 24], F32)
            g_t = sb(st, "c_lng", [128, D], F32)
            b_t = sb(st, "c_lnb", [128, D], F32)
            for kc in range(8):
                T.dma('pool', wg[:, kc, :], w_in[l * D + kc * 128:l * D + (kc + 1) * 128, 3072:6144],
                      writes=[("wg", kc)], key=("wg", kc % 2))
                T.dma('pool', wo[:, kc, :], w_out[l * D + kc * 128:l * D + (kc + 1) * 128, :], writes=[("wo", kc)], key=("wo", kc % 2))
            for c in range(2):
                T.dma('pool', wp[:, c, :], w_pa[l * 256 + c * 128:l * 256 + (c + 1) * 128, :], writes=[("wp", c)], key="wp")
            T.dma('pool', wp[:, 2, :], w_pb[l * 128:(l + 1) * 128, :], writes=[("wp", 2)], key="wp")
            for c in range(3):
                T.dma('pool', wp[:, 3 + c, :], w_pc[l * 384 + c * 128:l * 384 + (c + 1) * 128, :], writes=[("wp", 3 + c)], key="wp")
            T.dma('sp', bgt[:], bgT[l * 128:(l + 1) * 128, :], writes=[("bgt",)], key="cmisc")
            T.dma('sp', g_t[:], ln1g[l, :].partition_broadcast(128), writes=[("lng",)], key="cmisc")
            T.dma('sp', b_t[:], ln1b[l, :].partition_broadcast(128), writes=[("lnb",)], key="cmisc")
            xts = [sb(st, "c_xt%d" % i, [128, 8, 512], BF16) for i in range(2)]
            yts = [sb(st, "c_yt%d" % i, [128, 6, 512], BF16) for i in range(2)]
            gts = [sb(st, "c_g%d" % i, [128, 512], F32) for i in range(3)]
            mts = [sb(st, "c_m%d" % i, [128, 512], F32) for i in range(4)]
            mgs = [sb(st, "c_mg%d" % i, [128, 8, 512], BF16) for i in range(2)]
            xrs = [sb(st, "c_xr%d" % i, [128, D], F32) for i in range(2)]
            zs = [sb(st, "c_z%d" % i, [128, D], F32) for i in range(2)]
            lnpools = (Rot("c_st", [sb(st, "c_st%d" % i, [128, 12], F32) for i in range(2)]),
                       Rot("c_mv", [sb(st, "c_mv%d" % i, [128, 4], F32) for i in range(2)]),
                       Rot("c_xo", [sb(st, "c_xo%d" % i, [128, D], F32) for i in range(2)]),
                       Rot("c_xb", [sb(st, "c_xb%d" % i, [128, D], BF16) for i in range(2)]),
                       Rot("c_xT", [sb(st, "c_xT%d" % i, [128, D], BF16) for i in range(2)]))
            xt_p, yt_p, g_p, m_p, mg_p, xr_p, z_p = (Rot("c_xt", xts), Rot("c_yt", yts), Rot("c_g", gts), Rot("c_m", mts),
                                                     Rot("c_mg", mgs), Rot("c_xr", xrs), Rot("c_z", zs))
            br_chunks = [(0, 2), (2, 3), (3, 6)]
            alpha = (2 * depth) ** 0.25
            for tb in range(NTB):
                c0 = tb * 512
                xt, xtr = xt_p.next()
                T.dma('sp', xt[:], xT_d[:, c0:c0 + 512].rearrange("(kc p) t -> p kc t", p=128), writes=[xtr], key=("c_xt", xtr))
                yt, ytr = yt_p.next()
                T.dma('sp', yt[:], YT_d[:, c0:c0 + 512].rearrange("(c p) t -> p c t", p=128), writes=[ytr], key=("c_yt", ytr))
                mg, mgr = mg_p.next()
                for fc in range(8):
                    ms_ = []
                    for i in range(3):
                        pg, pgr = Spool.next()
                        for kc in range(8):
                            T.op('pe', lambda e, kc=kc, pg=pg, i=i: e.matmul(
                                pg[:, :], wg[:, kc, i * D + fc * 128:i * D + (fc + 1) * 128], xt[:, kc, :],
                                start=(kc == 0), stop=(kc == 7)), reads=[("wg", kc), xtr], writes=[pgr])
                        g, gr = g_p.next()
                        T.op('act', lambda e, pg=pg, g=g, i=i: e.activation(out=g[:], in_=pg[:, :], func=AF.Sigmoid,
                                                                         bias=bgt[:, i * 8 + fc:i * 8 + fc + 1]),
                             reads=[pgr, ("bgt",)], writes=[gr])
                        pp, ppr = Spool.next()
                        cs = list(range(*br_chunks[i]))
                        for ci, c in enumerate(cs):
                            T.op('pe', lambda e, c=c, ci=ci, pp=pp, cs=cs: e.matmul(
                                pp[:, :], wp[:, c, fc * 128:(fc + 1) * 128], yt[:, c, :],
                                start=(ci == 0), stop=(ci == len(cs) - 1)), reads=[("wp", c), ytr], writes=[ppr])
                        m, mr = m_p.next()
                        T.op('dve', lambda e, m=m, g=g, pp=pp: e.tensor_tensor(m[:], pp[:, :], g[:], ALU.mult),
                             reads=[ppr, gr], writes=[mr])
                        ms_.append((m, mr))
                    T.op('dve', lambda e, ms_=ms_: e.tensor_tensor(ms_[0][0][:], ms_[0][0][:], ms_[1][0][:], ALU.add),
                         reads=[ms_[0][1], ms_[1][1]], writes=[ms_[0][1]])
                    T.op('dve', lambda e, ms_=ms_, fc=fc: e.tensor_tensor(mg[:, fc, :], ms_[0][0][:], ms_[2][0][:], ALU.add),
                         reads=[ms_[0][1], ms_[2][1]], writes=[mgr + (fc,)])
                for tt in range(4):
                    row0 = c0 + tt * 128
                    xr, xrr = xr_p.next()
                    T.dma('sp', xr[:], x_cur[row0:row0 + 128, :], writes=[xrr], key=("c_xr", xrr))
                    z, zr = z_p.next()
                    for half in range(2):
                        py, pyr = Apool.next()
                        for fc in range(8):
                            T.op('pe', lambda e, fc=fc, py=py, half=half: e.matmul(
                                py[:, :], mg[:, fc, tt * 128:(tt + 1) * 128], wo[:, fc, half * 512:(half + 1) * 512],
                                start=(fc == 0), stop=(fc == 7)), reads=[mgr + (fc,), ("wo", fc)], writes=[pyr])
                        T.op('dve', lambda e, py=py, half=half: e.scalar_tensor_tensor(
                            z[:, half * 512:(half + 1) * 512], xr[:, half * 512:(half + 1) * 512], alpha, py[:, :], ALU.mult, ALU.add),
                            reads=[pyr, xrr], writes=[zr + (half,)])
                    ln_epilogue(st, z, [zr + (0,), zr + (1,)], g_t, b_t, row0, x1_d, x1T_d, lnpools, dst_b16=x1b_d)
            T.barrier()

        alpha = (2 * depth) ** 0.25
        dst_f32 = y_out if last else x2_d
        dstT = None if last else xT_d
        IND = bass.IndirectOffsetOnAxis
        with ExitStack() as stD:
            gT = sb(stD, "d_gT", [E, S], BF16)
            idxk = sb(stD, "d_idxk", [128, 4, 32], I32)
            wk = sb(stD, "d_wk", [128, 4, 32], F32)
            idxw = sb(stD, "d_idxw", [128, 8, 64], I32)
            idxb = sb(stD, "d_idxb", [128, 64], I32)
            with ExitStack() as st:
                wr = sb(st, "e_wr", [128, 8, E], BF16)
                brt = sb(st, "e_br", [128, E], F32)
                tri = sb(st, "e_tri", [128, 128], BF16)
                iop = sb(st, "e_iop", [128, 1], F32)
                T.dma('pool', wr[:], w_r[l * D:(l + 1) * D, :].rearrange("(kc p) e -> p kc e", p=128), writes=[("wr",)], key="emisc_p")
                T.dma('sp', brt[:], b_r[l, :].partition_broadcast(128), writes=[("brt",)], key="emisc")
                T.dma('sp', tri[:], c_tri[:, :], writes=[("tri",)], key="emisc")
                T.dma('sp', iop[:], c_iop[:, :], writes=[("iop",)], key="emisc")
                xq = sb(st, "e_xq", [128, 8, 1024], BF16)
                lgr = sb(st, "e_lgr", [128, 32, E], F32)
                lge = sb(st, "e_lge", [128, 32, E], F32)
                mk = sb(st, "e_mk", [128, 32, E], F32)
                mkb = sb(st, "e_mkb", [128, 32, E], BF16)
                gates = sb(st, "e_gates", [128, 32, E], F32)
                gbf = sb(st, "e_gbf", [128, 8, E], BF16)
                mx8 = sb(st, "e_mx8", [128, 32, 8], F32)
                sm = sb(st, "e_sm", [128, 32, 4], F32)
                pos = sb(st, "e_pos", [128, 32, E], F32)
                posf = sb(st, "e_posf", [128, 32, E], F32)
                oh = sb(st, "e_oh", [128, 32, E], F32)
                tmp = sb(st, "e_tmp", [128, 32, E], F32)
                tmp2 = sb(st, "e_tmp2", [128, 32, E], F32)
                cnt = sb(st, "e_cnt", [128, E], F32)
                scA = sb(st, "e_scA", [128, E], F32)
                scB = sb(st, "e_scB", [128, E], F32)
                ntl = sb(st, "e_ntl", [128, E], F32)
                off = sb(st, "e_off", [128, E], F32)
                cmp = sb(st, "e_cmp", [128, 64, E], F32)
                eidf = sb(st, "e_eidf", [128, 64], F32)
                ef2 = sb(st, "e_ef2", [128, 64], F32)
                pkf = sb(st, "e_pkf", [128, 4, 32], F32)
                xbs = [sb(st, "e_xb%d" % i, [128, D], BF16) for i in range(2)]
                xb_p = Rot("e_xb", xbs)
                for qi in range(4):
                    t0 = qi * 1024
                    T.dma('sp', xq[:], x1T_d[:, t0:t0 + 1024].rearrange("(kc p) t -> p kc t", p=128), writes=[("xq",)], key="xq")
                    for tt in range(8):
                        gtt = qi * 8 + tt
                        pl, plr = Spool.next()
                        for kc in range(8):
                            T.op('pe', lambda e, kc=kc, pl=pl: e.matmul(pl[:, 0:E], xq[:, kc, tt * 128:(tt + 1) * 128], wr[:, kc, :],
                                                                     start=(kc == 0), stop=(kc == 7)),
                                 reads=[("xq",), ("wr",)], writes=[plr])
                        R = lambda n: ("rt", n, gtt)
                        T.op('dve', lambda e, pl=pl: e.tensor_tensor(lgr[:, gtt, :], pl[:, 0:E], brt[:], ALU.add),
                             reads=[plr, ("brt",)], writes=[R("lg")])
                        T.op('dve', lambda e: e.max(mx8[:, gtt, :], lgr[:, gtt, :]), reads=[R("lg")], writes=[R("mx")])
                        T.op('dve', lambda e: e.tensor_scalar(mk[:, gtt, :], lgr[:, gtt, :], mx8[:, gtt, 3:4], None, ALU.is_ge),
                             reads=[R("lg"), R("mx")], writes=[R("mk")])
                        T.op('dve', lambda e: e.tensor_scalar(sm[:, gtt, 0:1], mx8[:, gtt, 0:1], -1.0, None, ALU.mult),
                             reads=[R("mx")], writes=[R("nm")])
                        T.op('act', lambda e: e.activation(out=lge[:, gtt, :], in_=lgr[:, gtt, :], func=AF.Exp, bias=sm[:, gtt, 0:1]),
                             reads=[R("lg"), R("nm")], writes=[R("le")])
                        T.op('act', lambda e: e.activation(out=mkb[:, gtt, :], in_=mk[:, gtt, :], func=AF.Copy),
                             reads=[R("mk")], writes=[("mkb", gtt)])
                        T.op('dve', lambda e: e.tensor_tensor(lge[:, gtt, :], mk[:, gtt, :], lge[:, gtt, :], ALU.mult),
                             reads=[R("le"), R("mk")], writes=[R("le")])
                        T.op('dve', lambda e: e.reduce_sum(sm[:, gtt, 1:2], lge[:, gtt, :], AX.X), reads=[R("le")], writes=[R("ss")])
                        T.op('dve', lambda e: e.reciprocal(sm[:, gtt, 2:3], sm[:, gtt, 1:2]), reads=[R("ss")], writes=[R("rs")])
                        T.op('dve', lambda e: e.tensor_scalar(gates[:, gtt, :], lge[:, gtt, :], sm[:, gtt, 2:3], None, ALU.mult),
                             reads=[R("le"), R("rs")], writes=[("gates", gtt)])
                        T.op('act', lambda e: e.activation(out=gbf[:, tt, :], in_=gates[:, gtt, :], func=AF.Copy),
                             reads=[("gates", gtt)], writes=[("gbf", tt)])
                        T.op('pe', lambda e: e.transpose(psT[0:E, tt * 128:(tt + 1) * 128], gbf[:, tt, :], ident[:]),
                             reads=[("gbf", tt), ("ident",)], writes=[("psT", tt)])
                    T.op('act', lambda e: e.activation(out=gT[:, t0:t0 + 1024], in_=psT[0:E, :], func=AF.Copy),
                         reads=[("psT", tt) for tt in range(8)], writes=[("gT", qi)])
                for i in range(32):
                    bank = psS[i // 16]
                    c = (i % 16) * E
                    for j in range(i + 1):
                        lhs, lres = (onesb, ("onesb",)) if j < i else (tri, ("tri",))
                        T.op('pe', lambda e, bank=bank, c=c, lhs=lhs, j=j, i=i: e.matmul(
                            bank[:, c:c + E], lhs[:, :], mkb[:, j, :], start=(j == 0), stop=(j == i)),
                            reads=[("mkb", j), lres], writes=[("psS", i // 16)])
                for j in range(32):
                    T.op('pe', lambda e, j=j: e.matmul(psS[2][:, 0:E], onesb[:, :], mkb[:, j, :], start=(j == 0), stop=(j == 31)),
                         reads=[("mkb", j), ("onesb",)], writes=[("psS", 2)])
                pos2 = pos[:].rearrange("p a e -> p (a e)")
                T.op('act', lambda e: e.activation(out=pos2[:, 0:16 * E], in_=psS[0][:, 0:16 * E], func=AF.Copy),
                     reads=[("psS", 0)], writes=[("pos", 0)])
                T.op('dve', lambda e: e.tensor_copy(pos2[:, 16 * E:32 * E], psS[1][:, 0:16 * E]), reads=[("psS", 1)], writes=[("pos", 1)])
                T.op('dve', lambda e: e.tensor_copy(cnt[:], psS[2][:, 0:E]), reads=[("psS", 2)], writes=[("cnt",)])
                RS = [("rsx",)]
                T.op('dve', lambda e: e.tensor_scalar(ntl[:], cnt[:], 0.0, None, ALU.is_gt), reads=[("cnt",)], writes=RS)
                for m_ in range(1, 8):
                    T.op('dve', lambda e, m_=m_: e.scalar_tensor_tensor(ntl[:], cnt[:], float(TS * m_), ntl[:], ALU.is_gt, ALU.add),
                         reads=RS, writes=RS)
                T.op('dve', lambda e: e.tensor_copy(scA[:], ntl[:]), reads=RS, writes=RS)
                bufs = [scA, scB]
                for s_, d_ in enumerate((1, 2, 4, 8, 16)):
                    src_, dst_ = bufs[s_ % 2], bufs[(s_ + 1) % 2]
                    T.op('dve', lambda e, src_=src_, dst_=dst_, d_=d_: e.tensor_copy(dst_[:, 0:d_], src_[:, 0:d_]), reads=RS, writes=RS)
                    T.op('dve', lambda e, src_=src_, dst_=dst_, d_=d_: e.tensor_tensor(dst_[:, d_:E], src_[:, d_:E], src_[:, 0:E - d_], ALU.add),
                         reads=RS, writes=RS)
                cend = scB
                T.op('dve', lambda e: e.tensor_tensor(off[:], cend[:], ntl[:], ALU.subtract), reads=RS, writes=RS)
                T.op('dve', lambda e: e.tensor_scalar(off[:], off[:], float(TS), None, ALU.mult), reads=RS, writes=RS)
                for j in range(NT):
                    T.op('dve', lambda e, j=j: e.tensor_scalar(cmp[:, j, :], cend[:], float(j), None, ALU.is_le), reads=RS, writes=RS)
                T.op('dve', lambda e: e.reduce_sum(eidf[:, 0:NT], cmp[:, 0:NT, :], AX.X), reads=RS, writes=RS)
                T.op('dve', lambda e: e.tensor_scalar(eidf[:, 0:NT], eidf[:, 0:NT], float(E - 1), None, ALU.min), reads=RS, writes=RS)
                T.op('dve', lambda e: e.tensor_scalar(ef2[:, 0:NT], eidf[:, 0:NT], float(D), None, ALU.mult), reads=RS, writes=RS)
                T.op('dve', lambda e: e.tensor_scalar(ef2[:, 0:NT], ef2[:, 0:NT], iop[:, 0:1], None, ALU.add), reads=RS + [("iop",)], writes=RS)
                for kc in range(8):
                    T.op('dve', lambda e, kc=kc: e.tensor_scalar(idxw[:, kc, 0:NT], ef2[:, 0:NT], float(l * E * D + kc * 128), None, ALU.add),
                         reads=RS, writes=[("idxw",)])
                T.op('dve', lambda e: e.tensor_scalar(ef2[:, 0:NT], eidf[:, 0:NT], 128.0, None, ALU.mult), reads=RS + [("idxw",)], writes=RS)
                T.op('dve', lambda e: e.tensor_scalar(ef2[:, 0:NT], ef2[:, 0:NT], iop[:, 0:1], None, ALU.add), reads=RS, writes=RS)
                T.op('dve', lambda e: e.tensor_scalar(idxb[:, 0:NT], ef2[:, 0:NT], float(l * E * 128), None, ALU.add), reads=RS, writes=[("idxb",)])
                T.op('dve', lambda e: e.tensor_tensor(posf[:], pos[:], off[:].unsqueeze(1).to_broadcast([128, 32, E]), ALU.add),
                     reads=RS + [("pos", 0), ("pos", 1)], writes=[("posf",)])
                all_lg = [("rt", "lg", g_) for g_ in range(32)] + [("rt", "mx", g_) for g_ in range(32)]
                all_gates = [("gates", g_) for g_ in range(32)]
                for k in range(4):
                    T.op('dve', lambda e, k=k: e.tensor_tensor(oh[:], lgr[:], mx8[:, :, k:k + 1].to_broadcast([128, 32, E]), ALU.is_equal),
                         reads=all_lg, writes=[("oh",)])
                    T.op('dve', lambda e: e.tensor_tensor(tmp[:], oh[:], posf[:], ALU.mult), reads=[("oh",), ("posf",)], writes=[("tmp",)])
                    T.op('dve', lambda e, k=k: e.reduce_sum(pkf[:, k, :], tmp[:], AX.X), reads=[("tmp",)], writes=[("pkf", k)])
                    T.op('dve', lambda e: e.tensor_tensor(tmp2[:], oh[:], gates[:], ALU.mult), reads=[("oh",)] + all_gates, writes=[("tmp2",)])
                    T.op('dve', lambda e, k=k: e.reduce_sum(wk[:, k, :], tmp2[:], AX.X), reads=[("tmp2",)], writes=[("wk", k)])
                T.op('dve', lambda e: e.tensor_copy(idxk[:], pkf[:]), reads=[("pkf", k) for k in range(4)], writes=[("idxk",)])
                for gtt in range(32):
                    xb, xbr = xb_p.next()
                    T.dma('sp', xb[:], x1b_d[gtt * 128:(gtt + 1) * 128, :], writes=[xbr], key=("e_xb", xbr))
                    for k in range(4):
                        T.idma(out=XS_d[:, :], out_off=IND(ap=idxk[:, k, gtt:gtt + 1], axis=0), in_=xb[:, :], bound=R_XS - 1,
                               reads=[xbr, ("idxk",)], key="scat")
                T.barrier()

            with ExitStack() as st:
                wgs = [sb(st, "x_wg%d" % i, [128, 8, D], BF16) for i in range(2)]
                wus = [sb(st, "x_wu%d" % i, [128, 8, D], BF16) for i in range(2)]
                wds = [sb(st, "x_wd%d" % i, [128, 8, D], BF16) for i in range(2)]
                bgs = [sb(st, "x_bg%d" % i, [128, 8], F32) for i in range(2)]
                bus = [sb(st, "x_bu%d" % i, [128, 8], F32) for i in range(2)]
                xsrs = [sb(st, "x_xsr%d" % i, [128, 4, D], BF16) for i in range(2)]
                xsTs = [sb(st, "x_xsT%d" % i, [128, 8, TS], BF16) for i in range(2)]
                hhs = [sb(st, "x_hh%d" % i, [128, 8, TS], BF16) for i in range(2)]
                hgs = [sb(st, "x_hg%d" % i, [128, 512], F32) for i in range(2)]
                sgs = [sb(st, "x_sg%d" % i, [128, 512], F32) for i in range(2)]
                hus = [sb(st, "x_hu%d" % i, [128, 512], F32) for i in range(2)]
                yrs = [sb(st, "x_yr%d" % i, [128, D], BF16) for i in range(3)]
                wg_p, wu_p, wd_p, bg_p, bu_p = Rot("x_wg", wgs), Rot("x_wu", wus), Rot("x_wd", wds), Rot("x_bg", bgs), Rot("x_bu", bus)
                xsr_p, xsT_p, hh_p, hg_p, sg_p, hu_p, yr_p = (Rot("x_xsr", xsrs), Rot("x_xsT", xsTs), Rot("x_hh", hhs), Rot("x_hg", hgs),
                                                              Rot("x_sg", sgs), Rot("x_hu", hus), Rot("x_yr", yrs))

                def prep(j):
                    P = {}
                    for nm, pool_, src_ in (("wg", wg_p, w_eg), ("wu", wu_p, w_eu), ("wd", wd_p, w_ed)):
                        wt, wres = pool_.next()
                        for kc in range(8):
                            T.idma(out=wt[:, kc, :], in_=src_[:, :], in_off=IND(ap=idxw[:, kc, j:j + 1], axis=0), bound=depth * E * D - 1,
                                   reads=[("idxw",)], writes=[wres + (kc,)], key=wres)
                        P[nm] = (wt, wres)
                    for nm, pool_, src_ in (("bg", bg_p, begP), ("bu", bu_p, beuP)):
                        bt, bres = pool_.next()
                        T.idma(out=bt[:, :], in_=src_[:, :], in_off=IND(ap=idxb[:, j:j + 1], axis=0), bound=depth * E * 128 - 1,
                               reads=[("idxb",)], writes=[bres], key="ebias")
                        P[nm] = (bt, bres)
                    xsr, xsrr = xsr_p.next()
                    T.dma('sp', xsr[:], XS_d[j * TS:(j + 1) * TS, :].rearrange("(a p) d -> p a d", p=128), writes=[xsrr], key=xsrr)
                    P["xsr"] = (xsr, xsrr)
                    return P

                nxt = prep(0)
                for j in range(NT):
                    P = nxt
                    xsr, xsrr = P["xsr"]
                    wgt, wut, wdt = P["wg"], P["wu"], P["wd"]
                    bg, bgr = P["bg"]
                    bu, bur = P["bu"]
                    xsT, xsTr = xsT_p.next()
                    for a in range(4):
                        for fc in range(8):
                            T.op('pe', lambda e, a=a, fc=fc: e.transpose(psT[:, fc * 128:(fc + 1) * 128], xsr[:, a, fc * 128:(fc + 1) * 128], ident[:]),
                                 reads=[xsrr, ("ident",)], writes=[("psT", fc)])
                        T.op('act', lambda e, a=a: e.activation(out=xsT[:, :, a * 128:(a + 1) * 128],
                                                                in_=psT[:].rearrange("p (fc t) -> p fc t", t=128), func=AF.Copy),
                             reads=[("psT", fc) for fc in range(8)], writes=[xsTr + (a,)])
                    if j + 1 < NT:
                        nxt = prep(j + 1)
                    xsT_res = [xsTr + (a,) for a in range(4)]
                    hh, hhr = hh_p.next()
                    for fc in range(8):
                        pg, pgr = Spool.next()
                        pu, pur = Spool.next()
                        for (pt, ptr, wt) in ((pg, pgr, wgt), (pu, pur, wut)):
                            for kc in range(8):
                                T.op('pe', lambda e, kc=kc, pt=pt, wt=wt, fc=fc: e.matmul(
                                    pt[:, :], wt[0][:, kc, fc * 128:(fc + 1) * 128], xsT[:, kc, :],
                                    start=(kc == 0), stop=(kc == 7)), reads=[wt[1] + (kc,)] + xsT_res, writes=[ptr])
                        hg, hgr = hg_p.next()
                        sg, sgr = sg_p.next()
                        hu, hur = hu_p.next()
                        T.op('dve', lambda e, pg=pg, hg=hg, fc=fc: e.tensor_scalar(hg[:], pg[:, :], bg[:, fc:fc + 1], SW_LIM, ALU.add, ALU.min),
                             reads=[pgr, bgr], writes=[hgr])
                        T.op('act', lambda e, hg=hg, sg=sg: e.activation(out=sg[:], in_=hg[:], func=AF.Sigmoid, scale=SW_ALPHA),
                             reads=[hgr], writes=[sgr])
                        T.op('dve', lambda e, pu=pu, hu=hu, fc=fc: e.tensor_scalar(hu[:], pu[:, :], bu[:, fc:fc + 1], SW_LIM, ALU.add, ALU.min),
                             reads=[pur, bur], writes=[hur])
                        T.op('dve', lambda e, hu=hu: e.tensor_scalar(hu[:], hu[:], -SW_LIM, 1.0, ALU.max, ALU.add),
                             reads=[hur], writes=[hur])
                        T.op('dve', lambda e, hg=hg, sg=sg: e.tensor_tensor(hg[:], hg[:], sg[:], ALU.mult),
                             reads=[hgr, sgr], writes=[hgr])
                        T.op('dve', lambda e, hg=hg, hu=hu, fc=fc: e.tensor_tensor(hh[:, fc, :], hg[:], hu[:], ALU.mult),
                             reads=[hgr, hur], writes=[hhr + (fc,)])
                    for a in range(4):
                        yr, yrr = yr_p.next()
                        for half in range(2):
                            py, pyr = Apool.next()
                            for fc in range(8):
                                T.op('pe', lambda e, fc=fc, py=py, half=half, a=a: e.matmul(
                                    py[:, :], hh[:, fc, a * 128:(a + 1) * 128], wdt[0][:, fc, half * 512:(half + 1) * 512],
                                    start=(fc == 0), stop=(fc == 7)),
                                    reads=[hhr + (fc,), wdt[1] + (fc,)], writes=[pyr])
                            if half == 0:
                                T.op('act', lambda e, py=py, yr=yr: e.activation(out=yr[:, 0:512], in_=py[:, :], func=AF.Copy),
                                     reads=[pyr], writes=[yrr + (0,)])
                            else:
                                T.op('dve', lambda e, py=py, yr=yr: e.tensor_copy(yr[:, 512:1024], py[:, :]), reads=[pyr], writes=[yrr + (1,)])
                        T.dma('sp', YS_d[j * TS + a * 128:j * TS + (a + 1) * 128, :], yr[:], reads=[yrr + (0,), yrr + (1,)], key=("st_ys",) + yrr)
                T.barrier()

            with ExitStack() as st:
                bdn = sb(st, "f_bdn", [E, D], BF16)
                g_t = sb(st, "f_lng", [128, D], F32)
                b_t = sb(st, "f_lnb", [128, D], F32)
                T.dma('pool', bdn[:], b_ed[l * E:(l + 1) * E, :], writes=[("bdn",)], key="emisc_p")
                T.dma('sp', g_t[:], ln2g[l, :].partition_broadcast(128), writes=[("lng",)], key="emisc")
                T.dma('sp', b_t[:], ln2b[l, :].partition_broadcast(128), writes=[("lnb",)], key="emisc")
                ygs = [sb(st, "f_yg%d" % i, [128, 4, D], BF16) for i in range(2)]
                xrs = [sb(st, "f_xr%d" % i, [128, D], F32) for i in range(2)]
                zs = [sb(st, "f_z%d" % i, [128, D], F32) for i in range(2)]
                lnpools = (Rot("f_st", [sb(st, "f_st%d" % i, [128, 12], F32) for i in range(2)]),
                           Rot("f_mv", [sb(st, "f_mv%d" % i, [128, 4], F32) for i in range(2)]),
                           Rot("f_xo", [sb(st, "f_xo%d" % i, [128, D], F32) for i in range(2)]),
                           Rot("f_xb", [sb(st, "f_xb%d" % i, [128, D], BF16) for i in range(2)]),
                           Rot("f_xT", [sb(st, "f_xT%d" % i, [128, D], BF16) for i in range(2)]))
                yg_p, xr_p, z_p = Rot("f_yg", ygs), Rot("f_xr", xrs), Rot("f_z", zs)
                for gtt in range(32):
                    row0 = gtt * 128
                    yg, ygr = yg_p.next()
                    for k in range(4):
                        T.idma(out=yg[:, k, :], in_=YS_d[:, :], in_off=IND(ap=idxk[:, k, gtt:gtt + 1], axis=0), bound=R_XS - 1,
                               reads=[("idxk",)], writes=[ygr + (k,)], key=ygr)
                    xr, xrr = xr_p.next()
                    T.dma('sp', xr[:], x1_d[row0:row0 + 128, :], writes=[xrr], key=xrr)
                    z, zr = z_p.next()
                    for half in range(2):
                        py, pyr = Apool.next()
                        T.op('pe', lambda e, py=py, half=half: e.matmul(py[:, :], gT[:, row0:row0 + 128],
                                                                      bdn[:, half * 512:(half + 1) * 512], start=True, stop=True),
                             reads=[("gT", gtt // 8), ("bdn",)], writes=[pyr])
                        T.op('dve', lambda e, py=py, half=half: e.scalar_tensor_tensor(
                            z[:, half * 512:(half + 1) * 512], xr[:, half * 512:(half + 1) * 512], alpha, py[:, :], ALU.mult, ALU.add),
                            reads=[pyr, xrr], writes=[zr + (half,)])
                    zres = [zr + (0,), zr + (1,)]
                    for k in range(4):
                        T.op('dve', lambda e, k=k: e.scalar_tensor_tensor(z[:], yg[:, k, :], wk[:, k, gtt:gtt + 1], z[:], ALU.mult, ALU.add),
                             reads=[ygr + (k,), ("wk", k)] + zres, writes=zres)
                    ln_epilogue(st, z, zres, g_t, b_t, row0, dst_f32, dstT, lnpools)
                T.barrier()
        x_cur = x2_d

    T.barrier()
    es.close()
    return nc


def _consts():
    bf = ml_dtypes.bfloat16
    c = {}
    c["c_ident"] = np.eye(128, dtype=np.float32).astype(bf)
    c["c_tri"] = np.triu(np.ones((128, 128), np.float32), k=1).astype(bf)
    c["c_iop"] = np.arange(128, dtype=np.float32).reshape(128, 1)
    t = np.arange(S, dtype=np.float32)

    def tab(dim, rows):
        inv = (1.0 / (np.float32(10000.0) ** (np.arange(0, dim, 2, dtype=np.float32) / np.float32(dim)))).astype(np.float32)
        ang = (t[:, None] * inv[None, :]).astype(np.float32)
        cs, sn = np.cos(ang).astype(np.float32), np.sin(ang).astype(np.float32)
        idx = (np.arange(rows) % dim) % (dim // 2)
        return np.ascontiguousarray(cs[:, idx].T), np.ascontiguousarray(sn[:, idx].T)

    c["c_cosB"], c["c_sinB"] = tab(64, 128)
    c["c_cosC"], c["c_sinC"] = tab(48, 96)
    k = np.arange(128)
    q = np.arange(512)
    mA = np.zeros((3, 8, 128, 512), np.float32)
    for var, qb in ((0, 0), (1, 3), (2, 7)):
        r0 = 8 * qb
        r = r0 + q // 64
        cc = q % 64
        rs = np.clip(r - 4, 0, 56)
        cs_ = np.clip(cc - 8, 0, 48)
        for j in range(8):
            kr = r0 - 4 + 2 * j + k // 64
            kc = k % 64
            ok = ((kr[:, None] >= rs[None, :]) & (kr[:, None] < rs[None, :] + 8) &
                  (kc[:, None] >= cs_[None, :]) & (kc[:, None] < cs_[None, :] + 16))
            mA[var, j] = np.where(ok, 0.0, NEG)
    c["c_maskA"] = np.ascontiguousarray(mA.reshape(24, 128, 512).transpose(1, 0, 2)).reshape(128, 24 * 512).astype(bf)
    mB = np.zeros((34, 128, 512), np.float32)
    for g, d in enumerate((1, 4, 16)):
        for jb in range(B_JB[g][0], B_JB[g][1] + 1):
            kk = jb * 128 + k
            diff = kk[:, None] - q[None, :]
            ok = (np.abs(diff) <= 64 * d) & (diff % d == 0)
            mB[B_OFF[g] + jb - B_JB[g][0]] = np.where(ok, 0.0, NEG)
    c["c_maskB"] = np.ascontiguousarray(mB.transpose(1, 0, 2)).reshape(128, 34 * 512).astype(bf)
    return c


def _bias_gather(na_rpb):
    L, H = na_rpb.shape[0], na_rpb.shape[1]
    k = np.arange(128)
    q = np.arange(512)
    j = np.arange(8)
    dr = (-4 + 2 * j[None, :, None] + k[:, None, None] // 64) - (q[None, None, :] // 64)
    dc = (k[:, None, None] % 64) - (q[None, None, :] % 64) + 0 * j[None, :, None]
    ri = np.clip(dr + 7, 0, 14)
    ci = np.clip(dc + 15, 0, 30)
    out = na_rpb[:, :, ri, ci]
    return np.ascontiguousarray(out).reshape(L * H * 128, 8 * 512)


def prepare_inputs(inp, depth, n_exp):
    f = lambda a: np.ascontiguousarray(np.asarray(a, dtype=np.float32))
    E = n_exp
    m = {}
    m["w_in"] = f(inp["w_in"]).reshape(depth * D, 6144)
    m["bgT"] = np.ascontiguousarray(f(inp["b_gates"]).reshape(depth, 24, 128).transpose(0, 2, 1)).reshape(depth * 128, 24)
    m["w_proj_a"] = f(inp["w_proj_a"]).reshape(depth * 256, D)
    m["w_proj_b"] = f(inp["w_proj_b"]).reshape(depth * 128, D)
    m["w_proj_c"] = f(inp["w_proj_c"]).reshape(depth * 384, D)
    m["w_out"] = f(inp["w_out"]).reshape(depth * D, D)
    m["biasA"] = _bias_gather(f(inp["na_rpb"]))
    m["diff_lambda"] = f(inp["diff_lambda"]).reshape(depth, 192)
    m["diff_norm_g"] = f(inp["diff_norm_g"]).reshape(depth * 96, 1)
    for k_ in ("ln1_g", "ln1_b", "ln2_g", "ln2_b"):
        m[k_] = f(inp[k_]).reshape(depth, D)
    m["w_router"] = f(inp["w_router"]).reshape(depth * D, E)
    m["b_router"] = f(inp["b_router"]).reshape(depth, E)
    m["w_exp_gate"] = f(inp["w_exp_gate"]).reshape(depth * E * D, D)
    m["w_exp_up"] = f(inp["w_exp_up"]).reshape(depth * E * D, D)
    m["w_exp_down"] = f(inp["w_exp_down"]).reshape(depth * E * D, D)
    m["begP"] = np.ascontiguousarray(f(inp["b_exp_gate"]).reshape(depth, E, 8, 128).transpose(0, 1, 3, 2)).reshape(depth * E * 128, 8)
    m["beuP"] = np.ascontiguousarray(f(inp["b_exp_up"]).reshape(depth, E, 8, 128).transpose(0, 1, 3, 2)).reshape(depth * E * 128, 8)
    m["b_exp_down"] = f(inp["b_exp_down"]).reshape(depth * E, D)
    m.update(_consts())
    return m


def kernel(**inputs):
    depth, n_exp, n_cores = 4, 32, 8
    x = np.asarray(inputs["x"], dtype=np.float32)
    shared = prepare_inputs(inputs, depth, n_exp)
    nc = build_program(depth, n_exp)
    in_maps = []
    for c in range(n_cores):
        m = dict(shared)
        m["x"] = np.ascontiguousarray(x[c])
        in_maps.append(m)
    res = run_bass_kernel_spmd(nc, in_maps, core_ids=list(range(n_cores)))
    return np.stack([np.asarray(res.results[c]["y"], dtype=np.float32) for c in range(n_cores)], axis=0)
```
